# Optimizing a Trainium2 kernel written in Bass

```python
import math
import jax, jax.numpy as jnp
from jax import lax
import numpy as np

D_MODEL = 1024
BATCH = 8
SEQ = 4096
DEPTH = 1

HEAD_DIM = 64
MIX_WIDTH = D_MODEL
SB_WIDTH = MIX_WIDTH // 2
SB_HEADS = SB_WIDTH // HEAD_DIM
DF_V_DIM = 2 * HEAD_DIM
DF_WIDTH = MIX_WIDTH - SB_WIDTH
DF_HEADS = DF_WIDTH // DF_V_DIM
DF_QK_WIDTH = DF_HEADS * 2 * HEAD_DIM
IN_PROJ_WIDTH = 3 * SB_WIDTH + 2 * DF_QK_WIDTH + DF_WIDTH
SCALE = HEAD_DIM ** -0.5
Q_BLOCK = 128
ALIBI_SLOPES = tuple(2.0 ** (-8.0 * (h + 1) / DF_HEADS) for h in range(DF_HEADS))
N_GROUPS = 4
EXPERTS_PER_GROUP = 4
N_EXPERTS = N_GROUPS * EXPERTS_PER_GROUP
TOP_K_IN_GROUP = 2
D_EXPERT = D_MODEL // 2
PLE_DIM = 256
NORM_EPS = 1e-6

kernel_name = 'hymba_stickbreak_diffattn_hmoe_ple'


def _rmsnorm(x, g):
    xf = x.astype(jnp.float32)
    y = xf * lax.rsqrt(jnp.mean(xf * xf, axis=-1, keepdims=True) + NORM_EPS)
    return (y * g.astype(jnp.float32)).astype(x.dtype)


def _stick_breaking_block(q_blk, k_pre, v_pre, q_start):
    n_keys = k_pre.shape[2]
    z = jnp.einsum('bhqd,bhkd->bhqk', q_blk, k_pre).astype(jnp.float32) * SCALE
    t_pos = q_start + jnp.arange(Q_BLOCK)[:, None]
    s_pos = jnp.arange(n_keys)[None, :]
    strict = s_pos < t_pos
    log_beta = jax.nn.log_sigmoid(z)
    log_keep = jnp.where(strict, jax.nn.log_sigmoid(-z), 0.0)
    log_survive = lax.cumsum(log_keep, axis=3, reverse=True) - log_keep
    weights = jnp.where(strict, jnp.exp(log_beta + log_survive), 0.0)
    return jnp.einsum('bhqk,bhkd->bhqd', weights.astype(v_pre.dtype), v_pre)


def _differential_block(q_blk, k_pre, v_pre, q_start, lam, slopes):
    n_keys = k_pre.shape[3]
    z = jnp.einsum('bhcqd,bhckd->bhcqk', q_blk, k_pre).astype(jnp.float32) * SCALE
    t_pos = q_start + jnp.arange(Q_BLOCK)[:, None]
    s_pos = jnp.arange(n_keys)[None, :]
    dist = (t_pos - s_pos).astype(jnp.float32)
    z = z - slopes[None, :, None, None, None] * dist
    z = jnp.where(s_pos <= t_pos, z, -jnp.inf)
    probs = jax.nn.softmax(z, axis=-1)
    attn = probs[:, :, 0] - lam * probs[:, :, 1]
    return jnp.einsum('bhqk,bhkd->bhqd', attn.astype(v_pre.dtype), v_pre)


def _hybrid_layer(x, p_i, layer_idx, g_mix, w_in, lambda_q1, lambda_k1, lambda_q2,
                  lambda_k2, g_sb_out, g_df_out, w_out, g_ffn, w_router_group,
                  b_router_group, w_router_expert, b_router_expert, w_expert_gate,
                  w_expert_up, w_expert_down, g_ple, w_ple_gate, w_ple_proj):
    bsz, seq, _ = x.shape
    h = _rmsnorm(x, g_mix)
    proj = h @ w_in
    q_sb, k_sb, v_sb, q_df, k_df, v_df = jnp.split(
        proj, [SB_WIDTH, 2 * SB_WIDTH, 3 * SB_WIDTH,
               3 * SB_WIDTH + DF_QK_WIDTH, 3 * SB_WIDTH + 2 * DF_QK_WIDTH], axis=-1)
    q_sb = q_sb.reshape(bsz, seq, SB_HEADS, HEAD_DIM).transpose(0, 2, 1, 3)
    k_sb = k_sb.reshape(bsz, seq, SB_HEADS, HEAD_DIM).transpose(0, 2, 1, 3)
    v_sb = v_sb.reshape(bsz, seq, SB_HEADS, HEAD_DIM).transpose(0, 2, 1, 3)
    q_df = q_df.reshape(bsz, seq, DF_HEADS, 2, HEAD_DIM).transpose(0, 2, 3, 1, 4)
    k_df = k_df.reshape(bsz, seq, DF_HEADS, 2, HEAD_DIM).transpose(0, 2, 3, 1, 4)
    v_df = v_df.reshape(bsz, seq, DF_HEADS, DF_V_DIM).transpose(0, 2, 1, 3)

    lambda_init = 0.8 - 0.6 * math.exp(-0.3 * layer_idx)
    lam = (jnp.exp(jnp.sum(lambda_q1.astype(jnp.float32) * lambda_k1.astype(jnp.float32)))
           - jnp.exp(jnp.sum(lambda_q2.astype(jnp.float32) * lambda_k2.astype(jnp.float32)))
           + lambda_init)
    slopes = jnp.asarray(ALIBI_SLOPES, dtype=jnp.float32)

    sb_blocks, df_blocks = [], []
    for blk in range(seq // Q_BLOCK):
        qs = blk * Q_BLOCK
        qe = qs + Q_BLOCK
        sb_blocks.append(_stick_breaking_block(q_sb[:, :, qs:qe], k_sb[:, :, :qe],
                                               v_sb[:, :, :qe], qs))
        df_blocks.append(_differential_block(q_df[:, :, :, qs:qe], k_df[:, :, :, :qe],
                                             v_df[:, :, :qe], qs, lam, slopes))
    o_sb = jnp.concatenate(sb_blocks, axis=2).transpose(0, 2, 1, 3).reshape(bsz, seq, SB_WIDTH)
    o_sb = _rmsnorm(o_sb, g_sb_out)
    o_df = jnp.concatenate(df_blocks, axis=2)
    o_df = _rmsnorm(o_df, g_df_out) * (1.0 - lambda_init)
    o_df = o_df.transpose(0, 2, 1, 3).reshape(bsz, seq, DF_WIDTH)
    x = x + jnp.concatenate([o_sb, o_df], axis=-1) @ w_out

    h2 = _rmsnorm(x, g_ffn).reshape(-1, D_MODEL)
    group_logits = (h2 @ w_router_group).astype(jnp.float32) + b_router_group.astype(jnp.float32)
    group_probs = jax.nn.softmax(group_logits, axis=-1)
    g_idx = jnp.argmax(group_logits, axis=-1)
    g_w = jnp.take_along_axis(group_probs, g_idx[:, None], axis=1)[:, 0]
    expert_logits = ((h2 @ w_router_expert).astype(jnp.float32)
                     + b_router_expert.astype(jnp.float32)).reshape(-1, N_GROUPS, EXPERTS_PER_GROUP)
    in_group = jnp.take_along_axis(expert_logits, g_idx[:, None, None], axis=1)[:, 0]
    top_vals, top_idx = lax.top_k(in_group, TOP_K_IN_GROUP)
    top_w = jax.nn.softmax(top_vals, axis=-1) * g_w[:, None]
    expert_ids = g_idx[:, None] * EXPERTS_PER_GROUP + top_idx
    gates = jnp.sum(jax.nn.one_hot(expert_ids, N_EXPERTS, dtype=jnp.float32)
                    * top_w[..., None], axis=1).astype(h2.dtype)
    y = jnp.zeros_like(h2)
    for e in range(N_EXPERTS):
        hid = jax.nn.silu(h2 @ w_expert_gate[e]) * (h2 @ w_expert_up[e])
        y = y + gates[:, e:e + 1] * (hid @ w_expert_down[e])
    x = x + y.reshape(bsz, seq, D_MODEL)

    gate = jax.nn.sigmoid(_rmsnorm(x, g_ple) @ w_ple_gate)
    x = x + gate * (p_i @ w_ple_proj)
    return x


def setup_inputs(seed: int = 0) -> dict:
    key = jax.random.key(seed)
    ks = jax.random.split(key, 23)
    f32 = jnp.float32

    def nrm(k, shape, scale):
        return jax.random.normal(k, shape, f32) * scale

    def gain(k, shape):
        return 1.0 + 0.02 * jax.random.normal(k, shape, f32)

    return {
        'x': nrm(ks[0], (BATCH, SEQ, D_MODEL), 1.0),
        'p': nrm(ks[1], (DEPTH, BATCH, SEQ, PLE_DIM), 1.0),
        'g_mix': gain(ks[2], (DEPTH, D_MODEL)),
        'w_in': nrm(ks[3], (DEPTH, D_MODEL, IN_PROJ_WIDTH), D_MODEL ** -0.5),
        'lambda_q1': nrm(ks[4], (DEPTH, HEAD_DIM), 0.1),
        'lambda_k1': nrm(ks[5], (DEPTH, HEAD_DIM), 0.1),
        'lambda_q2': nrm(ks[6], (DEPTH, HEAD_DIM), 0.1),
        'lambda_k2': nrm(ks[7], (DEPTH, HEAD_DIM), 0.1),
        'g_sb_out': gain(ks[8], (DEPTH, SB_WIDTH)),
        'g_df_out': gain(ks[9], (DEPTH, DF_V_DIM)),
        'w_out': nrm(ks[10], (DEPTH, MIX_WIDTH, D_MODEL), MIX_WIDTH ** -0.5),
        'g_ffn': gain(ks[11], (DEPTH, D_MODEL)),
        'w_router_group': nrm(ks[12], (DEPTH, D_MODEL, N_GROUPS), D_MODEL ** -0.5),
        'b_router_group': nrm(ks[13], (DEPTH, N_GROUPS), 0.01),
        'w_router_expert': nrm(ks[14], (DEPTH, D_MODEL, N_EXPERTS), D_MODEL ** -0.5),
        'b_router_expert': nrm(ks[15], (DEPTH, N_EXPERTS), 0.01),
        'w_expert_gate': nrm(ks[16], (DEPTH, N_EXPERTS, D_MODEL, D_EXPERT), D_MODEL ** -0.5),
        'w_expert_up': nrm(ks[17], (DEPTH, N_EXPERTS, D_MODEL, D_EXPERT), D_MODEL ** -0.5),
        'w_expert_down': nrm(ks[18], (DEPTH, N_EXPERTS, D_EXPERT, D_MODEL), D_EXPERT ** -0.5),
        'g_ple': gain(ks[19], (DEPTH, D_MODEL)),
        'w_ple_gate': nrm(ks[20], (DEPTH, D_MODEL, D_MODEL), D_MODEL ** -0.5),
        'w_ple_proj': nrm(ks[21], (DEPTH, PLE_DIM, D_MODEL), PLE_DIM ** -0.5),
        'g_final': gain(ks[22], (D_MODEL,)),
    }


def reference(x, p, g_mix, w_in, lambda_q1, lambda_k1, lambda_q2, lambda_k2, g_sb_out,
              g_df_out, w_out, g_ffn, w_router_group, b_router_group, w_router_expert,
              b_router_expert, w_expert_gate, w_expert_up, w_expert_down, g_ple,
              w_ple_gate, w_ple_proj, g_final):
    for i in range(DEPTH):
        x = _hybrid_layer(x, p[i], i, g_mix[i], w_in[i], lambda_q1[i], lambda_k1[i],
                          lambda_q2[i], lambda_k2[i], g_sb_out[i], g_df_out[i], w_out[i],
                          g_ffn[i], w_router_group[i], b_router_group[i],
                          w_router_expert[i], b_router_expert[i], w_expert_gate[i],
                          w_expert_up[i], w_expert_down[i], g_ple[i], w_ple_gate[i],
                          w_ple_proj[i])
    return _rmsnorm(x, g_final)
```

```python
import math
from contextlib import ExitStack

import numpy as np
import concourse.bass as bass
import concourse.mybir as mybir
from concourse.bass_utils import run_bass_kernel_spmd

F32 = mybir.dt.float32
BF16 = mybir.dt.bfloat16
AF = mybir.ActivationFunctionType
ALU = mybir.AluOpType
AX = mybir.AxisListType

D = 1024
NCH = 8
HD = 64
NEXP = 16
DEXP = 512
PLE = 256
EPS = 1e-6
SCALE = HD ** -0.5
SLOPES = [2.0 ** (-8.0 * (h + 1) / 4) for h in range(4)]
LAMBDA_INIT = 0.8 - 0.6 * math.exp(-0.3 * 0)
SAME_ENGINE_SYNC = True

C_ID, C_NEGU, C_ONES, C_NEGONES = 0, 128, 256, 384
C_KAUG = 512
C_QAUG = 1024
C_FULL = 3072
C_TRI = 3200
C_TRI2 = 3328
CW = 3456
MASK_BIG = 30000.0


def make_consts():
    c = np.zeros((128, CW), np.float32)
    j = np.arange(128)[:, None]
    s = np.arange(128)[None, :]
    c[:, C_ID:C_ID + 128] = (j == s)
    c[:, C_NEGU:C_NEGU + 128] = -(j >= s).astype(np.float32)
    c[:, C_ONES:C_ONES + 128] = 1.0
    c[:, C_NEGONES:C_NEGONES + 128] = -1.0
    c[:, C_FULL:C_FULL + 128] = -MASK_BIG
    c[:, C_TRI:C_TRI + 128] = np.where(s <= j, -MASK_BIG, 0.0)
    c[:, C_TRI2:C_TRI2 + 128] = np.where(s < j, -MASK_BIG, 0.0)
    tl = np.arange(512)
    for h in range(4):
        sl = SLOPES[h]
        k = c[:, C_KAUG + 128 * h:C_KAUG + 128 * (h + 1)]
        k[0, :] = sl * np.arange(128)
        k[1, :] = 1.0
        k[2, :] = 1.0
        q = c[:, C_QAUG + 512 * h:C_QAUG + 512 * (h + 1)]
        q[0, :] = 1.0
        q[1, :] = -sl * (tl % 128)
        q[2, :] = -sl * 128.0 * (tl // 128)
    return c


def make_aug(S):
    a = np.zeros((4, 2, 3, S), np.float32)
    t = np.arange(S)
    for h in range(4):
        sl = SLOPES[h]
        a[h, 0, 0] = 1.0
        a[h, 0, 1] = -sl * (t % 128)
        a[h, 0, 2] = -sl * 128.0 * ((t // 128) % 4)
        a[h, 1, 0] = sl * (t % 128)
        a[h, 1, 1] = 1.0
        a[h, 1, 2] = 1.0
    return a


class _Rec:
    def __getattr__(self, name):
        def f(*a, **k):
            return (name, a, k)
        return f


class FW:
    def __init__(self, nc, es):
        self.nc = nc
        self.es = es
        self.engs = {"pe": nc.tensor, "act": nc.scalar, "dve": nc.vector, "pool": nc.gpsimd, "sp": nc.sync}
        self.prog = {e: es.enter_context(nc.semaphore("prog_" + e)) for e in self.engs}
        self.cnt = {e: 0 for e in self.engs}
        self.waited = {e: {} for e in self.engs}
        self.dma_cnt = {}
        self.sems = {}
        self.reset()

    def reset(self):
        self.ops = {e: [] for e in self.engs}
        self.lastw = {}
        self.readers = {}

    def dsem(self, name):
        if name not in self.sems:
            self.sems[name] = self.es.enter_context(self.nc.semaphore("d_" + name))
            self.dma_cnt[name] = 0
        return name

    def emit(self, eng, fn, reads=(), writes=(), dma=None):
        rec = fn(_Rec())
        deps = []
        for r in reads:
            t = self.lastw.get(r)
            if t is not None:
                deps.append(t)
        for w in writes:
            t = self.lastw.get(w)
            if t is not None:
                deps.append(t)
            deps.extend(self.readers.get(w, {}).values())
        waits = {}
        for (skey, sem, val, teng) in deps:
            if teng == eng and (eng == "pe" or not SAME_ENGINE_SYNC):
                continue
            if self.waited[eng].get(skey, -1) >= val:
                continue
            if waits.get(skey, (None, -1))[1] < val:
                waits[skey] = (sem, val)
        for skey, (sem, val) in waits.items():
            self.waited[eng][skey] = val
        if dma is not None:
            self.dsem(dma)
            self.dma_cnt[dma] += 16
            tok = ("d_" + dma, self.sems[dma], self.dma_cnt[dma], None)
            inc = (self.sems[dma], 16)
        else:
            self.cnt[eng] += 1
            tok = ("p_" + eng, self.prog[eng], self.cnt[eng], eng)
            inc = (self.prog[eng], 1)
        for w in writes:
            self.lastw[w] = tok
            self.readers[w] = {}
        for r in reads:
            self.readers.setdefault(r, {})[tok[0]] = tok
        self.ops[eng].append((list(waits.values()), rec, inc))
        return tok

    def flush(self, name, final_waits=()):
        nc = self.nc
        ops = self.ops
        with nc.Block() as block:
            def mk(ename):
                def body(e):
                    for waits, rec, inc in ops[ename]:
                        for sem, val in waits:
                            e.wait_ge(sem, val)
                        inst = getattr(e, rec[0])(*rec[1], **rec[2])
                        inst.then_inc(inc[0], inc[1])
                    if ename == "sp":
                        for sem, val in final_waits:
                            e.wait_ge(sem, val)
                return body
            block.sync(mk("sp"))
            block.tensor(mk("pe"))
            block.scalar(mk("act"))
            block.vector(mk("dve"))
            block.gpsimd(mk("pool"))
        self.reset()


def build(S, dbg=False, stop=9):
    assert S % 1024 == 0
    NT = S // 128
    NQB = S // 512
    CH = 1024
    NCK = S // CH
    TPC = CH // 128

    nc = bass.Bass("TRN2", target_bir_lowering=False)

    def din(name, shape):
        return nc.dram_tensor(name, list(shape), F32, kind="ExternalInput").ap()

    x_d = din("x", [S, D])
    p_d = din("p", [S, PLE])
    cst_d = din("cst", [128, CW])
    aug_d = din("aug", [4, 2, 3, S])
    gmix_d = din("g_mix", [D])
    win_d = din("w_in", [D, 3072])
    lam_d = din("lam4", [4, HD])
    gsb_d = din("g_sb_out", [512])
    gdf_d = din("g_df_out", [128])
    wout_d = din("w_out", [D, D])
    gffn_d = din("g_ffn", [D])
    wr_d = din("w_router", [D, 20])
    br_d = din("b_router", [20])
    weg_d = din("w_expert_gate", [NEXP, D, DEXP])
    weu_d = din("w_expert_up", [NEXP, D, DEXP])
    wed_d = din("w_expert_down", [NEXP, DEXP, D])
    gple_d = din("g_ple", [D])
    wpg_d = din("w_ple_gate", [D, D])
    wpp_d = din("w_ple_proj", [PLE, D])
    gfin_d = din("g_final", [D])
    out_d = nc.dram_tensor("out", [S, D], F32, kind="ExternalOutput").ap()
    if dbg:
        dbg_o = nc.dram_tensor("dbg_o", [128, NCH, S], BF16, kind="ExternalOutput").ap()
        dbg_x1 = nc.dram_tensor("dbg_x1", [S, D], F32, kind="ExternalOutput").ap()

    with ExitStack() as es:
        fw = FW(nc, es)

        def sb(name, shape, dt, stack=es):
            return stack.enter_context(nc.sbuf_tensor(name, list(shape), dt))

        def pst(name, shape, dt, stack):
            return stack.enter_context(nc.psum_tensor(name, list(shape), dt))

        oT = sb("oT", [128, NCH, S], BF16)
        cb = sb("cb", [128, CW], BF16)
        identf = sb("identf", [128, 128], F32)
        gcols = sb("gcols", [128, 5, NCH], F32)
        lamt = sb("lamt", [128, 4, HD], F32)
        lamw = sb("lamw", [128, 8], F32)
        neglam = sb("neglam", [128, 1], F32)

        ident = cb[:, C_ID:C_ID + 128]
        negU = cb[:, C_NEGU:C_NEGU + 128]
        ones = cb[:, C_ONES:C_ONES + 128]
        negones = cb[:, C_NEGONES:C_NEGONES + 128]

        with ExitStack() as esA:
            hT = sb("hT", [128, NCH, S], BF16, esA)
            with ExitStack() as es0:
                xt = [sb(f"xt{i}", [128, D], F32, es0) for i in range(2)]
                xn = [sb(f"xn{i}", [128, D], BF16, es0) for i in range(2)]
                junk = sb("junk0", [128, D], BF16, es0)
                ssq = sb("ssq0", [128, NT], F32, es0)
                lnv = sb("lnv0", [128, NT], F32, es0)
                rstd = sb("rstd0", [128, NT], F32, es0)
                tp = [pst(f"tp{i}", [128, D], BF16, es0) for i in range(2)]

                fw.emit("pool", lambda e: e.dma_start(out=cb[:], in_=cst_d[:, :]), writes=["cb"], dma="cst")
                fw.emit("sp", lambda e: e.dma_start(out=identf[:], in_=cst_d[:, C_ID:C_ID + 128]), writes=["identf"], dma="cst2")
                gst = sb("gst", [8, 5, 128], F32, es0)
                gps = pst("gps", [128, 5, 8], F32, es0)
                for k, (gd, nr) in enumerate([(gmix_d, 8), (gffn_d, 8), (gple_d, 8), (gsb_d, 4), (gdf_d, 1)]):
                    fw.emit("sp", lambda e, k=k, gd=gd, nr=nr: e.dma_start(out=gst[0:nr, k, :], in_=gd.rearrange("(c p) -> c p", p=128)),
                            writes=[("gst", k)], dma="gc%d" % k)
                    fw.emit("pe", lambda e, k=k, nr=nr: e.transpose(out=gps[:, k, 0:nr], in_=gst[0:nr, k, :], identity=identf[0:nr, 0:nr]),
                            reads=[("gst", k), "identf"], writes=["gps"])
                    fw.emit("dve", lambda e, k=k, nr=nr: e.tensor_copy(out=gcols[:, k, 0:nr], in_=gps[:, k, 0:nr]),
                            reads=["gps"], writes=[("gcols", k)])
                fw.emit("dve", lambda e: e.tensor_scalar(out=gcols[:, 4, 0:1], in0=gcols[:, 4, 0:1], scalar1=float(1.0 - LAMBDA_INIT),
                                                          scalar2=None, op0=ALU.mult),
                        reads=[("gcols", 4)], writes=[("gcols", 4)])
                if True:
                    fw.emit("sp", lambda e: e.dma_start(out=lamt[:].rearrange("p a b -> p (a b)"),
                                                         in_=lam_d.rearrange("a b -> (a b)").partition_broadcast(128)),
                            writes=["lamt"], dma="lam")
                    fw.emit("dve", lambda e: e.tensor_tensor(out=lamt[:, 0, :], in0=lamt[:, 0, :], in1=lamt[:, 1, :], op=ALU.mult),
                            reads=["lamt"], writes=["lamt"])
                    fw.emit("dve", lambda e: e.tensor_tensor(out=lamt[:, 2, :], in0=lamt[:, 2, :], in1=lamt[:, 3, :], op=ALU.mult),
                            reads=["lamt"], writes=["lamt"])
                    fw.emit("dve", lambda e: e.reduce_sum(out=lamw[:, 0:1], in_=lamt[:, 0, :], axis=AX.X), reads=["lamt"], writes=["lamw"])
                    fw.emit("dve", lambda e: e.reduce_sum(out=lamw[:, 1:2], in_=lamt[:, 2, :], axis=AX.X), reads=["lamt"], writes=["lamw"])
                    fw.emit("act", lambda e: e.activation(out=lamw[:, 2:4], in_=lamw[:, 0:2], func=AF.Exp), reads=["lamw"], writes=["lamw"])
                    fw.emit("dve", lambda e: e.tensor_tensor(out=lamw[:, 4:5], in0=lamw[:, 3:4], in1=lamw[:, 2:3], op=ALU.subtract),
                            reads=["lamw"], writes=["lamw"])
                    fw.emit("dve", lambda e: e.tensor_scalar(out=neglam[:], in0=lamw[:, 4:5], scalar1=float(-LAMBDA_INIT), scalar2=None, op0=ALU.add),
                            reads=["lamw"], writes=["neglam"])

                for i in range(NT):
                    b = i % 2
                    fw.emit("sp", lambda e, i=i, b=b: e.dma_start(out=xt[b][:], in_=x_d[i * 128:(i + 1) * 128, :]),
                            writes=[("xt", b)], dma="xt%d" % b)
                    fw.emit("act", lambda e, i=i, b=b: e.activation(out=junk[:], in_=xt[b][:], func=AF.Square, accum_out=ssq[:, i:i + 1]),
                            reads=[("xt", b)], writes=["junk", ("ssq", i)])
                    fw.emit("act", lambda e, i=i: e.activation(out=lnv[:, i:i + 1], in_=ssq[:, i:i + 1], func=AF.Ln, scale=1.0 / D, bias=EPS),
                            reads=[("ssq", i)], writes=[("lnv", i)])
                    fw.emit("act", lambda e, i=i: e.activation(out=rstd[:, i:i + 1], in_=lnv[:, i:i + 1], func=AF.Exp, scale=-0.5),
                            reads=[("lnv", i)], writes=[("rstd", i)])
                    fw.emit("dve", lambda e, i=i, b=b: e.tensor_scalar(out=xn[b][:], in0=xt[b][:], scalar1=rstd[:, i:i + 1], scalar2=None, op0=ALU.mult),
                            reads=[("xt", b), ("rstd", i)], writes=[("xn", b)])
                    for c in range(NCH):
                        fw.emit("pe", lambda e, b=b, c=c: e.transpose(out=tp[b][:, c * 128:(c + 1) * 128], in_=xn[b][:, c * 128:(c + 1) * 128], identity=ident),
                                reads=[("xn", b), "cb"], writes=[("tp", b)])
                    for c in range(NCH):
                        if True:
                            fw.emit("dve", lambda e, i=i, b=b, c=c: e.tensor_scalar(out=hT[:, c, i * 128:(i + 1) * 128], in0=tp[b][:, c * 128:(c + 1) * 128],
                                                                                   scalar1=gcols[:, 0, c:c + 1], scalar2=None, op0=ALU.mult),
                                    reads=[("tp", b), ("gcols", 0)], writes=[("hT", i, c)])
                        else:
                            fw.emit("act", lambda e, i=i, b=b, c=c: e.activation(out=hT[:, c, i * 128:(i + 1) * 128], in_=tp[b][:, c * 128:(c + 1) * 128],
                                                                                func=AF.Copy, scale=gcols[:, 0, c:c + 1]),
                                    reads=[("tp", b), ("gcols", 0)], writes=[("hT", i, c)])
                if stop == 0:
                    fw.emit("sp", lambda e: e.dma_start(out=dbg_o[:, :, :], in_=hT[:]), reads=[("hT", i, c) for i in range(NT) for c in range(NCH)], dma="dbgo")
                    fw.flush("p0", final_waits=[(fw.sems["dbgo"], 16)])
                    return nc
                fw.flush("p0")

            with ExitStack() as esa:
                QA = [sb(f"QA{i}", [128, S], BF16, esa) for i in range(2)]
                KA = [sb(f"KA{i}", [128, S], BF16, esa) for i in range(2)]
                V = sb("V", [128, NT, 128], BF16, esa)
                wsl = sb("wsl", [128, NCH, 3, 128], BF16, esa)
                E2 = [sb(f"E{i}", [128, 1024], BF16, esa) for i in range(2)]
                SP2 = [sb(f"SP{i}", [128, 1024], BF16, esa) for i in range(2)]
                SC = [sb(f"SC{i}", [128, 512], BF16, esa) for i in range(2)]
                W2 = [sb(f"W{i}", [128, 1024], BF16, esa) for i in range(3)]
                W = [w[:, 0:512] for w in W2]
                OS = [sb(f"OS{i}", [128, 512], F32, esa) for i in range(2)]
                R1 = E2[0][:].bitcast(F32)
                R2 = E2[1][:].bitcast(F32)
                ZZ = [pst(f"ZZ{i}", [128, 1024], F32, esa) for i in range(3)]
                Zv = [ZZ[j // 2][:, (j % 2) * 512:(j % 2 + 1) * 512] for j in range(6)]
                Z = Zv[0:4]
                D1 = Zv[4]
                D2 = Zv[5]
                O1 = pst("O1", [128, 512], F32, esa)
                O2 = pst("O2", [128, 512], F32, esa)
                OB = [O1, O2]
                win_v = win_d.rearrange("(c p) n -> p c n", p=128)

                fw.emit("pool", lambda e: e.memset(QA[0][64:128, :], 0.0), writes=[("QA", 0)])
                fw.emit("pool", lambda e: e.memset(QA[1][0:64, :], 0.0), writes=[("QA", 1)])

                for g in range(8):
                    is_sb = g < 4
                    if is_sb:
                        offs = (128 * g, 512 + 128 * g, 1024 + 128 * g)
                    else:
                        h = g - 4
                        offs = (1536 + 128 * h, 2048 + 128 * h, 2560 + 128 * h)
                    for j in range(3):
                        fw.emit("pool", lambda e, j=j, o=offs[j]: e.dma_start(out=wsl[:, :, j, :], in_=win_v[:, :, o:o + 128]),
                                writes=[("wsl", j)], dma="wsl%d" % j)
                    if g == 4:
                        for m2 in range(2):
                            fw.emit("pool", lambda e, m2=m2: e.memset(QA[m2][64:128, :], 0.0), writes=[("QA", m2)])
                            fw.emit("pool", lambda e, m2=m2: e.memset(KA[m2][64:128, :], 0.0), writes=[("KA", m2)])
                    if not is_sb:
                        for m2 in range(2):
                            fw.emit("pool", lambda e, m2=m2, h=h: e.dma_start(out=QA[m2][64:67, :], in_=aug_d[h, 0, :, :]), writes=[("QA", m2)], dma="augq%d" % m2)
                            fw.emit("pool", lambda e, m2=m2, h=h: e.dma_start(out=KA[m2][64:67, :], in_=aug_d[h, 1, :, :]), writes=[("KA", m2)], dma="augk%d" % m2)
                    def sb_inproj_pieces(tb):
                        cols = slice(tb * 512, (tb + 1) * 512)
                        pk = ("OB", 1)
                        pieces = []

                        def mmK(c_lo, c_hi, j):
                            def f():
                                for c in range(c_lo, c_hi):
                                    fw.emit("pe", lambda e, c=c: e.matmul(O2[:, :], lhsT=wsl[:, c, j, :], rhs=hT[:, c, cols], start=(c == 0), stop=(c == NCH - 1)),
                                            reads=[("wsl", j)], writes=[pk])
                            return f

                        def evK():
                            fw.emit("dve", lambda e: e.tensor_copy(out=KA[0][:, cols], in_=O2[:, :]), reads=[pk], writes=[("KA", 0)])

                        def evQ():
                            fw.emit("dve", lambda e: e.tensor_scalar(out=QA[0][0:64, cols], in0=O2[0:64, :], scalar1=float(SCALE), scalar2=None, op0=ALU.mult),
                                    reads=[pk], writes=[("QA", 0)])
                            fw.emit("dve", lambda e: e.tensor_scalar(out=QA[1][64:128, cols], in0=O2[64:128, :], scalar1=float(SCALE), scalar2=None, op0=ALU.mult),
                                    reads=[pk], writes=[("QA", 1)])

                        def mmV(q):
                            def f():
                                tt = tb * 4 + q
                                for c in range(NCH):
                                    fw.emit("pe", lambda e, c=c: e.matmul(O2[:, q * 128:(q + 1) * 128], lhsT=hT[:, c, tt * 128:(tt + 1) * 128],
                                                                         rhs=wsl[:, c, 2, :], start=(c == 0), stop=(c == NCH - 1)),
                                            reads=[("wsl", 2)], writes=[pk])
                            return f

                        def evV():
                            fw.emit("dve", lambda e: e.tensor_copy(out=V[:, tb * 4:(tb + 1) * 4, :].rearrange("p a b -> p (a b)"), in_=O2[:, :]),
                                    reads=[pk], writes=["V"])

                        def seq(*fs):
                            def f():
                                for x in fs:
                                    x()
                            return f
                        pieces.append(mmK(0, 4, 1))
                        pieces.append(seq(mmK(4, 8, 1), evK))
                        pieces.append(mmK(0, 4, 0))
                        pieces.append(seq(mmK(4, 8, 0), evQ))
                        pieces.append(mmV(0)); pieces.append(mmV(1)); pieces.append(mmV(2))
                        pieces.append(seq(mmV(3), evV))
                        return pieces

                    if is_sb:
                        for u in sb_inproj_pieces(0):
                            u()
                    if not is_sb:
                        inb = Zv
                        ib = 0
                        for j in (1, 0):
                            for tb in range(NQB):
                                cols = slice(tb * 512, (tb + 1) * 512)
                                if is_sb:
                                    ps = inb[ib % 6]; pk = ("zb", ib % 6); ib += 1
                                    for c in range(NCH):
                                        fw.emit("pe", lambda e, ps=ps, j=j, c=c, cols=cols: e.matmul(ps[:, :], lhsT=wsl[:, c, j, :], rhs=hT[:, c, cols],
                                                                                                    start=(c == 0), stop=(c == NCH - 1)),
                                                reads=[("wsl", j)], writes=[pk])
                                    if j == 0:
                                        fw.emit("dve", lambda e, ps=ps, cols=cols: e.tensor_scalar(out=QA[0][0:64, cols], in0=ps[0:64, :], scalar1=float(SCALE),
                                                                                                  scalar2=None, op0=ALU.mult),
                                                reads=[pk], writes=[("QA", 0)])
                                        fw.emit("dve", lambda e, ps=ps, cols=cols: e.tensor_scalar(out=QA[1][64:128, cols], in0=ps[64:128, :], scalar1=float(SCALE),
                                                                                                  scalar2=None, op0=ALU.mult),
                                                reads=[pk], writes=[("QA", 1)])
                                    else:
                                        fw.emit("act", lambda e, ps=ps, cols=cols: e.activation(out=KA[0][:, cols], in_=ps[:, :], func=AF.Copy),
                                                reads=[pk], writes=[("KA", 0)])
                                else:
                                    for m2 in range(2):
                                        ps = inb[ib % 6]; pk = ("zb", ib % 6); ib += 1
                                        for c in range(NCH):
                                            fw.emit("pe", lambda e, ps=ps, j=j, c=c, cols=cols, m2=m2: e.matmul(ps[0:64, :], lhsT=wsl[:, c, j, 64 * m2:64 * m2 + 64], rhs=hT[:, c, cols],
                                                                                                              start=(c == 0), stop=(c == NCH - 1)),
                                                    reads=[("wsl", j)], writes=[pk])
                                        if j == 0:
                                            fw.emit("dve", lambda e, ps=ps, cols=cols, m2=m2: e.tensor_scalar(out=QA[m2][0:64, cols], in0=ps[0:64, :], scalar1=float(SCALE),
                                                                                                             scalar2=None, op0=ALU.mult),
                                                    reads=[pk], writes=[("QA", m2)])
                                        else:
                                            fw.emit("act", lambda e, ps=ps, cols=cols, m2=m2: e.activation(out=KA[m2][0:64, cols], in_=ps[0:64, :], func=AF.Copy),
                                                    reads=[pk], writes=[("KA", m2)])
                        for t4 in range(NT // 4):
                            ps = inb[ib % 6]; pk = ("zb", ib % 6); ib += 1
                            for q in range(4):
                                tt = t4 * 4 + q
                                for c in range(NCH):
                                    fw.emit("pe", lambda e, ps=ps, q=q, c=c, tt=tt: e.matmul(ps[:, q * 128:(q + 1) * 128], lhsT=hT[:, c, tt * 128:(tt + 1) * 128],
                                                                                            rhs=wsl[:, c, 2, :], start=(c == 0), stop=(c == NCH - 1)),
                                            reads=[("wsl", 2)], writes=[pk])
                            if t4 % 2 == 0:
                                fw.emit("dve", lambda e, ps=ps, t4=t4: e.tensor_copy(out=V[:, t4 * 4:(t4 + 1) * 4, :].rearrange("p a b -> p (a b)"), in_=ps[:, :]),
                                        reads=[pk], writes=["V"])
                            else:
                                fw.emit("act", lambda e, ps=ps, t4=t4: e.activation(out=V[:, t4 * 4:(t4 + 1) * 4, :].rearrange("p a b -> p (a b)"), in_=ps[:, :], func=AF.Copy),
                                        reads=[pk], writes=["V"])
                    zk = [("zb", 0), ("zb", 1), ("zb", 2), ("zb", 3)]

                    if is_sb:
                        tasks = []
                        for qb in range(NQB):
                            for hh in range(2):
                                nkb = 4 * (qb + 1)
                                kbs = list(reversed(range(nkb)))
                                for n in range(nkb // 2):
                                    kA, kB = kbs[2 * n], kbs[2 * n + 1]
                                    tasks.append(dict(qb=qb, hh=hh, kA=kA, kB=kB, first=(n == 0), last=(n == nkb // 2 - 1),
                                                      lA=kA - 4 * qb, lB=kB - 4 * qb))
                        NTK = len(tasks)

                        def pairv(tile, c0):
                            if c0 == 0:
                                return tile[:, :]
                            return tile[:, :].rearrange("p (h x) -> p h x", h=2)[:, :, c0:512]

                        def sb_s1(i):
                            t = tasks[i]
                            hh = t["hh"]; q0 = 512 * t["qb"]
                            c0 = 128 * max(t["lB"], 0)
                            zz = ZZ[i % 3]
                            zkA, zkB = ("zb", 2 * (i % 3)), ("zb", 2 * (i % 3) + 1)
                            for half, kb, zkk in ((0, t["kA"], zkA), (1, t["kB"], zkB)):
                                fw.emit("pe", lambda e, half=half, kb=kb: e.matmul(zz[:, half * 512 + c0:(half + 1) * 512], lhsT=KA[0][:, kb * 128:(kb + 1) * 128],
                                                                                  rhs=QA[hh][:, q0 + c0:q0 + 512], start=True, stop=True),
                                        reads=[("KA", 0), ("QA", hh)], writes=[zkk])
                            if t["lA"] >= 0:
                                fw.emit("pe", lambda e: e.matmul(zz[:, c0:c0 + 256], lhsT=ident, rhs=cb[:, C_FULL:C_FULL + 256], start=False, stop=True, skip_group_check=True),
                                        reads=["cb"], writes=[zkA])
                            if t["lB"] >= 0:
                                fw.emit("pe", lambda e: e.matmul(zz[:, 512 + c0:512 + c0 + 128], lhsT=ident, rhs=cb[:, C_TRI:C_TRI + 128], start=False, stop=True, skip_group_check=True),
                                        reads=["cb"], writes=[zkB])
                            fw.emit("act", lambda e: e.activation(out=pairv(E2[i % 2], c0), in_=pairv(zz, c0), func=AF.Exp),
                                    reads=[zkA, zkB], writes=[("E", i % 2)])
                            fw.emit("act", lambda e: e.activation(out=pairv(SP2[i % 2], c0), in_=pairv(E2[i % 2], c0), func=AF.Ln, bias=1.0),
                                    reads=[("E", i % 2)], writes=[("SP", i % 2)])

                        def sb_s2(i):
                            t = tasks[i]
                            c0 = 128 * max(t["lB"], 0)
                            zz = ZZ[i % 3]
                            sp = SP2[i % 2]
                            zkA, zkB = ("zb", 2 * (i % 3)), ("zb", 2 * (i % 3) + 1)
                            fw.emit("pe", lambda e: e.matmul(zz[:, c0:512], lhsT=negU, rhs=sp[:, c0:512], start=False, stop=True, skip_group_check=True),
                                    reads=[("SP", i % 2), "cb"], writes=[zkA])
                            if not t["first"]:
                                fw.emit("pe", lambda e: e.matmul(zz[:, c0:512], lhsT=negones, rhs=SC[1][:, c0:512], start=False, stop=True, skip_group_check=True),
                                        reads=[("SC", 1), "cb"], writes=[zkA])
                            if t["first"]:
                                fw.emit("pool", lambda e: e.memset(SC[0][:, 0:256], 0.0), writes=[("SC", 0)])
                                fw.emit("pool", lambda e: e.memset(SC[1][:, 0:256], 0.0), writes=[("SC", 1)])
                                fw.emit("dve", lambda e: e.tensor_copy(out=SC[0][:, c0:512], in_=sp[:, c0:512]),
                                        reads=[("SP", i % 2)], writes=[("SC", 0)])
                            else:
                                fw.emit("dve", lambda e: e.tensor_tensor(out=SC[0][:, c0:512], in0=SC[1][:, c0:512], in1=sp[:, c0:512], op=ALU.add),
                                        reads=[("SP", i % 2), ("SC", 1)], writes=[("SC", 0)])
                            fw.emit("pe", lambda e: e.matmul(zz[:, 512 + c0:1024], lhsT=negU, rhs=sp[:, 512 + c0:1024], start=False, stop=True, skip_group_check=True),
                                    reads=[("SP", i % 2), "cb"], writes=[zkB])
                            fw.emit("pe", lambda e: e.matmul(zz[:, 512 + c0:1024], lhsT=negones, rhs=SC[0][:, c0:512], start=False, stop=True, skip_group_check=True),
                                    reads=[("SC", 0), "cb"], writes=[zkB])
                            if not t["last"]:
                                fw.emit("dve", lambda e: e.tensor_tensor(out=SC[1][:, c0:512], in0=SC[0][:, c0:512], in1=sp[:, 512 + c0:1024], op=ALU.add),
                                        reads=[("SP", i % 2), ("SC", 0)], writes=[("SC", 1)])
                            fw.emit("act", lambda e: e.activation(out=pairv(W2[i % 3], c0), in_=pairv(zz, c0), func=AF.Exp),
                                    reads=[zkA, zkB], writes=[("W", i % 3)])

                        def sb_s3(i):
                            t = tasks[i]
                            hh = t["hh"]; qb = t["qb"]
                            c0 = 128 * max(t["lB"], 0)
                            for half, kb in ((0, t["kA"]), (1, t["kB"])):
                                fw.emit("pe", lambda e, half=half, kb=kb: e.matmul(O1[:, c0:512], lhsT=V[:, kb, :], rhs=W2[i % 3][:, half * 512 + c0:(half + 1) * 512],
                                                                                  start=(t["first"] and half == 0), stop=(t["last"] and half == 1), skip_group_check=True),
                                        reads=[("W", i % 3), "V"], writes=[("OB", 0)])
                            if t["last"]:
                                hb = 64 * hh
                                fw.emit("dve", lambda e: e.tensor_copy(out=oT[hb:hb + 64, g, qb * 512:(qb + 1) * 512], in_=O1[hb:hb + 64, :]),
                                        reads=[("OB", 0)], writes=[("oT", g, qb, hh)])

                        pending = []
                        last_qb = -1
                        for i in range(NTK + 2):
                            if i < NTK:
                                if tasks[i]["qb"] != last_qb:
                                    last_qb = tasks[i]["qb"]
                                    assert not pending
                                    if last_qb + 1 < NQB:
                                        pending = sb_inproj_pieces(last_qb + 1)
                                sb_s1(i)
                            if 0 <= i - 1 < NTK:
                                sb_s2(i - 1)
                            if 0 <= i - 2 < NTK:
                                sb_s3(i - 2)
                            if i < NTK:
                                for _ in range(2 if tasks[i]["qb"] == 0 else 1):
                                    if pending:
                                        pending.pop(0)()
                    else:
                        h = g - 4
                        tasks = []
                        for qb in range(NQB):
                            for c in range(2):
                                nkb = 4 * (qb + 1)
                                for kb in range(nkb):
                                    tasks.append(dict(qb=qb, c=c, kb=kb, first=(kb == 0), last=(kb == nkb - 1), kbl=kb - 4 * qb))
                        NTK = len(tasks)

                        def df_s1(i):
                            t = tasks[i]
                            m2 = t["c"]; q0 = 512 * t["qb"]; kb = t["kb"]
                            c0 = 128 * max(t["kbl"], 0)
                            z = Z[i % 4]
                            fw.emit("pe", lambda e: e.matmul(z[:, c0:512], lhsT=KA[m2][:, kb * 128:(kb + 1) * 128], rhs=QA[m2][:, q0 + c0:q0 + 512],
                                                             start=True, stop=True),
                                    reads=[("KA", m2), ("QA", m2)], writes=[zk[i % 4]])
                            if t["kbl"] >= 0:
                                fw.emit("pe", lambda e: e.matmul(z[:, c0:c0 + 128], lhsT=ident, rhs=cb[:, C_TRI2:C_TRI2 + 128], start=False, stop=True, skip_group_check=True),
                                        reads=["cb"], writes=[zk[i % 4]])
                            cblk = -SLOPES[h] * 128.0 * (4 * t["qb"] - kb)
                            fw.emit("act", lambda e: e.activation(out=W[i % 3][:, c0:512], in_=z[:, c0:512], func=AF.Exp, bias=float(cblk)),
                                    reads=[zk[i % 4]], writes=[("W", i % 3)])

                        def df_s2(i):
                            t = tasks[i]
                            kb = t["kb"]; qb = t["qb"]; m2 = t["c"]
                            c0 = 128 * max(t["kbl"], 0)
                            OO = OB[m2]
                            DD = D1 if m2 == 0 else D2
                            fw.emit("pe", lambda e: e.matmul(OO[:, c0:512], lhsT=V[:, kb, :], rhs=W[i % 3][:, c0:512], start=t["first"], stop=t["last"]),
                                    reads=[("W", i % 3), "V"], writes=[("OB", m2)])
                            fw.emit("pe", lambda e: e.matmul(DD[:, c0:512], lhsT=ones, rhs=W[i % 3][:, c0:512], start=t["first"], stop=t["last"]),
                                    reads=[("W", i % 3), "cb"], writes=[("zb", 4 + m2)])
                            if t["last"] and m2 == 1:
                                fw.emit("act", lambda e: e.activation(out=OS[0][:], in_=O1[:, :], func=AF.Copy), reads=[("OB", 0)], writes=[("OS", 0)])
                                fw.emit("dve", lambda e: e.reciprocal(out=R1, in_=D1), reads=[("zb", 4)], writes=[("E", 0)])
                                fw.emit("act", lambda e: e.activation(out=OS[1][:], in_=O2[:, :], func=AF.Copy), reads=[("OB", 1)], writes=[("OS", 1)])
                                fw.emit("dve", lambda e: e.reciprocal(out=R2, in_=D2), reads=[("zb", 5)], writes=[("E", 1)])
                                fw.emit("pool", lambda e: e.tensor_tensor(out=R1, in0=OS[0][:], in1=R1, op=ALU.mult),
                                        reads=[("OS", 0), ("E", 0)], writes=[("E", 0)])
                                fw.emit("dve", lambda e: e.scalar_tensor_tensor(out=R2, in0=R2, scalar=neglam[:, 0:1], in1=OS[1][:], op0=ALU.mult, op1=ALU.mult),
                                        reads=[("OS", 1), ("E", 1), "neglam"], writes=[("E", 1)])
                                fw.emit("pool", lambda e: e.tensor_tensor(out=oT[:, g, qb * 512:(qb + 1) * 512], in0=R1, in1=R2, op=ALU.add),
                                        reads=[("E", 0), ("E", 1)], writes=[("oT", g, qb, 0)])

                        for i in range(NTK + 2):
                            if i < NTK:
                                df_s1(i)
                            if 0 <= i - 2 < NTK:
                                df_s2(i - 2)
                if dbg:
                    fw.emit("sp", lambda e: e.dma_start(out=dbg_o[:, :, :], in_=oT[:]), reads=[("oT", g, qb, hh) for g in range(8) for qb in range(NQB) for hh in range(2)], dma="dbgo")
                if stop == 1:
                    fw.flush("pa", final_waits=[(fw.sems["dbgo"], 16)])
                    return nc
                fw.flush("pa", final_waits=[(fw.sems["dbgo"], 16)] if dbg else ())

        with ExitStack() as esb:
            X1 = sb("X1", [128, TPC, D], F32, esb)
            wmisc = sb("wmisc", [128, NCH, D], BF16, esb)
            wpp = sb("wpp", [128, 2, D], BF16, esb)
            wg = [sb(f"wg{i}", [128, NCH, DEXP], BF16, esb) for i in range(2)]
            wu = [sb(f"wu{i}", [128, NCH, DEXP], BF16, esb) for i in range(2)]
            wd = [sb(f"wd{i}", [128, 4, D], BF16, esb) for i in range(2)]
            hid = [sb("hid0", [128, 4, 512], BF16, esb)] * 2
            sg = [sb(f"sg{i}", [128, 512], BF16, esb) for i in range(2)]
            xn32 = sb("xn32", [128, D], F32, esb)
            h32 = sb("h32", [128, NCH, 128], F32, esb)
            wr32 = sb("wr32", [128, NCH, 20], F32, esb)
            brb = sb("brb", [128, 20], F32, esb)
            gfb = sb("gfb", [128, D], F32, esb)
            sq2 = sb("sq2", [128, 1024], BF16, esb)
            sq = [sq2[:, 0:512], sq2[:, 512:1024]]
            sqf = sq2[:].bitcast(F32)
            rs_t = sb("rs", [128, 512], F32, esb)
            rs = rs_t[:]
            junkb = sb("junkb", [128, D], BF16, esb)
            st = sb("stat", [128, 3, TPC, 4], F32, esb)
            lg = sb("lg", [128, TPC, 20], F32, esb)
            rt = sb("rt", [128, TPC, 64], F32, esb)
            gates = sb("gates", [128, TPC, NEXP], F32, esb)
            pl = [sb(f"pl{i}", [128, PLE], F32, esb) for i in range(2)]
            plb = sb("plb", [128, PLE], BF16, esb)
            pT = sb("pT", [128, 2, 128], BF16, esb)
            PB = [pst(f"PB{i}", [128, 512], F32, esb) for i in range(8)]

            wout_v = wout_d.rearrange("(c p) n -> p c n", p=128)
            wpg_v = wpg_d.rearrange("(c p) n -> p c n", p=128)
            wpp_v = wpp_d.rearrange("(c p) n -> p c n", p=128)

            fw.emit("sp", lambda e: e.dma_start(out=wr32[:], in_=wr_d.rearrange("(c p) n -> p c n", p=128)), writes=["wr32"], dma="wr32")
            fw.emit("sp", lambda e: e.dma_start(out=brb[:], in_=br_d.partition_broadcast(128)), writes=["brb"], dma="brb")
            fw.emit("sp", lambda e: e.dma_start(out=gfb[:], in_=gfin_d.partition_broadcast(128)), writes=["gfb"], dma="gfb")
            fw.emit("pool", lambda e: e.dma_start(out=wpp[:], in_=wpp_v), writes=["wpp"], dma="wpp")

            def load_expert(e_idx, b):
                for c in range(0, NCH, 4):
                    fw.emit("pool", lambda e, c=c: e.dma_start(out=wg[b][:, c:c + 4, :], in_=weg_d[e_idx].rearrange("(c p) n -> p c n", p=128)[:, c:c + 4, :]),
                            writes=[("wg", b, c)], dma="wg%d_%d" % (b, c))
                    fw.emit("pool", lambda e, c=c: e.dma_start(out=wu[b][:, c:c + 4, :], in_=weu_d[e_idx].rearrange("(c p) n -> p c n", p=128)[:, c:c + 4, :]),
                            writes=[("wu", b, c)], dma="wu%d_%d" % (b, c))
                for c in range(0, 4, 2):
                    fw.emit("pool", lambda e, c=c: e.dma_start(out=wd[b][:, c:c + 2, :], in_=wed_d[e_idx].rearrange("(c p) n -> p c n", p=128)[:, c:c + 2, :]),
                            writes=[("wd", b, c)], dma="wd%d_%d" % (b, c))

            def rstd_batch(kind):
                fw.emit("act", lambda e: e.activation(out=st[:, kind, :, 1], in_=st[:, kind, :, 0], func=AF.Ln, scale=1.0 / D, bias=EPS),
                        reads=[("st", kind, t) for t in range(TPC)], writes=[("stln", kind)])
                fw.emit("act", lambda e: e.activation(out=st[:, kind, :, 2], in_=st[:, kind, :, 1], func=AF.Exp, scale=-0.5),
                        reads=[("stln", kind)], writes=[("strs", kind)])

            def norm_T(kind, t, gk, tok0, want32):
                fw.emit("dve", lambda e: e.tensor_scalar(out=xn32[:], in0=X1[:, t, :], scalar1=st[:, kind, t, 2:3], scalar2=None, op0=ALU.mult),
                        reads=[("X1", t), ("strs", kind)], writes=["xn32"])
                for c in range(NCH):
                    bk = 2 + c // 4
                    fw.emit("pe", lambda e, c=c, bk=bk: e.transpose(out=PB[bk][:, (c % 4) * 128:(c % 4 + 1) * 128], in_=xn32[:, c * 128:(c + 1) * 128], identity=identf[:]),
                            reads=["xn32", "identf"], writes=[("PB", bk)])
                for c in range(NCH):
                    bk = 2 + c // 4
                    src = PB[bk][:, (c % 4) * 128:(c % 4 + 1) * 128]
                    if want32:
                        fw.emit("dve", lambda e, c=c, src=src: e.tensor_scalar(out=h32[:, c, :], in0=src, scalar1=gcols[:, gk, c:c + 1], scalar2=None, op0=ALU.mult),
                                reads=[("PB", bk), ("gcols", gk)], writes=[("h32", c)])
                    else:
                        if True:
                            fw.emit("dve", lambda e, c=c, src=src: e.tensor_scalar(out=oT[:, c, tok0:tok0 + 128], in0=src, scalar1=gcols[:, gk, c:c + 1], scalar2=None, op0=ALU.mult),
                                    reads=[("PB", bk), ("gcols", gk)], writes=[("oTt", tok0)])
                        else:
                            fw.emit("act", lambda e, c=c, src=src: e.activation(out=oT[:, c, tok0:tok0 + 128], in_=src, func=AF.Copy, scale=gcols[:, gk, c:c + 1]),
                                    reads=[("PB", bk), ("gcols", gk)], writes=[("oTt", tok0)])
                if want32:
                    fw.emit("dve", lambda e: e.tensor_copy(out=oT[:, :, tok0:tok0 + 128], in_=h32[:]),
                            reads=[("h32", c) for c in range(NCH)], writes=[("oTt", tok0)])

            def onorm(t0):
                for blk in range(CH // 512):
                    c0 = t0 + blk * 512
                    for grp in range(5):
                        chunks = [0, 1, 2, 3] if grp == 0 else [3 + grp]
                        nfeat = 512.0 if grp == 0 else 128.0
                        for n, c in enumerate(chunks):
                            sqb = sq[n % 2]
                            fw.emit("act", lambda e, c=c, sqb=sqb: e.activation(out=sqb, in_=oT[:, c, c0:c0 + 512], func=AF.Square),
                                    reads=[("oTb", c, c0)], writes=[("sq", n % 2)])
                            fw.emit("pe", lambda e, sqb=sqb, n=n: e.matmul(PB[7][:, :], lhsT=ones, rhs=sqb, start=(n == 0), stop=(n == len(chunks) - 1)),
                                    reads=[("sq", n % 2), "cb"], writes=[("PB", 7)])
                        fw.emit("act", lambda e, nfeat=nfeat: e.activation(out=rs, in_=PB[7][:, :], func=AF.Ln, scale=1.0 / nfeat, bias=EPS),
                                reads=[("PB", 7)], writes=["rs"])
                        fw.emit("act", lambda e: e.activation(out=rs, in_=rs, func=AF.Exp, scale=-0.5), reads=["rs"], writes=["rs"])
                        for c in chunks:
                            gcol = gcols[:, 3, c:c + 1] if grp == 0 else gcols[:, 4, 0:1]
                            fw.emit("dve", lambda e, c=c, gcol=gcol: e.scalar_tensor_tensor(out=oT[:, c, c0:c0 + 512], in0=oT[:, c, c0:c0 + 512], scalar=gcol, in1=rs,
                                                                                           op0=ALU.mult, op1=ALU.mult),
                                    reads=["rs", ("oTb", c, c0), ("gcols", 3), ("gcols", 4)], writes=[("oTb", c, c0)])

            for ck in range(NCK):
                t0 = ck * CH
                fw.emit("pool", lambda e: e.dma_start(out=wmisc[:], in_=wout_v), writes=["wmisc"], dma="wmisc")
                load_expert(0, 0)
                if ck == 0:
                    onorm(t0)
                for t in range(TPC):
                    tok0 = t0 + t * 128
                    c0 = t0 + (t // 4) * 512
                    b = t % 2
                    fw.emit("sp", lambda e, t=t, tok0=tok0: e.dma_start(out=X1[:, t, :], in_=x_d[tok0:tok0 + 128, :]), writes=[("X1", t)], dma="xl%d" % t)
                    for nh in range(2):
                        for c in range(NCH):
                            fw.emit("pe", lambda e, nh=nh, c=c, tok0=tok0: e.matmul(PB[nh][:, :], lhsT=oT[:, c, tok0:tok0 + 128], rhs=wmisc[:, c, nh * 512:(nh + 1) * 512],
                                                                                   start=(c == 0), stop=(c == NCH - 1)),
                                    reads=[("oTb", c, c0), "wmisc", ("oTt", tok0)], writes=[("PB", nh)])
                        fw.emit("dve", lambda e, nh=nh, t=t, b=b: e.tensor_tensor(out=X1[:, t, nh * 512:(nh + 1) * 512], in0=PB[nh][:, :], in1=X1[:, t, nh * 512:(nh + 1) * 512], op=ALU.add),
                                reads=[("PB", nh), ("X1", t)], writes=[("X1", t)])
                    fw.emit("act", lambda e, t=t: e.activation(out=junkb[:], in_=X1[:, t, :], func=AF.Square, accum_out=st[:, 0, t, 0:1]),
                            reads=[("X1", t)], writes=["junkb", ("st", 0, t)])
                    if dbg:
                        fw.emit("sp", lambda e, t=t, tok0=tok0: e.dma_start(out=dbg_x1[tok0:tok0 + 128, :], in_=X1[:, t, :]), reads=[("X1", t)], dma="dbgx%d" % t)
                rstd_batch(0)
                for t in range(TPC):
                    tok0 = t0 + t * 128
                    norm_T(0, t, 1, tok0, True)
                    for c in range(NCH):
                        fw.emit("pe", lambda e, c=c: e.matmul(PB[7][:, 0:20], lhsT=h32[:, c, :], rhs=wr32[:, c, :], start=(c == 0), stop=(c == NCH - 1)),
                                reads=[("h32", cc) for cc in range(NCH)] + ["wr32"], writes=[("PB", 7)])
                    fw.emit("dve", lambda e, t=t: e.tensor_tensor(out=lg[:, t, :], in0=PB[7][:, 0:20], in1=brb[:], op=ALU.add),
                            reads=[("PB", 7), "brb"], writes=["lg"])
                G = lg[:, :, 0:4]
                EL = lg[:, :, 4:20]
                gmax = rt[:, :, 0:1]
                goh = rt[:, :, 1:5]
                gex = rt[:, :, 5:9]
                gsum = rt[:, :, 9:10]
                gw = rt[:, :, 10:11]
                msk = rt[:, :, 16:32]
                m1 = rt[:, :, 11:12]
                m2 = rt[:, :, 12:13]
                oh1 = rt[:, :, 32:48]
                oh2 = rt[:, :, 48:64]
                dlt = rt[:, :, 13:14]
                w1 = rt[:, :, 14:15]
                w2 = rt[:, :, 15:16]
                BIG = 1.0e4

                def dv(fn, r=("lg", "rt"), w=("rt",)):
                    fw.emit("dve", fn, reads=list(r), writes=list(w))

                dv(lambda e: e.tensor_reduce(out=gmax, in_=G, axis=AX.X, op=ALU.max))
                dv(lambda e: e.tensor_tensor(out=goh, in0=G, in1=gmax.to_broadcast([128, TPC, 4]), op=ALU.is_equal))
                dv(lambda e: e.tensor_tensor(out=gex, in0=G, in1=gmax.to_broadcast([128, TPC, 4]), op=ALU.subtract))
                fw.emit("act", lambda e: e.activation(out=gex, in_=gex, func=AF.Exp), reads=["rt"], writes=["rt"])
                dv(lambda e: e.tensor_reduce(out=gsum, in_=gex, axis=AX.X, op=ALU.add))
                dv(lambda e: e.reciprocal(out=gw, in_=gsum))
                dv(lambda e: e.tensor_scalar(out=gex, in0=goh, scalar1=-1.0, scalar2=BIG, op0=ALU.add, op1=ALU.mult))
                dv(lambda e: e.tensor_tensor(out=msk.rearrange("p t (g k) -> p t g k", k=4), in0=EL.rearrange("p t (g k) -> p t g k", k=4),
                                             in1=gex.unsqueeze(3).to_broadcast([128, TPC, 4, 4]), op=ALU.add))
                dv(lambda e: e.tensor_reduce(out=m1, in_=msk, axis=AX.X, op=ALU.max))
                dv(lambda e: e.tensor_tensor(out=oh1, in0=msk, in1=m1.to_broadcast([128, TPC, 16]), op=ALU.is_equal))
                dv(lambda e: e.scalar_tensor_tensor(out=msk, in0=oh1, scalar=-BIG, in1=msk, op0=ALU.mult, op1=ALU.add))
                dv(lambda e: e.tensor_reduce(out=m2, in_=msk, axis=AX.X, op=ALU.max))
                dv(lambda e: e.tensor_tensor(out=oh2, in0=msk, in1=m2.to_broadcast([128, TPC, 16]), op=ALU.is_equal))
                dv(lambda e: e.tensor_tensor(out=dlt, in0=m2, in1=m1, op=ALU.subtract))
                fw.emit("act", lambda e: e.activation(out=dlt, in_=dlt, func=AF.Exp), reads=["rt"], writes=["rt"])
                dv(lambda e: e.tensor_scalar(out=w1, in0=dlt, scalar1=1.0, scalar2=None, op0=ALU.add))
                dv(lambda e: e.reciprocal(out=w1, in_=w1))
                dv(lambda e: e.tensor_tensor(out=w2, in0=dlt, in1=w1, op=ALU.mult))
                dv(lambda e: e.tensor_tensor(out=w1, in0=w1, in1=gw, op=ALU.mult))
                dv(lambda e: e.tensor_tensor(out=w2, in0=w2, in1=gw, op=ALU.mult))
                dv(lambda e: e.tensor_tensor(out=oh1, in0=oh1, in1=w1.to_broadcast([128, TPC, 16]), op=ALU.mult))
                dv(lambda e: e.tensor_tensor(out=oh2, in0=oh2, in1=w2.to_broadcast([128, TPC, 16]), op=ALU.mult))
                dv(lambda e: e.tensor_tensor(out=gates[:], in0=oh1, in1=oh2, op=ALU.add), w=("gates",))

                for ex in range(NEXP):
                    b = ex % 2
                    if ex + 1 < NEXP:
                        load_expert(ex + 1, (ex + 1) % 2)
                    if ex == 4 and ck + 1 < NCK:
                        onorm(t0 + CH)
                    for tb in range(CH // 512):
                        hb_ = hid[tb % 2]
                        c0 = t0 + tb * 512
                        for jc in range(4):
                            gp = PB[jc % 2]; up = PB[2 + jc % 2]
                            for c in range(NCH):
                                fw.emit("pe", lambda e, gp=gp, c=c, jc=jc: e.matmul(gp[:, :], lhsT=wg[b][:, c, jc * 128:(jc + 1) * 128], rhs=oT[:, c, c0:c0 + 512],
                                                                                   start=(c == 0), stop=(c == NCH - 1)),
                                        reads=[("wg", b, 4 * (c // 4))] + [("oTt", c0 + 128 * q) for q in range(4)], writes=[("PB", jc % 2)])
                            for c in range(NCH):
                                fw.emit("pe", lambda e, up=up, c=c, jc=jc: e.matmul(up[:, :], lhsT=wu[b][:, c, jc * 128:(jc + 1) * 128], rhs=oT[:, c, c0:c0 + 512],
                                                                                   start=(c == 0), stop=(c == NCH - 1)),
                                        reads=[("wu", b, 4 * (c // 4))] + [("oTt", c0 + 128 * q) for q in range(4)], writes=[("PB", 2 + jc % 2)])
                            fw.emit("act", lambda e, gp=gp, jc=jc: e.activation(out=sg[jc % 2][:], in_=gp[:, :], func=AF.Silu),
                                    reads=[("PB", jc % 2)], writes=[("sg", jc % 2)])
                            fw.emit("dve", lambda e, up=up, jc=jc, hb_=hb_: e.tensor_tensor(out=hb_[:, jc, :], in0=up[:, :], in1=sg[jc % 2][:], op=ALU.mult),
                                    reads=[("PB", 2 + jc % 2), ("sg", jc % 2)], writes=[("hid", tb % 2, jc)])
                        for q in range(4):
                            t = tb * 4 + q
                            for nh in range(2):
                                yp = PB[4 + (2 * q + nh) % 3]
                                yk = ("PB", 4 + (2 * q + nh) % 3)
                                for jc in range(4):
                                    fw.emit("pe", lambda e, yp=yp, jc=jc, q=q, nh=nh, hb_=hb_: e.matmul(yp[:, :], lhsT=hb_[:, jc, q * 128:(q + 1) * 128],
                                                                                                       rhs=wd[b][:, jc, nh * 512:(nh + 1) * 512], start=(jc == 0), stop=(jc == 3)),
                                            reads=[("hid", tb % 2, jc), ("wd", b, 2 * (jc // 2))], writes=[yk])
                                fw.emit("dve", lambda e, yp=yp, t=t, nh=nh: e.scalar_tensor_tensor(out=X1[:, t, nh * 512:(nh + 1) * 512], in0=yp[:, :], scalar=gates[:, t, ex:ex + 1],
                                                                                                  in1=X1[:, t, nh * 512:(nh + 1) * 512], op0=ALU.mult, op1=ALU.add),
                                        reads=[yk, "gates", ("X1", t)], writes=[("X1", t)])
                fw.emit("pool", lambda e: e.dma_start(out=wmisc[:], in_=wpg_v), writes=["wmisc"], dma="wmisc")
                for t in range(TPC):
                    fw.emit("act", lambda e, t=t: e.activation(out=junkb[:], in_=X1[:, t, :], func=AF.Square, accum_out=st[:, 1, t, 0:1]),
                            reads=[("X1", t)], writes=["junkb", ("st", 1, t)])
                rstd_batch(1)
                for t in range(TPC):
                    tok0 = t0 + t * 128
                    b = t % 2
                    norm_T(1, t, 2, tok0, False)
                    fw.emit("sp", lambda e, b=b, tok0=tok0: e.dma_start(out=pl[b][:], in_=p_d[tok0:tok0 + 128, :]), writes=[("pl", b)], dma="pl%d" % b)
                    fw.emit("pool", lambda e, b=b: e.tensor_copy(out=plb[:], in_=pl[b][:]), reads=[("pl", b)], writes=["plb"])
                    tpv = PB[6][:, 0:128].bitcast(BF16)
                    for c2 in range(2):
                        fw.emit("pe", lambda e, c2=c2, tpv=tpv: e.transpose(out=tpv[:, c2 * 128:(c2 + 1) * 128], in_=plb[:, c2 * 128:(c2 + 1) * 128], identity=ident),
                                reads=["plb", "cb"], writes=[("PB", 6)])
                    fw.emit("dve", lambda e, tpv=tpv: e.tensor_copy(out=pT[:].rearrange("p a b -> p (a b)"), in_=tpv), reads=[("PB", 6)], writes=["pT"])
                    for nh in range(2):
                        for c in range(NCH):
                            fw.emit("pe", lambda e, nh=nh, c=c, tok0=tok0: e.matmul(PB[nh][:, :], lhsT=oT[:, c, tok0:tok0 + 128], rhs=wmisc[:, c, nh * 512:(nh + 1) * 512],
                                                                                   start=(c == 0), stop=(c == NCH - 1)),
                                    reads=[("oTt", tok0), "wmisc"], writes=[("PB", nh)])
                        for c2 in range(2):
                            fw.emit("pe", lambda e, nh=nh, c2=c2: e.matmul(PB[4 + nh][:, :], lhsT=pT[:, c2, :], rhs=wpp[:, c2, nh * 512:(nh + 1) * 512],
                                                                          start=(c2 == 0), stop=(c2 == 1)),
                                    reads=["pT", "wpp"], writes=[("PB", 4 + nh)])
                        sgv = h32[:].rearrange("p a b -> p (a b)")[:, nh * 512:(nh + 1) * 512]
                        sgk = [("h32", 4 * nh + cc) for cc in range(4)]
                        fw.emit("act", lambda e, nh=nh, sgv=sgv: e.activation(out=sgv, in_=PB[nh][:, :], func=AF.Sigmoid),
                                reads=[("PB", nh)], writes=sgk)
                        tmpb = rs if nh == 0 else sqf
                        tmpk = ["rs"] if nh == 0 else [("sq", 0), ("sq", 1)]
                        fw.emit("dve", lambda e, nh=nh, tmpb=tmpb, sgv=sgv: e.tensor_tensor(out=tmpb, in0=PB[4 + nh][:, :], in1=sgv, op=ALU.mult),
                                reads=[("PB", 4 + nh)] + sgk, writes=tmpk)
                        fw.emit("pool", lambda e, nh=nh, t=t, tmpb=tmpb: e.tensor_tensor(out=X1[:, t, nh * 512:(nh + 1) * 512], in0=X1[:, t, nh * 512:(nh + 1) * 512],
                                                                                        in1=tmpb, op=ALU.add),
                                reads=tmpk + [("X1", t)], writes=[("X1", t)])
                    fw.emit("act", lambda e, t=t: e.activation(out=junkb[:], in_=X1[:, t, :], func=AF.Square, accum_out=st[:, 2, t, 0:1]),
                            reads=[("X1", t)], writes=["junkb", ("st", 2, t)])
                rstd_batch(2)
                for t in range(TPC):
                    tok0 = t0 + t * 128
                    b = t % 2
                    fw.emit("dve", lambda e, t=t, b=b: e.scalar_tensor_tensor(out=X1[:, t, :], in0=X1[:, t, :], scalar=st[:, 2, t, 2:3], in1=gfb[:], op0=ALU.mult, op1=ALU.mult),
                            reads=[("X1", t), ("strs", 2), "gfb"], writes=[("X1", t)])
                    fw.emit("sp", lambda e, t=t, tok0=tok0: e.dma_start(out=out_d[tok0:tok0 + 128, :], in_=X1[:, t, :]), reads=[("X1", t)], dma="out%d" % t)
            finals = [(fw.sems[n], fw.dma_cnt[n]) for n in fw.sems if n.startswith("out") or n.startswith("dbg")]
            fw.flush("pb", final_waits=finals)
    return nc


_NC_CACHE = {}


def _prep_inputs(inputs, S):
    f = lambda a: np.ascontiguousarray(np.asarray(a, dtype=np.float32))
    shared = {
        "cst": make_consts(),
        "aug": make_aug(S),
        "g_mix": f(inputs["g_mix"][0]),
        "w_in": f(inputs["w_in"][0]),
        "lam4": f(np.stack([inputs["lambda_q1"][0], inputs["lambda_k1"][0], inputs["lambda_q2"][0], inputs["lambda_k2"][0]], 0)),
        "g_sb_out": f(inputs["g_sb_out"][0]),
        "g_df_out": f(inputs["g_df_out"][0]),
        "w_out": f(inputs["w_out"][0]),
        "g_ffn": f(inputs["g_ffn"][0]),
        "w_router": f(np.concatenate([inputs["w_router_group"][0], inputs["w_router_expert"][0]], axis=1)),
        "b_router": f(np.concatenate([inputs["b_router_group"][0], inputs["b_router_expert"][0]], axis=0)),
        "w_expert_gate": f(inputs["w_expert_gate"][0]),
        "w_expert_up": f(inputs["w_expert_up"][0]),
        "w_expert_down": f(inputs["w_expert_down"][0]),
        "g_ple": f(inputs["g_ple"][0]),
        "w_ple_gate": f(inputs["w_ple_gate"][0]),
        "w_ple_proj": f(inputs["w_ple_proj"][0]),
        "g_final": f(inputs["g_final"]),
    }
    return shared


def kernel(**inputs):
    x = np.asarray(inputs["x"], dtype=np.float32)
    p = np.asarray(inputs["p"], dtype=np.float32)
    B, S, _ = x.shape
    if S not in _NC_CACHE:
        _NC_CACHE[S] = build(S)
    nc = _NC_CACHE[S]
    shared = _prep_inputs(inputs, S)
    in_maps = []
    for b in range(B):
        m = dict(shared)
        m["x"] = np.ascontiguousarray(x[b])
        m["p"] = np.ascontiguousarray(p[0, b])
        in_maps.append(m)
    res = run_bass_kernel_spmd(nc, in_maps, core_ids=list(range(B)))
    return np.stack([np.asarray(r["out"], dtype=np.float32) for r in res.results], axis=0)
```

```python
import math
from contextlib import ExitStack

import numpy as np
import concourse.bass as bass
import concourse.mybir as mybir
from concourse.bass_utils import run_bass_kernel_spmd

F32 = mybir.dt.float32
BF16 = mybir.dt.bfloat16
AF = mybir.ActivationFunctionType
ALU = mybir.AluOpType
AX = mybir.AxisListType

D = 1024
NCH = 8
HD = 64
NEXP = 16
DEXP = 512
PLE = 256
EPS = 1e-6
SCALE = HD ** -0.5
SLOPES = [2.0 ** (-8.0 * (h + 1) / 4) for h in range(4)]
LAMBDA_INIT = 0.8 - 0.6 * math.exp(-0.3 * 0)
SAME_ENGINE_SYNC = True

C_ID, C_NEGU, C_ONES, C_NEGONES = 0, 128, 256, 384
C_KAUG = 512
C_QAUG = 1024
C_FULL = 3072
C_TRI = 3200
C_TRI2 = 3328
CW = 3456
MASK_BIG = 30000.0


def make_consts():
    c = np.zeros((128, CW), np.float32)
    j = np.arange(128)[:, None]
    s = np.arange(128)[None, :]
    c[:, C_ID:C_ID + 128] = (j == s)
    c[:, C_NEGU:C_NEGU + 128] = -(j >= s).astype(np.float32)
    c[:, C_ONES:C_ONES + 128] = 1.0
    c[:, C_NEGONES:C_NEGONES + 128] = -1.0
    c[:, C_FULL:C_FULL + 128] = -MASK_BIG
    c[:, C_TRI:C_TRI + 128] = np.where(s <= j, -MASK_BIG, 0.0)
    c[:, C_TRI2:C_TRI2 + 128] = np.where(s < j, -MASK_BIG, 0.0)
    tl = np.arange(512)
    for h in range(4):
        sl = SLOPES[h]
        k = c[:, C_KAUG + 128 * h:C_KAUG + 128 * (h + 1)]
        k[0, :] = sl * np.arange(128)
        k[1, :] = 1.0
        k[2, :] = 1.0
        q = c[:, C_QAUG + 512 * h:C_QAUG + 512 * (h + 1)]
        q[0, :] = 1.0
        q[1, :] = -sl * (tl % 128)
        q[2, :] = -sl * 128.0 * (tl // 128)
    return c


def make_aug(S):
    a = np.zeros((4, 2, 3, S), np.float32)
    t = np.arange(S)
    for h in range(4):
        sl = SLOPES[h]
        a[h, 0, 0] = 1.0
        a[h, 0, 1] = -sl * (t % 128)
        a[h, 0, 2] = -sl * 128.0 * ((t // 128) % 4)
        a[h, 1, 0] = sl * (t % 128)
        a[h, 1, 1] = 1.0
        a[h, 1, 2] = 1.0
    return a


class _Rec:
    def __getattr__(self, name):
        def f(*a, **k):
            return (name, a, k)
        return f


class FW:
    def __init__(self, nc, es):
        self.nc = nc
        self.es = es
        self.engs = {"pe": nc.tensor, "act": nc.scalar, "dve": nc.vector, "pool": nc.gpsimd, "sp": nc.sync}
        self.prog = {e: es.enter_context(nc.semaphore("prog_" + e)) for e in self.engs}
        self.cnt = {e: 0 for e in self.engs}
        self.waited = {e: {} for e in self.engs}
        self.dma_cnt = {}
        self.sems = {}
        self.reset()

    def reset(self):
        self.ops = {e: [] for e in self.engs}
        self.lastw = {}
        self.readers = {}

    def dsem(self, name):
        if name not in self.sems:
            self.sems[name] = self.es.enter_context(self.nc.semaphore("d_" + name))
            self.dma_cnt[name] = 0
        return name

    def emit(self, eng, fn, reads=(), writes=(), dma=None):
        rec = fn(_Rec())
        deps = []
        for r in reads:
            t = self.lastw.get(r)
            if t is not None:
                deps.append(t)
        for w in writes:
            t = self.lastw.get(w)
            if t is not None:
                deps.append(t)
            deps.extend(self.readers.get(w, {}).values())
        waits = {}
        for (skey, sem, val, teng) in deps:
            if teng == eng and (eng == "pe" or not SAME_ENGINE_SYNC):
                continue
            if self.waited[eng].get(skey, -1) >= val:
                continue
            if waits.get(skey, (None, -1))[1] < val:
                waits[skey] = (sem, val)
        for skey, (sem, val) in waits.items():
            self.waited[eng][skey] = val
        if dma is not None:
            self.dsem(dma)
            self.dma_cnt[dma] += 16
            tok = ("d_" + dma, self.sems[dma], self.dma_cnt[dma], None)
            inc = (self.sems[dma], 16)
        else:
            self.cnt[eng] += 1
            tok = ("p_" + eng, self.prog[eng], self.cnt[eng], eng)
            inc = (self.prog[eng], 1)
        for w in writes:
            self.lastw[w] = tok
            self.readers[w] = {}
        for r in reads:
            self.readers.setdefault(r, {})[tok[0]] = tok
        self.ops[eng].append((list(waits.values()), rec, inc))
        return tok

    def flush(self, name, final_waits=()):
        nc = self.nc
        ops = self.ops
        with nc.Block() as block:
            def mk(ename):
                def body(e):
                    for waits, rec, inc in ops[ename]:
                        for sem, val in waits:
                            e.wait_ge(sem, val)
                        inst = getattr(e, rec[0])(*rec[1], **rec[2])
                        inst.then_inc(inc[0], inc[1])
                    if ename == "sp":
                        for sem, val in final_waits:
                            e.wait_ge(sem, val)
                return body
            block.sync(mk("sp"))
            block.tensor(mk("pe"))
            block.scalar(mk("act"))
            block.vector(mk("dve"))
            block.gpsimd(mk("pool"))
        self.reset()


def build(S, dbg=False, stop=9):
    assert S % 1024 == 0
    NT = S // 128
    NQB = S // 512
    CH = 1024
    NCK = S // CH
    TPC = CH // 128

    nc = bass.Bass("TRN2", target_bir_lowering=False)

    def din(name, shape):
        return nc.dram_tensor(name, list(shape), F32, kind="ExternalInput").ap()

    x_d = din("x", [S, D])
    p_d = din("p", [S, PLE])
    cst_d = din("cst", [128, CW])
    aug_d = din("aug", [4, 2, 3, S])
    gmix_d = din("g_mix", [D])
    win_d = din("w_in", [D, 3072])
    lam_d = din("lam4", [4, HD])
    gsb_d = din("g_sb_out", [512])
    gdf_d = din("g_df_out", [128])
    wout_d = din("w_out", [D, D])
    gffn_d = din("g_ffn", [D])
    wr_d = din("w_router", [D, 20])
    br_d = din("b_router", [20])
    weg_d = din("w_expert_gate", [NEXP, D, DEXP])
    weu_d = din("w_expert_up", [NEXP, D, DEXP])
    wed_d = din("w_expert_down", [NEXP, DEXP, D])
    gple_d = din("g_ple", [D])
    wpg_d = din("w_ple_gate", [D, D])
    wpp_d = din("w_ple_proj", [PLE, D])
    gfin_d = din("g_final", [D])
    out_d = nc.dram_tensor("out", [S, D], F32, kind="ExternalOutput").ap()
    if dbg:
        dbg_o = nc.dram_tensor("dbg_o", [128, NCH, S], BF16, kind="ExternalOutput").ap()
        dbg_x1 = nc.dram_tensor("dbg_x1", [S, D], F32, kind="ExternalOutput").ap()

    with ExitStack() as es:
        fw = FW(nc, es)

        def sb(name, shape, dt, stack=es):
            return stack.enter_context(nc.sbuf_tensor(name, list(shape), dt))

        def pst(name, shape, dt, stack):
            return stack.enter_context(nc.psum_tensor(name, list(shape), dt))

        oT = sb("oT", [128, NCH, S], BF16)
        cb = sb("cb", [128, CW], BF16)
        identf = sb("identf", [128, 128], F32)
        gcols = sb("gcols", [128, 5, NCH], F32)
        lamt = sb("lamt", [128, 4, HD], F32)
        lamw = sb("lamw", [128, 8], F32)
        neglam = sb("neglam", [128, 1], F32)

        ident = cb[:, C_ID:C_ID + 128]
        negU = cb[:, C_NEGU:C_NEGU + 128]
        ones = cb[:, C_ONES:C_ONES + 128]
        negones = cb[:, C_NEGONES:C_NEGONES + 128]

        with ExitStack() as esA:
            hT = sb("hT", [128, NCH, S], BF16, esA)
            with ExitStack() as es0:
                xt = [sb(f"xt{i}", [128, D], F32, es0) for i in range(2)]
                xn = [sb(f"xn{i}", [128, D], BF16, es0) for i in range(2)]
                junk = sb("junk0", [128, D], BF16, es0)
                ssq = sb("ssq0", [128, NT], F32, es0)
                lnv = sb("lnv0", [128, NT], F32, es0)
                rstd = sb("rstd0", [128, NT], F32, es0)
                tp = [pst(f"tp{i}", [128, D], BF16, es0) for i in range(2)]

                fw.emit("pool", lambda e: e.dma_start(out=cb[:], in_=cst_d[:, :]), writes=["cb"], dma="cst")
                fw.emit("sp", lambda e: e.dma_start(out=identf[:], in_=cst_d[:, C_ID:C_ID + 128]), writes=["identf"], dma="cst2")
                gst = sb("gst", [8, 5, 128], F32, es0)
                gps = pst("gps", [128, 5, 8], F32, es0)
                for k, (gd, nr) in enumerate([(gmix_d, 8), (gffn_d, 8), (gple_d, 8), (gsb_d, 4), (gdf_d, 1)]):
                    fw.emit("sp", lambda e, k=k, gd=gd, nr=nr: e.dma_start(out=gst[0:nr, k, :], in_=gd.rearrange("(c p) -> c p", p=128)),
                            writes=[("gst", k)], dma="gc%d" % k)
                    fw.emit("pe", lambda e, k=k, nr=nr: e.transpose(out=gps[:, k, 0:nr], in_=gst[0:nr, k, :], identity=identf[0:nr, 0:nr]),
                            reads=[("gst", k), "identf"], writes=["gps"])
                    fw.emit("dve", lambda e, k=k, nr=nr: e.tensor_copy(out=gcols[:, k, 0:nr], in_=gps[:, k, 0:nr]),
                            reads=["gps"], writes=[("gcols", k)])
                fw.emit("dve", lambda e: e.tensor_scalar(out=gcols[:, 4, 0:1], in0=gcols[:, 4, 0:1], scalar1=float(1.0 - LAMBDA_INIT),
                                                          scalar2=None, op0=ALU.mult),
                        reads=[("gcols", 4)], writes=[("gcols", 4)])
                if True:
                    fw.emit("sp", lambda e: e.dma_start(out=lamt[:].rearrange("p a b -> p (a b)"),
                                                         in_=lam_d.rearrange("a b -> (a b)").partition_broadcast(128)),
                            writes=["lamt"], dma="lam")
                    fw.emit("dve", lambda e: e.tensor_tensor(out=lamt[:, 0, :], in0=lamt[:, 0, :], in1=lamt[:, 1, :], op=ALU.mult),
                            reads=["lamt"], writes=["lamt"])
                    fw.emit("dve", lambda e: e.tensor_tensor(out=lamt[:, 2, :], in0=lamt[:, 2, :], in1=lamt[:, 3, :], op=ALU.mult),
                            reads=["lamt"], writes=["lamt"])
                    fw.emit("dve", lambda e: e.reduce_sum(out=lamw[:, 0:1], in_=lamt[:, 0, :], axis=AX.X), reads=["lamt"], writes=["lamw"])
                    fw.emit("dve", lambda e: e.reduce_sum(out=lamw[:, 1:2], in_=lamt[:, 2, :], axis=AX.X), reads=["lamt"], writes=["lamw"])
                    fw.emit("act", lambda e: e.activation(out=lamw[:, 2:4], in_=lamw[:, 0:2], func=AF.Exp), reads=["lamw"], writes=["lamw"])
                    fw.emit("dve", lambda e: e.tensor_tensor(out=lamw[:, 4:5], in0=lamw[:, 3:4], in1=lamw[:, 2:3], op=ALU.subtract),
                            reads=["lamw"], writes=["lamw"])
                    fw.emit("dve", lambda e: e.tensor_scalar(out=neglam[:], in0=lamw[:, 4:5], scalar1=float(-LAMBDA_INIT), scalar2=None, op0=ALU.add),
                            reads=["lamw"], writes=["neglam"])

                for i in range(NT):
                    b = i % 2
                    fw.emit("sp", lambda e, i=i, b=b: e.dma_start(out=xt[b][:], in_=x_d[i * 128:(i + 1) * 128, :]),
                            writes=[("xt", b)], dma="xt%d" % b)
                    fw.emit("act", lambda e, i=i, b=b: e.activation(out=junk[:], in_=xt[b][:], func=AF.Square, accum_out=ssq[:, i:i + 1]),
                            reads=[("xt", b)], writes=["junk", ("ssq", i)])
                    fw.emit("act", lambda e, i=i: e.activation(out=lnv[:, i:i + 1], in_=ssq[:, i:i + 1], func=AF.Ln, scale=1.0 / D, bias=EPS),
                            reads=[("ssq", i)], writes=[("lnv", i)])
                    fw.emit("act", lambda e, i=i: e.activation(out=rstd[:, i:i + 1], in_=lnv[:, i:i + 1], func=AF.Exp, scale=-0.5),
                            reads=[("lnv", i)], writes=[("rstd", i)])
                    fw.emit("dve", lambda e, i=i, b=b: e.tensor_scalar(out=xn[b][:], in0=xt[b][:], scalar1=rstd[:, i:i + 1], scalar2=None, op0=ALU.mult),
                            reads=[("xt", b), ("rstd", i)], writes=[("xn", b)])
                    for c in range(NCH):
                        fw.emit("pe", lambda e, b=b, c=c: e.transpose(out=tp[b][:, c * 128:(c + 1) * 128], in_=xn[b][:, c * 128:(c + 1) * 128], identity=ident),
                                reads=[("xn", b), "cb"], writes=[("tp", b)])
                    for c in range(NCH):
                        if True:
                            fw.emit("dve", lambda e, i=i, b=b, c=c: e.tensor_scalar(out=hT[:, c, i * 128:(i + 1) * 128], in0=tp[b][:, c * 128:(c + 1) * 128],
                                                                                   scalar1=gcols[:, 0, c:c + 1], scalar2=None, op0=ALU.mult),
                                    reads=[("tp", b), ("gcols", 0)], writes=[("hT", i, c)])
                        else:
                            fw.emit("act", lambda e, i=i, b=b, c=c: e.activation(out=hT[:, c, i * 128:(i + 1) * 128], in_=tp[b][:, c * 128:(c + 1) * 128],
                                                                                func=AF.Copy, scale=gcols[:, 0, c:c + 1]),
                                    reads=[("tp", b), ("gcols", 0)], writes=[("hT", i, c)])
                if stop == 0:
                    fw.emit("sp", lambda e: e.dma_start(out=dbg_o[:, :, :], in_=hT[:]), reads=[("hT", i, c) for i in range(NT) for c in range(NCH)], dma="dbgo")
                    fw.flush("p0", final_waits=[(fw.sems["dbgo"], 16)])
                    return nc
                fw.flush("p0")

            with ExitStack() as esa:
                QA = [sb(f"QA{i}", [128, S], BF16, esa) for i in range(2)]
                KA = [sb(f"KA{i}", [128, S], BF16, esa) for i in range(2)]
                V = sb("V", [128, NT, 128], BF16, esa)
                wsl = sb("wsl", [128, NCH, 3, 128], BF16, esa)
                E2 = [sb(f"E{i}", [128, 1024], BF16, esa) for i in range(2)]
                SP2 = [sb(f"SP{i}", [128, 1024], BF16, esa) for i in range(2)]
                SC = [sb(f"SC{i}", [128, 512], BF16, esa) for i in range(2)]
                W2 = [sb(f"W{i}", [128, 1024], BF16, esa) for i in range(3)]
                W = [w[:, 0:512] for w in W2]
                OS = [sb(f"OS{i}", [128, 512], F32, esa) for i in range(2)]
                R1 = E2[0][:].bitcast(F32)
                R2 = E2[1][:].bitcast(F32)
                ZZ = [pst(f"ZZ{i}", [128, 1024], F32, esa) for i in range(3)]
                Zv = [ZZ[j // 2][:, (j % 2) * 512:(j % 2 + 1) * 512] for j in range(6)]
                Z = Zv[0:4]
                D1 = Zv[4]
                D2 = Zv[5]
                O1 = pst("O1", [128, 512], F32, esa)
                O2 = pst("O2", [128, 512], F32, esa)
                OB = [O1, O2]
                win_v = win_d.rearrange("(c p) n -> p c n", p=128)

                fw.emit("pool", lambda e: e.memset(QA[0][64:128, :], 0.0), writes=[("QA", 0)])
                fw.emit("pool", lambda e: e.memset(QA[1][0:64, :], 0.0), writes=[("QA", 1)])

                for g in range(8):
                    is_sb = g < 4
                    if is_sb:
                        offs = (128 * g, 512 + 128 * g, 1024 + 128 * g)
                    else:
                        h = g - 4
                        offs = (1536 + 128 * h, 2048 + 128 * h, 2560 + 128 * h)
                    def load_wsl(gg):
                        o3 = (128 * gg, 512 + 128 * gg, 1024 + 128 * gg) if gg < 4 else (1536 + 128 * (gg - 4), 2048 + 128 * (gg - 4), 2560 + 128 * (gg - 4))
                        for j in range(3):
                            fw.emit("pool", lambda e, j=j, o=o3[j]: e.dma_start(out=wsl[:, :, j, :], in_=win_v[:, :, o:o + 128]),
                                    writes=[("wsl", j)], dma="wsl%d" % j)
                    if g == 0:
                        load_wsl(0)
                    if g == 4:
                        for m2 in range(2):
                            fw.emit("pool", lambda e, m2=m2: e.memset(QA[m2][64:128, :], 0.0), writes=[("QA", m2)])
                            fw.emit("pool", lambda e, m2=m2: e.memset(KA[m2][64:128, :], 0.0), writes=[("KA", m2)])
                    if not is_sb:
                        for m2 in range(2):
                            fw.emit("pool", lambda e, m2=m2, h=h: e.dma_start(out=QA[m2][64:67, :], in_=aug_d[h, 0, :, :]), writes=[("QA", m2)], dma="augq%d" % m2)
                            fw.emit("pool", lambda e, m2=m2, h=h: e.dma_start(out=KA[m2][64:67, :], in_=aug_d[h, 1, :, :]), writes=[("KA", m2)], dma="augk%d" % m2)
                    def sb_inproj_pieces(tb):
                        cols = slice(tb * 512, (tb + 1) * 512)
                        pk = ("OB", 1)
                        pieces = []

                        def mmK(c_lo, c_hi, j):
                            def f():
                                for c in range(c_lo, c_hi):
                                    fw.emit("pe", lambda e, c=c: e.matmul(O2[:, :], lhsT=wsl[:, c, j, :], rhs=hT[:, c, cols], start=(c == 0), stop=(c == NCH - 1)),
                                            reads=[("wsl", j)], writes=[pk])
                            return f

                        def evK():
                            fw.emit("dve", lambda e: e.tensor_copy(out=KA[0][:, cols], in_=O2[:, :]), reads=[pk], writes=[("KA", 0)])

                        def evQ():
                            fw.emit("dve", lambda e: e.tensor_scalar(out=QA[0][0:64, cols], in0=O2[0:64, :], scalar1=float(SCALE), scalar2=None, op0=ALU.mult),
                                    reads=[pk], writes=[("QA", 0)])
                            fw.emit("dve", lambda e: e.tensor_scalar(out=QA[1][64:128, cols], in0=O2[64:128, :], scalar1=float(SCALE), scalar2=None, op0=ALU.mult),
                                    reads=[pk], writes=[("QA", 1)])

                        def mmV(q):
                            def f():
                                tt = tb * 4 + q
                                for c in range(NCH):
                                    fw.emit("pe", lambda e, c=c: e.matmul(O2[:, q * 128:(q + 1) * 128], lhsT=hT[:, c, tt * 128:(tt + 1) * 128],
                                                                         rhs=wsl[:, c, 2, :], start=(c == 0), stop=(c == NCH - 1)),
                                            reads=[("wsl", 2)], writes=[pk])
                            return f

                        def evV():
                            fw.emit("dve", lambda e: e.tensor_copy(out=V[:, tb * 4:(tb + 1) * 4, :].rearrange("p a b -> p (a b)"), in_=O2[:, :]),
                                    reads=[pk], writes=["V"])

                        def seq(*fs):
                            def f():
                                for x in fs:
                                    x()
                            return f
                        pieces.append(mmK(0, 4, 1))
                        pieces.append(seq(mmK(4, 8, 1), evK))
                        pieces.append(mmK(0, 4, 0))
                        pieces.append(seq(mmK(4, 8, 0), evQ))
                        pieces.append(mmV(0)); pieces.append(mmV(1)); pieces.append(mmV(2))
                        pieces.append(seq(mmV(3), evV))
                        return pieces

                    if is_sb:
                        for u in sb_inproj_pieces(0):
                            u()
                    if not is_sb:
                        inb = Zv
                        ib = 0
                        for j in (1, 0):
                            for tb in range(NQB):
                                cols = slice(tb * 512, (tb + 1) * 512)
                                if is_sb:
                                    ps = inb[ib % 6]; pk = ("zb", ib % 6); ib += 1
                                    for c in range(NCH):
                                        fw.emit("pe", lambda e, ps=ps, j=j, c=c, cols=cols: e.matmul(ps[:, :], lhsT=wsl[:, c, j, :], rhs=hT[:, c, cols],
                                                                                                    start=(c == 0), stop=(c == NCH - 1)),
                                                reads=[("wsl", j)], writes=[pk])
                                    if j == 0:
                                        fw.emit("dve", lambda e, ps=ps, cols=cols: e.tensor_scalar(out=QA[0][0:64, cols], in0=ps[0:64, :], scalar1=float(SCALE),
                                                                                                  scalar2=None, op0=ALU.mult),
                                                reads=[pk], writes=[("QA", 0)])
                                        fw.emit("dve", lambda e, ps=ps, cols=cols: e.tensor_scalar(out=QA[1][64:128, cols], in0=ps[64:128, :], scalar1=float(SCALE),
                                                                                                  scalar2=None, op0=ALU.mult),
                                                reads=[pk], writes=[("QA", 1)])
                                    else:
                                        fw.emit("act", lambda e, ps=ps, cols=cols: e.activation(out=KA[0][:, cols], in_=ps[:, :], func=AF.Copy),
                                                reads=[pk], writes=[("KA", 0)])
                                else:
                                    for m2 in range(2):
                                        ps = inb[ib % 6]; pk = ("zb", ib % 6); ib += 1
                                        for c in range(NCH):
                                            fw.emit("pe", lambda e, ps=ps, j=j, c=c, cols=cols, m2=m2: e.matmul(ps[0:64, :], lhsT=wsl[:, c, j, 64 * m2:64 * m2 + 64], rhs=hT[:, c, cols],
                                                                                                              start=(c == 0), stop=(c == NCH - 1)),
                                                    reads=[("wsl", j)], writes=[pk])
                                        if j == 0:
                                            fw.emit("dve", lambda e, ps=ps, cols=cols, m2=m2: e.tensor_scalar(out=QA[m2][0:64, cols], in0=ps[0:64, :], scalar1=float(SCALE),
                                                                                                             scalar2=None, op0=ALU.mult),
                                                    reads=[pk], writes=[("QA", m2)])
                                        else:
                                            fw.emit("act", lambda e, ps=ps, cols=cols, m2=m2: e.activation(out=KA[m2][0:64, cols], in_=ps[0:64, :], func=AF.Copy),
                                                    reads=[pk], writes=[("KA", m2)])
                        for t4 in range(NT // 4):
                            ps = inb[ib % 6]; pk = ("zb", ib % 6); ib += 1
                            for q in range(4):
                                tt = t4 * 4 + q
                                for c in range(NCH):
                                    fw.emit("pe", lambda e, ps=ps, q=q, c=c, tt=tt: e.matmul(ps[:, q * 128:(q + 1) * 128], lhsT=hT[:, c, tt * 128:(tt + 1) * 128],
                                                                                            rhs=wsl[:, c, 2, :], start=(c == 0), stop=(c == NCH - 1)),
                                            reads=[("wsl", 2)], writes=[pk])
                            if t4 % 2 == 0:
                                fw.emit("dve", lambda e, ps=ps, t4=t4: e.tensor_copy(out=V[:, t4 * 4:(t4 + 1) * 4, :].rearrange("p a b -> p (a b)"), in_=ps[:, :]),
                                        reads=[pk], writes=["V"])
                            else:
                                fw.emit("act", lambda e, ps=ps, t4=t4: e.activation(out=V[:, t4 * 4:(t4 + 1) * 4, :].rearrange("p a b -> p (a b)"), in_=ps[:, :], func=AF.Copy),
                                        reads=[pk], writes=["V"])
                    if not is_sb and g + 1 < 8:
                        load_wsl(g + 1)
                    zk = [("zb", 0), ("zb", 1), ("zb", 2), ("zb", 3)]

                    if is_sb:
                        tasks = []
                        for qb in range(NQB):
                            for hh in range(2):
                                nkb = 4 * (qb + 1)
                                kbs = list(reversed(range(nkb)))
                                for n in range(nkb // 2):
                                    kA, kB = kbs[2 * n], kbs[2 * n + 1]
                                    tasks.append(dict(qb=qb, hh=hh, kA=kA, kB=kB, first=(n == 0), last=(n == nkb // 2 - 1),
                                                      lA=kA - 4 * qb, lB=kB - 4 * qb))
                        NTK = len(tasks)

                        def pairv(tile, c0):
                            if c0 == 0:
                                return tile[:, :]
                            return tile[:, :].rearrange("p (h x) -> p h x", h=2)[:, :, c0:512]

                        def sb_s1(i):
                            t = tasks[i]
                            hh = t["hh"]; q0 = 512 * t["qb"]
                            c0 = 128 * max(t["lB"], 0)
                            zz = ZZ[i % 3]
                            zkA, zkB = ("zb", 2 * (i % 3)), ("zb", 2 * (i % 3) + 1)
                            for half, kb, zkk in ((0, t["kA"], zkA), (1, t["kB"], zkB)):
                                fw.emit("pe", lambda e, half=half, kb=kb: e.matmul(zz[:, half * 512 + c0:(half + 1) * 512], lhsT=KA[0][:, kb * 128:(kb + 1) * 128],
                                                                                  rhs=QA[hh][:, q0 + c0:q0 + 512], start=True, stop=True),
                                        reads=[("KA", 0), ("QA", hh)], writes=[zkk])
                            if t["lA"] >= 0:
                                fw.emit("pe", lambda e: e.matmul(zz[:, c0:c0 + 256], lhsT=ident, rhs=cb[:, C_FULL:C_FULL + 256], start=False, stop=True, skip_group_check=True),
                                        reads=["cb"], writes=[zkA])
                            if t["lB"] >= 0:
                                fw.emit("pe", lambda e: e.matmul(zz[:, 512 + c0:512 + c0 + 128], lhsT=ident, rhs=cb[:, C_TRI:C_TRI + 128], start=False, stop=True, skip_group_check=True),
                                        reads=["cb"], writes=[zkB])
                            fw.emit("act", lambda e: e.activation(out=pairv(E2[i % 2], c0), in_=pairv(zz, c0), func=AF.Exp),
                                    reads=[zkA, zkB], writes=[("E", i % 2)])
                            fw.emit("act", lambda e: e.activation(out=pairv(SP2[i % 2], c0), in_=pairv(E2[i % 2], c0), func=AF.Ln, bias=1.0),
                                    reads=[("E", i % 2)], writes=[("SP", i % 2)])

                        def sb_s2(i):
                            t = tasks[i]
                            c0 = 128 * max(t["lB"], 0)
                            zz = ZZ[i % 3]
                            sp = SP2[i % 2]
                            zkA, zkB = ("zb", 2 * (i % 3)), ("zb", 2 * (i % 3) + 1)
                            fw.emit("pe", lambda e: e.matmul(zz[:, c0:512], lhsT=negU, rhs=sp[:, c0:512], start=False, stop=True, skip_group_check=True),
                                    reads=[("SP", i % 2), "cb"], writes=[zkA])
                            if not t["first"]:
                                fw.emit("pe", lambda e: e.matmul(zz[:, c0:512], lhsT=negones, rhs=SC[1][:, c0:512], start=False, stop=True, skip_group_check=True),
                                        reads=[("SC", 1), "cb"], writes=[zkA])
                            if t["first"]:
                                fw.emit("pool", lambda e: e.memset(SC[0][:, 0:256], 0.0), writes=[("SC", 0)])
                                fw.emit("pool", lambda e: e.memset(SC[1][:, 0:256], 0.0), writes=[("SC", 1)])
                                fw.emit("dve", lambda e: e.tensor_copy(out=SC[0][:, c0:512], in_=sp[:, c0:512]),
                                        reads=[("SP", i % 2)], writes=[("SC", 0)])
                            else:
                                fw.emit("dve", lambda e: e.tensor_tensor(out=SC[0][:, c0:512], in0=SC[1][:, c0:512], in1=sp[:, c0:512], op=ALU.add),
                                        reads=[("SP", i % 2), ("SC", 1)], writes=[("SC", 0)])
                            fw.emit("pe", lambda e: e.matmul(zz[:, 512 + c0:1024], lhsT=negU, rhs=sp[:, 512 + c0:1024], start=False, stop=True, skip_group_check=True),
                                    reads=[("SP", i % 2), "cb"], writes=[zkB])
                            fw.emit("pe", lambda e: e.matmul(zz[:, 512 + c0:1024], lhsT=negones, rhs=SC[0][:, c0:512], start=False, stop=True, skip_group_check=True),
                                    reads=[("SC", 0), "cb"], writes=[zkB])
                            if not t["last"]:
                                fw.emit("dve", lambda e: e.tensor_tensor(out=SC[1][:, c0:512], in0=SC[0][:, c0:512], in1=sp[:, 512 + c0:1024], op=ALU.add),
                                        reads=[("SP", i % 2), ("SC", 0)], writes=[("SC", 1)])
                            fw.emit("act", lambda e: e.activation(out=pairv(W2[i % 3], c0), in_=pairv(zz, c0), func=AF.Exp),
                                    reads=[zkA, zkB], writes=[("W", i % 3)])

                        def sb_s3(i):
                            t = tasks[i]
                            hh = t["hh"]; qb = t["qb"]
                            c0 = 128 * max(t["lB"], 0)
                            for half, kb in ((0, t["kA"]), (1, t["kB"])):
                                fw.emit("pe", lambda e, half=half, kb=kb: e.matmul(O1[:, c0:512], lhsT=V[:, kb, :], rhs=W2[i % 3][:, half * 512 + c0:(half + 1) * 512],
                                                                                  start=(t["first"] and half == 0), stop=(t["last"] and half == 1), skip_group_check=True),
                                        reads=[("W", i % 3), "V"], writes=[("OB", 0)])
                            if t["last"]:
                                hb = 64 * hh
                                fw.emit("dve", lambda e: e.tensor_copy(out=oT[hb:hb + 64, g, qb * 512:(qb + 1) * 512], in_=O1[hb:hb + 64, :]),
                                        reads=[("OB", 0)], writes=[("oT", g, qb, hh)])

                        pending = []
                        last_qb = -1
                        for i in range(NTK + 2):
                            if i < NTK:
                                if tasks[i]["qb"] != last_qb:
                                    last_qb = tasks[i]["qb"]
                                    assert not pending
                                    if last_qb + 1 < NQB:
                                        pending = sb_inproj_pieces(last_qb + 1)
                                sb_s1(i)
                            if 0 <= i - 1 < NTK:
                                sb_s2(i - 1)
                            if 0 <= i - 2 < NTK:
                                sb_s3(i - 2)
                            if i < NTK:
                                for _ in range(2 if tasks[i]["qb"] == 0 else 1):
                                    if pending:
                                        pending.pop(0)()
                                        if not pending and tasks[i]["qb"] == NQB - 2 and g + 1 < 8:
                                            load_wsl(g + 1)
                    else:
                        h = g - 4
                        tasks = []
                        for qb in range(NQB):
                            for c in range(2):
                                nkb = 4 * (qb + 1)
                                for kb in range(nkb):
                                    tasks.append(dict(qb=qb, c=c, kb=kb, first=(kb == 0), last=(kb == nkb - 1), kbl=kb - 4 * qb))
                        NTK = len(tasks)

                        def df_s1(i):
                            t = tasks[i]
                            m2 = t["c"]; q0 = 512 * t["qb"]; kb = t["kb"]
                            c0 = 128 * max(t["kbl"], 0)
                            z = Z[i % 4]
                            fw.emit("pe", lambda e: e.matmul(z[:, c0:512], lhsT=KA[m2][:, kb * 128:(kb + 1) * 128], rhs=QA[m2][:, q0 + c0:q0 + 512],
                                                             start=True, stop=True),
                                    reads=[("KA", m2), ("QA", m2)], writes=[zk[i % 4]])
                            if t["kbl"] >= 0:
                                fw.emit("pe", lambda e: e.matmul(z[:, c0:c0 + 128], lhsT=ident, rhs=cb[:, C_TRI2:C_TRI2 + 128], start=False, stop=True, skip_group_check=True),
                                        reads=["cb"], writes=[zk[i % 4]])
                            cblk = -SLOPES[h] * 128.0 * (4 * t["qb"] - kb)
                            fw.emit("act", lambda e: e.activation(out=W[i % 3][:, c0:512], in_=z[:, c0:512], func=AF.Exp, bias=float(cblk)),
                                    reads=[zk[i % 4]], writes=[("W", i % 3)])

                        def df_s2(i):
                            t = tasks[i]
                            kb = t["kb"]; qb = t["qb"]; m2 = t["c"]
                            c0 = 128 * max(t["kbl"], 0)
                            OO = OB[m2]
                            DD = D1 if m2 == 0 else D2
                            fw.emit("pe", lambda e: e.matmul(OO[:, c0:512], lhsT=V[:, kb, :], rhs=W[i % 3][:, c0:512], start=t["first"], stop=t["last"]),
                                    reads=[("W", i % 3), "V"], writes=[("OB", m2)])
                            fw.emit("pe", lambda e: e.matmul(DD[:, c0:512], lhsT=ones, rhs=W[i % 3][:, c0:512], start=t["first"], stop=t["last"]),
                                    reads=[("W", i % 3), "cb"], writes=[("zb", 4 + m2)])
                            if t["last"] and m2 == 1:
                                fw.emit("act", lambda e: e.activation(out=OS[0][:], in_=O1[:, :], func=AF.Copy), reads=[("OB", 0)], writes=[("OS", 0)])
                                fw.emit("dve", lambda e: e.reciprocal(out=R1, in_=D1), reads=[("zb", 4)], writes=[("E", 0)])
                                fw.emit("act", lambda e: e.activation(out=OS[1][:], in_=O2[:, :], func=AF.Copy), reads=[("OB", 1)], writes=[("OS", 1)])
                                fw.emit("dve", lambda e: e.reciprocal(out=R2, in_=D2), reads=[("zb", 5)], writes=[("E", 1)])
                                fw.emit("pool", lambda e: e.tensor_tensor(out=R1, in0=OS[0][:], in1=R1, op=ALU.mult),
                                        reads=[("OS", 0), ("E", 0)], writes=[("E", 0)])
                                fw.emit("dve", lambda e: e.scalar_tensor_tensor(out=R2, in0=R2, scalar=neglam[:, 0:1], in1=OS[1][:], op0=ALU.mult, op1=ALU.mult),
                                        reads=[("OS", 1), ("E", 1), "neglam"], writes=[("E", 1)])
                                fw.emit("pool", lambda e: e.tensor_tensor(out=oT[:, g, qb * 512:(qb + 1) * 512], in0=R1, in1=R2, op=ALU.add),
                                        reads=[("E", 0), ("E", 1)], writes=[("oT", g, qb, 0)])

                        for i in range(NTK + 2):
                            if i < NTK:
                                df_s1(i)
                            if 0 <= i - 2 < NTK:
                                df_s2(i - 2)
                if dbg:
                    fw.emit("sp", lambda e: e.dma_start(out=dbg_o[:, :, :], in_=oT[:]), reads=[("oT", g, qb, hh) for g in range(8) for qb in range(NQB) for hh in range(2)], dma="dbgo")
                if stop == 1:
                    fw.flush("pa", final_waits=[(fw.sems["dbgo"], 16)])
                    return nc
                fw.flush("pa", final_waits=[(fw.sems["dbgo"], 16)] if dbg else ())

        with ExitStack() as esb:
            X1 = sb("X1", [128, TPC, D], F32, esb)
            wmisc = sb("wmisc", [128, NCH, D], BF16, esb)
            wpp = sb("wpp", [128, 2, D], BF16, esb)
            wg = [sb(f"wg{i}", [128, NCH, DEXP], BF16, esb) for i in range(2)]
            wu = [sb(f"wu{i}", [128, NCH, DEXP], BF16, esb) for i in range(2)]
            wd = [sb(f"wd{i}", [128, 4, D], BF16, esb) for i in range(2)]
            hid = [sb("hid0", [128, 4, 512], BF16, esb)] * 2
            sg = [sb(f"sg{i}", [128, 512], BF16, esb) for i in range(2)]
            xn32 = sb("xn32", [128, D], F32, esb)
            h32 = sb("h32", [128, NCH, 128], F32, esb)
            wr32 = sb("wr32", [128, NCH, 20], F32, esb)
            brb = sb("brb", [128, 20], F32, esb)
            gfb = sb("gfb", [128, D], F32, esb)
            sq2 = sb("sq2", [128, 1024], BF16, esb)
            sq = [sq2[:, 0:512], sq2[:, 512:1024]]
            sqf = sq2[:].bitcast(F32)
            rs_t = sb("rs", [128, 512], F32, esb)
            rs = rs_t[:]
            junkb = sb("junkb", [128, D], BF16, esb)
            st = sb("stat", [128, 3, TPC, 4], F32, esb)
            lg = sb("lg", [128, TPC, 20], F32, esb)
            rt = sb("rt", [128, TPC, 64], F32, esb)
            gates = sb("gates", [128, TPC, NEXP], F32, esb)
            pl = [sb(f"pl{i}", [128, PLE], F32, esb) for i in range(2)]
            plb = sb("plb", [128, PLE], BF16, esb)
            pT = sb("pT", [128, 2, 128], BF16, esb)
            PB = [pst(f"PB{i}", [128, 512], F32, esb) for i in range(8)]

            wout_v = wout_d.rearrange("(c p) n -> p c n", p=128)
            wpg_v = wpg_d.rearrange("(c p) n -> p c n", p=128)
            wpp_v = wpp_d.rearrange("(c p) n -> p c n", p=128)

            fw.emit("sp", lambda e: e.dma_start(out=wr32[:], in_=wr_d.rearrange("(c p) n -> p c n", p=128)), writes=["wr32"], dma="wr32")
            fw.emit("sp", lambda e: e.dma_start(out=brb[:], in_=br_d.partition_broadcast(128)), writes=["brb"], dma="brb")
            fw.emit("sp", lambda e: e.dma_start(out=gfb[:], in_=gfin_d.partition_broadcast(128)), writes=["gfb"], dma="gfb")
            fw.emit("pool", lambda e: e.dma_start(out=wpp[:], in_=wpp_v), writes=["wpp"], dma="wpp")

            def load_expert(e_idx, b):
                for c in range(0, NCH, 4):
                    fw.emit("pool", lambda e, c=c: e.dma_start(out=wg[b][:, c:c + 4, :], in_=weg_d[e_idx].rearrange("(c p) n -> p c n", p=128)[:, c:c + 4, :]),
                            writes=[("wg", b, c)], dma="wg%d_%d" % (b, c))
                    fw.emit("pool", lambda e, c=c: e.dma_start(out=wu[b][:, c:c + 4, :], in_=weu_d[e_idx].rearrange("(c p) n -> p c n", p=128)[:, c:c + 4, :]),
                            writes=[("wu", b, c)], dma="wu%d_%d" % (b, c))
                for c in range(0, 4, 2):
                    fw.emit("pool", lambda e, c=c: e.dma_start(out=wd[b][:, c:c + 2, :], in_=wed_d[e_idx].rearrange("(c p) n -> p c n", p=128)[:, c:c + 2, :]),
                            writes=[("wd", b, c)], dma="wd%d_%d" % (b, c))

            def rstd_batch(kind):
                fw.emit("act", lambda e: e.activation(out=st[:, kind, :, 1], in_=st[:, kind, :, 0], func=AF.Ln, scale=1.0 / D, bias=EPS),
                        reads=[("st", kind, t) for t in range(TPC)], writes=[("stln", kind)])
                fw.emit("act", lambda e: e.activation(out=st[:, kind, :, 2], in_=st[:, kind, :, 1], func=AF.Exp, scale=-0.5),
                        reads=[("stln", kind)], writes=[("strs", kind)])

            def norm_T(kind, t, gk, tok0, want32):
                fw.emit("dve", lambda e: e.tensor_scalar(out=xn32[:], in0=X1[:, t, :], scalar1=st[:, kind, t, 2:3], scalar2=None, op0=ALU.mult),
                        reads=[("X1", t), ("strs", kind)], writes=["xn32"])
                for c in range(NCH):
                    bk = 2 + c // 4
                    fw.emit("pe", lambda e, c=c, bk=bk: e.transpose(out=PB[bk][:, (c % 4) * 128:(c % 4 + 1) * 128], in_=xn32[:, c * 128:(c + 1) * 128], identity=identf[:]),
                            reads=["xn32", "identf"], writes=[("PB", bk)])
                for c in range(NCH):
                    bk = 2 + c // 4
                    src = PB[bk][:, (c % 4) * 128:(c % 4 + 1) * 128]
                    if want32:
                        fw.emit("dve", lambda e, c=c, src=src: e.tensor_scalar(out=h32[:, c, :], in0=src, scalar1=gcols[:, gk, c:c + 1], scalar2=None, op0=ALU.mult),
                                reads=[("PB", bk), ("gcols", gk)], writes=[("h32", c)])
                    else:
                        if True:
                            fw.emit("dve", lambda e, c=c, src=src: e.tensor_scalar(out=oT[:, c, tok0:tok0 + 128], in0=src, scalar1=gcols[:, gk, c:c + 1], scalar2=None, op0=ALU.mult),
                                    reads=[("PB", bk), ("gcols", gk)], writes=[("oTt", tok0)])
                        else:
                            fw.emit("act", lambda e, c=c, src=src: e.activation(out=oT[:, c, tok0:tok0 + 128], in_=src, func=AF.Copy, scale=gcols[:, gk, c:c + 1]),
                                    reads=[("PB", bk), ("gcols", gk)], writes=[("oTt", tok0)])
                if want32:
                    fw.emit("dve", lambda e: e.tensor_copy(out=oT[:, :, tok0:tok0 + 128], in_=h32[:]),
                            reads=[("h32", c) for c in range(NCH)], writes=[("oTt", tok0)])

            def onorm(t0):
                for blk in range(CH // 512):
                    c0 = t0 + blk * 512
                    for grp in range(5):
                        chunks = [0, 1, 2, 3] if grp == 0 else [3 + grp]
                        nfeat = 512.0 if grp == 0 else 128.0
                        for n, c in enumerate(chunks):
                            sqb = sq[n % 2]
                            fw.emit("act", lambda e, c=c, sqb=sqb: e.activation(out=sqb, in_=oT[:, c, c0:c0 + 512], func=AF.Square),
                                    reads=[("oTb", c, c0)], writes=[("sq", n % 2)])
                            fw.emit("pe", lambda e, sqb=sqb, n=n: e.matmul(PB[7][:, :], lhsT=ones, rhs=sqb, start=(n == 0), stop=(n == len(chunks) - 1)),
                                    reads=[("sq", n % 2), "cb"], writes=[("PB", 7)])
                        fw.emit("act", lambda e, nfeat=nfeat: e.activation(out=rs, in_=PB[7][:, :], func=AF.Ln, scale=1.0 / nfeat, bias=EPS),
                                reads=[("PB", 7)], writes=["rs"])
                        fw.emit("act", lambda e: e.activation(out=rs, in_=rs, func=AF.Exp, scale=-0.5), reads=["rs"], writes=["rs"])
                        for c in chunks:
                            gcol = gcols[:, 3, c:c + 1] if grp == 0 else gcols[:, 4, 0:1]
                            fw.emit("dve", lambda e, c=c, gcol=gcol: e.scalar_tensor_tensor(out=oT[:, c, c0:c0 + 512], in0=oT[:, c, c0:c0 + 512], scalar=gcol, in1=rs,
                                                                                           op0=ALU.mult, op1=ALU.mult),
                                    reads=["rs", ("oTb", c, c0), ("gcols", 3), ("gcols", 4)], writes=[("oTb", c, c0)])

            for ck in range(NCK):
                t0 = ck * CH
                fw.emit("pool", lambda e: e.dma_start(out=wmisc[:], in_=wout_v), writes=["wmisc"], dma="wmisc")
                load_expert(0, 0)
                if ck == 0:
                    onorm(t0)
                for t in range(TPC):
                    tok0 = t0 + t * 128
                    c0 = t0 + (t // 4) * 512
                    b = t % 2
                    fw.emit("sp", lambda e, t=t, tok0=tok0: e.dma_start(out=X1[:, t, :], in_=x_d[tok0:tok0 + 128, :]), writes=[("X1", t)], dma="xl%d" % t)
                    for nh in range(2):
                        for c in range(NCH):
                            fw.emit("pe", lambda e, nh=nh, c=c, tok0=tok0: e.matmul(PB[nh][:, :], lhsT=oT[:, c, tok0:tok0 + 128], rhs=wmisc[:, c, nh * 512:(nh + 1) * 512],
                                                                                   start=(c == 0), stop=(c == NCH - 1)),
                                    reads=[("oTb", c, c0), "wmisc", ("oTt", tok0)], writes=[("PB", nh)])
                        fw.emit("dve", lambda e, nh=nh, t=t, b=b: e.tensor_tensor(out=X1[:, t, nh * 512:(nh + 1) * 512], in0=PB[nh][:, :], in1=X1[:, t, nh * 512:(nh + 1) * 512], op=ALU.add),
                                reads=[("PB", nh), ("X1", t)], writes=[("X1", t)])
                    fw.emit("act", lambda e, t=t: e.activation(out=junkb[:], in_=X1[:, t, :], func=AF.Square, accum_out=st[:, 0, t, 0:1]),
                            reads=[("X1", t)], writes=["junkb", ("st", 0, t)])
                    if dbg:
                        fw.emit("sp", lambda e, t=t, tok0=tok0: e.dma_start(out=dbg_x1[tok0:tok0 + 128, :], in_=X1[:, t, :]), reads=[("X1", t)], dma="dbgx%d" % t)
                rstd_batch(0)
                for t in range(TPC):
                    tok0 = t0 + t * 128
                    norm_T(0, t, 1, tok0, True)
                    for c in range(NCH):
                        fw.emit("pe", lambda e, c=c: e.matmul(PB[7][:, 0:20], lhsT=h32[:, c, :], rhs=wr32[:, c, :], start=(c == 0), stop=(c == NCH - 1)),
                                reads=[("h32", cc) for cc in range(NCH)] + ["wr32"], writes=[("PB", 7)])
                    fw.emit("dve", lambda e, t=t: e.tensor_tensor(out=lg[:, t, :], in0=PB[7][:, 0:20], in1=brb[:], op=ALU.add),
                            reads=[("PB", 7), "brb"], writes=["lg"])
                G = lg[:, :, 0:4]
                EL = lg[:, :, 4:20]
                gmax = rt[:, :, 0:1]
                goh = rt[:, :, 1:5]
                gex = rt[:, :, 5:9]
                gsum = rt[:, :, 9:10]
                gw = rt[:, :, 10:11]
                msk = rt[:, :, 16:32]
                m1 = rt[:, :, 11:12]
                m2 = rt[:, :, 12:13]
                oh1 = rt[:, :, 32:48]
                oh2 = rt[:, :, 48:64]
                dlt = rt[:, :, 13:14]
                w1 = rt[:, :, 14:15]
                w2 = rt[:, :, 15:16]
                BIG = 1.0e4

                def dv(fn, r=("lg", "rt"), w=("rt",)):
                    fw.emit("dve", fn, reads=list(r), writes=list(w))

                dv(lambda e: e.tensor_reduce(out=gmax, in_=G, axis=AX.X, op=ALU.max))
                dv(lambda e: e.tensor_tensor(out=goh, in0=G, in1=gmax.to_broadcast([128, TPC, 4]), op=ALU.is_equal))
                dv(lambda e: e.tensor_tensor(out=gex, in0=G, in1=gmax.to_broadcast([128, TPC, 4]), op=ALU.subtract))
                fw.emit("act", lambda e: e.activation(out=gex, in_=gex, func=AF.Exp), reads=["rt"], writes=["rt"])
                dv(lambda e: e.tensor_reduce(out=gsum, in_=gex, axis=AX.X, op=ALU.add))
                dv(lambda e: e.reciprocal(out=gw, in_=gsum))
                dv(lambda e: e.tensor_scalar(out=gex, in0=goh, scalar1=-1.0, scalar2=BIG, op0=ALU.add, op1=ALU.mult))
                dv(lambda e: e.tensor_tensor(out=msk.rearrange("p t (g k) -> p t g k", k=4), in0=EL.rearrange("p t (g k) -> p t g k", k=4),
                                             in1=gex.unsqueeze(3).to_broadcast([128, TPC, 4, 4]), op=ALU.add))
                dv(lambda e: e.tensor_reduce(out=m1, in_=msk, axis=AX.X, op=ALU.max))
                dv(lambda e: e.tensor_tensor(out=oh1, in0=msk, in1=m1.to_broadcast([128, TPC, 16]), op=ALU.is_equal))
                dv(lambda e: e.scalar_tensor_tensor(out=msk, in0=oh1, scalar=-BIG, in1=msk, op0=ALU.mult, op1=ALU.add))
                dv(lambda e: e.tensor_reduce(out=m2, in_=msk, axis=AX.X, op=ALU.max))
                dv(lambda e: e.tensor_tensor(out=oh2, in0=msk, in1=m2.to_broadcast([128, TPC, 16]), op=ALU.is_equal))
                dv(lambda e: e.tensor_tensor(out=dlt, in0=m2, in1=m1, op=ALU.subtract))
                fw.emit("act", lambda e: e.activation(out=dlt, in_=dlt, func=AF.Exp), reads=["rt"], writes=["rt"])
                dv(lambda e: e.tensor_scalar(out=w1, in0=dlt, scalar1=1.0, scalar2=None, op0=ALU.add))
                dv(lambda e: e.reciprocal(out=w1, in_=w1))
                dv(lambda e: e.tensor_tensor(out=w2, in0=dlt, in1=w1, op=ALU.mult))
                dv(lambda e: e.tensor_tensor(out=w1, in0=w1, in1=gw, op=ALU.mult))
                dv(lambda e: e.tensor_tensor(out=w2, in0=w2, in1=gw, op=ALU.mult))
                dv(lambda e: e.tensor_tensor(out=oh1, in0=oh1, in1=w1.to_broadcast([128, TPC, 16]), op=ALU.mult))
                dv(lambda e: e.tensor_tensor(out=oh2, in0=oh2, in1=w2.to_broadcast([128, TPC, 16]), op=ALU.mult))
                dv(lambda e: e.tensor_tensor(out=gates[:], in0=oh1, in1=oh2, op=ALU.add), w=("gates",))

                for ex in range(NEXP):
                    b = ex % 2
                    if ex + 1 < NEXP:
                        load_expert(ex + 1, (ex + 1) % 2)
                    if ex == 4 and ck + 1 < NCK:
                        onorm(t0 + CH)
                    for tb in range(CH // 512):
                        hb_ = hid[tb % 2]
                        c0 = t0 + tb * 512
                        for jc in range(4):
                            gp = PB[jc % 2]; up = PB[2 + jc % 2]
                            for c in range(NCH):
                                fw.emit("pe", lambda e, gp=gp, c=c, jc=jc: e.matmul(gp[:, :], lhsT=wg[b][:, c, jc * 128:(jc + 1) * 128], rhs=oT[:, c, c0:c0 + 512],
                                                                                   start=(c == 0), stop=(c == NCH - 1)),
                                        reads=[("wg", b, 4 * (c // 4))] + [("oTt", c0 + 128 * q) for q in range(4)], writes=[("PB", jc % 2)])
                            for c in range(NCH):
                                fw.emit("pe", lambda e, up=up, c=c, jc=jc: e.matmul(up[:, :], lhsT=wu[b][:, c, jc * 128:(jc + 1) * 128], rhs=oT[:, c, c0:c0 + 512],
                                                                                   start=(c == 0), stop=(c == NCH - 1)),
                                        reads=[("wu", b, 4 * (c // 4))] + [("oTt", c0 + 128 * q) for q in range(4)], writes=[("PB", 2 + jc % 2)])
                            fw.emit("act", lambda e, gp=gp, jc=jc: e.activation(out=sg[jc % 2][:], in_=gp[:, :], func=AF.Silu),
                                    reads=[("PB", jc % 2)], writes=[("sg", jc % 2)])
                            fw.emit("dve", lambda e, up=up, jc=jc, hb_=hb_: e.tensor_tensor(out=hb_[:, jc, :], in0=up[:, :], in1=sg[jc % 2][:], op=ALU.mult),
                                    reads=[("PB", 2 + jc % 2), ("sg", jc % 2)], writes=[("hid", tb % 2, jc)])
                        for q in range(4):
                            t = tb * 4 + q
                            for nh in range(2):
                                yp = PB[4 + (2 * q + nh) % 3]
                                yk = ("PB", 4 + (2 * q + nh) % 3)
                                for jc in range(4):
                                    fw.emit("pe", lambda e, yp=yp, jc=jc, q=q, nh=nh, hb_=hb_: e.matmul(yp[:, :], lhsT=hb_[:, jc, q * 128:(q + 1) * 128],
                                                                                                       rhs=wd[b][:, jc, nh * 512:(nh + 1) * 512], start=(jc == 0), stop=(jc == 3)),
                                            reads=[("hid", tb % 2, jc), ("wd", b, 2 * (jc // 2))], writes=[yk])
                                fw.emit("dve", lambda e, yp=yp, t=t, nh=nh: e.scalar_tensor_tensor(out=X1[:, t, nh * 512:(nh + 1) * 512], in0=yp[:, :], scalar=gates[:, t, ex:ex + 1],
                                                                                                  in1=X1[:, t, nh * 512:(nh + 1) * 512], op0=ALU.mult, op1=ALU.add),
                                        reads=[yk, "gates", ("X1", t)], writes=[("X1", t)])
                fw.emit("pool", lambda e: e.dma_start(out=wmisc[:], in_=wpg_v), writes=["wmisc"], dma="wmisc")
                for t in range(TPC):
                    fw.emit("act", lambda e, t=t: e.activation(out=junkb[:], in_=X1[:, t, :], func=AF.Square, accum_out=st[:, 1, t, 0:1]),
                            reads=[("X1", t)], writes=["junkb", ("st", 1, t)])
                rstd_batch(1)
                for t in range(TPC):
                    tok0 = t0 + t * 128
                    b = t % 2
                    norm_T(1, t, 2, tok0, False)
                    fw.emit("sp", lambda e, b=b, tok0=tok0: e.dma_start(out=pl[b][:], in_=p_d[tok0:tok0 + 128, :]), writes=[("pl", b)], dma="pl%d" % b)
                    fw.emit("pool", lambda e, b=b: e.tensor_copy(out=plb[:], in_=pl[b][:]), reads=[("pl", b)], writes=["plb"])
                    tpv = PB[6][:, 0:128].bitcast(BF16)
                    for c2 in range(2):
                        fw.emit("pe", lambda e, c2=c2, tpv=tpv: e.transpose(out=tpv[:, c2 * 128:(c2 + 1) * 128], in_=plb[:, c2 * 128:(c2 + 1) * 128], identity=ident),
                                reads=["plb", "cb"], writes=[("PB", 6)])
                    fw.emit("dve", lambda e, tpv=tpv: e.tensor_copy(out=pT[:].rearrange("p a b -> p (a b)"), in_=tpv), reads=[("PB", 6)], writes=["pT"])
                    for nh in range(2):
                        for c in range(NCH):
                            fw.emit("pe", lambda e, nh=nh, c=c, tok0=tok0: e.matmul(PB[nh][:, :], lhsT=oT[:, c, tok0:tok0 + 128], rhs=wmisc[:, c, nh * 512:(nh + 1) * 512],
                                                                                   start=(c == 0), stop=(c == NCH - 1)),
                                    reads=[("oTt", tok0), "wmisc"], writes=[("PB", nh)])
                        for c2 in range(2):
                            fw.emit("pe", lambda e, nh=nh, c2=c2: e.matmul(PB[4 + nh][:, :], lhsT=pT[:, c2, :], rhs=wpp[:, c2, nh * 512:(nh + 1) * 512],
                                                                          start=(c2 == 0), stop=(c2 == 1)),
                                    reads=["pT", "wpp"], writes=[("PB", 4 + nh)])
                        sgv = h32[:].rearrange("p a b -> p (a b)")[:, nh * 512:(nh + 1) * 512]
                        sgk = [("h32", 4 * nh + cc) for cc in range(4)]
                        fw.emit("act", lambda e, nh=nh, sgv=sgv: e.activation(out=sgv, in_=PB[nh][:, :], func=AF.Sigmoid),
                                reads=[("PB", nh)], writes=sgk)
                        tmpb = rs if nh == 0 else sqf
                        tmpk = ["rs"] if nh == 0 else [("sq", 0), ("sq", 1)]
                        fw.emit("dve", lambda e, nh=nh, tmpb=tmpb, sgv=sgv: e.tensor_tensor(out=tmpb, in0=PB[4 + nh][:, :], in1=sgv, op=ALU.mult),
                                reads=[("PB", 4 + nh)] + sgk, writes=tmpk)
                        fw.emit("pool", lambda e, nh=nh, t=t, tmpb=tmpb: e.tensor_tensor(out=X1[:, t, nh * 512:(nh + 1) * 512], in0=X1[:, t, nh * 512:(nh + 1) * 512],
                                                                                        in1=tmpb, op=ALU.add),
                                reads=tmpk + [("X1", t)], writes=[("X1", t)])
                    fw.emit("act", lambda e, t=t: e.activation(out=junkb[:], in_=X1[:, t, :], func=AF.Square, accum_out=st[:, 2, t, 0:1]),
                            reads=[("X1", t)], writes=["junkb", ("st", 2, t)])
                rstd_batch(2)
                for t in range(TPC):
                    tok0 = t0 + t * 128
                    b = t % 2
                    fw.emit("dve", lambda e, t=t, b=b: e.scalar_tensor_tensor(out=X1[:, t, :], in0=X1[:, t, :], scalar=st[:, 2, t, 2:3], in1=gfb[:], op0=ALU.mult, op1=ALU.mult),
                            reads=[("X1", t), ("strs", 2), "gfb"], writes=[("X1", t)])
                    fw.emit("sp", lambda e, t=t, tok0=tok0: e.dma_start(out=out_d[tok0:tok0 + 128, :], in_=X1[:, t, :]), reads=[("X1", t)], dma="out%d" % t)
            finals = [(fw.sems[n], fw.dma_cnt[n]) for n in fw.sems if n.startswith("out") or n.startswith("dbg")]
            fw.flush("pb", final_waits=finals)
    return nc


_NC_CACHE = {}


def _prep_inputs(inputs, S):
    f = lambda a: np.ascontiguousarray(np.asarray(a, dtype=np.float32))
    shared = {
        "cst": make_consts(),
        "aug": make_aug(S),
        "g_mix": f(inputs["g_mix"][0]),
        "w_in": f(inputs["w_in"][0]),
        "lam4": f(np.stack([inputs["lambda_q1"][0], inputs["lambda_k1"][0], inputs["lambda_q2"][0], inputs["lambda_k2"][0]], 0)),
        "g_sb_out": f(inputs["g_sb_out"][0]),
        "g_df_out": f(inputs["g_df_out"][0]),
        "w_out": f(inputs["w_out"][0]),
        "g_ffn": f(inputs["g_ffn"][0]),
        "w_router": f(np.concatenate([inputs["w_router_group"][0], inputs["w_router_expert"][0]], axis=1)),
        "b_router": f(np.concatenate([inputs["b_router_group"][0], inputs["b_router_expert"][0]], axis=0)),
        "w_expert_gate": f(inputs["w_expert_gate"][0]),
        "w_expert_up": f(inputs["w_expert_up"][0]),
        "w_expert_down": f(inputs["w_expert_down"][0]),
        "g_ple": f(inputs["g_ple"][0]),
        "w_ple_gate": f(inputs["w_ple_gate"][0]),
        "w_ple_proj": f(inputs["w_ple_proj"][0]),
        "g_final": f(inputs["g_final"]),
    }
    return shared


def kernel(**inputs):
    x = np.asarray(inputs["x"], dtype=np.float32)
    p = np.asarray(inputs["p"], dtype=np.float32)
    B, S, _ = x.shape
    if S not in _NC_CACHE:
        _NC_CACHE[S] = build(S)
    nc = _NC_CACHE[S]
    shared = _prep_inputs(inputs, S)
    in_maps = []
    for b in range(B):
        m = dict(shared)
        m["x"] = np.ascontiguousarray(x[b])
        m["p"] = np.ascontiguousarray(p[0, b])
        in_maps.append(m)
    res = run_bass_kernel_spmd(nc, in_maps, core_ids=list(range(B)))
    return np.stack([np.asarray(r["out"], dtype=np.float32) for r in res.results], axis=0)
```

```python
import math
from contextlib import ExitStack

import numpy as np
import concourse.bass as bass
import concourse.mybir as mybir
from concourse.bass_utils import run_bass_kernel_spmd

F32 = mybir.dt.float32
BF16 = mybir.dt.bfloat16
AF = mybir.ActivationFunctionType
ALU = mybir.AluOpType
AX = mybir.AxisListType

D = 1024
NCH = 8
HD = 64
NEXP = 16
DEXP = 512
PLE = 256
EPS = 1e-6
SCALE = HD ** -0.5
SLOPES = [2.0 ** (-8.0 * (h + 1) / 4) for h in range(4)]
LAMBDA_INIT = 0.8 - 0.6 * math.exp(-0.3 * 0)
SAME_ENGINE_SYNC = True

C_ID, C_NEGU, C_ONES, C_NEGONES = 0, 128, 256, 384
C_KAUG = 512
C_QAUG = 1024
C_FULL = 3072
C_TRI = 3200
C_TRI2 = 3328
CW = 3456
MASK_BIG = 30000.0


def make_consts():
    c = np.zeros((128, CW), np.float32)
    j = np.arange(128)[:, None]
    s = np.arange(128)[None, :]
    c[:, C_ID:C_ID + 128] = (j == s)
    c[:, C_NEGU:C_NEGU + 128] = -(j >= s).astype(np.float32)
    c[:, C_ONES:C_ONES + 128] = 1.0
    c[:, C_NEGONES:C_NEGONES + 128] = -1.0
    c[:, C_FULL:C_FULL + 128] = -MASK_BIG
    c[:, C_TRI:C_TRI + 128] = np.where(s <= j, -MASK_BIG, 0.0)
    c[:, C_TRI2:C_TRI2 + 128] = np.where(s < j, -MASK_BIG, 0.0)
    tl = np.arange(512)
    for h in range(4):
        sl = SLOPES[h]
        k = c[:, C_KAUG + 128 * h:C_KAUG + 128 * (h + 1)]
        k[0, :] = sl * np.arange(128)
        k[1, :] = 1.0
        k[2, :] = 1.0
        q = c[:, C_QAUG + 512 * h:C_QAUG + 512 * (h + 1)]
        q[0, :] = 1.0
        q[1, :] = -sl * (tl % 128)
        q[2, :] = -sl * 128.0 * (tl // 128)
    return c


def make_aug(S):
    a = np.zeros((4, 2, 3, S), np.float32)
    t = np.arange(S)
    for h in range(4):
        sl = SLOPES[h]
        a[h, 0, 0] = 1.0
        a[h, 0, 1] = -sl * (t % 128)
        a[h, 0, 2] = -sl * 128.0 * ((t // 128) % 4)
        a[h, 1, 0] = sl * (t % 128)
        a[h, 1, 1] = 1.0
        a[h, 1, 2] = 1.0
    return a


class _Rec:
    def __getattr__(self, name):
        def f(*a, **k):
            return (name, a, k)
        return f


class FW:
    def __init__(self, nc, es):
        self.nc = nc
        self.es = es
        self.engs = {"pe": nc.tensor, "act": nc.scalar, "dve": nc.vector, "pool": nc.gpsimd, "sp": nc.sync}
        self.prog = {e: es.enter_context(nc.semaphore("prog_" + e)) for e in self.engs}
        self.cnt = {e: 0 for e in self.engs}
        self.waited = {e: {} for e in self.engs}
        self.dma_cnt = {}
        self.sems = {}
        self.reset()

    def reset(self):
        self.ops = {e: [] for e in self.engs}
        self.lastw = {}
        self.readers = {}

    def dsem(self, name):
        if name not in self.sems:
            self.sems[name] = self.es.enter_context(self.nc.semaphore("d_" + name))
            self.dma_cnt[name] = 0
        return name

    def emit(self, eng, fn, reads=(), writes=(), dma=None):
        rec = fn(_Rec())
        deps = []
        for r in reads:
            t = self.lastw.get(r)
            if t is not None:
                deps.append(t)
        for w in writes:
            t = self.lastw.get(w)
            if t is not None:
                deps.append(t)
            deps.extend(self.readers.get(w, {}).values())
        waits = {}
        for (skey, sem, val, teng) in deps:
            if teng == eng and (eng == "pe" or not SAME_ENGINE_SYNC):
                continue
            if self.waited[eng].get(skey, -1) >= val:
                continue
            if waits.get(skey, (None, -1))[1] < val:
                waits[skey] = (sem, val)
        for skey, (sem, val) in waits.items():
            self.waited[eng][skey] = val
        if dma is not None:
            self.dsem(dma)
            self.dma_cnt[dma] += 16
            tok = ("d_" + dma, self.sems[dma], self.dma_cnt[dma], None)
            inc = (self.sems[dma], 16)
        else:
            self.cnt[eng] += 1
            tok = ("p_" + eng, self.prog[eng], self.cnt[eng], eng)
            inc = (self.prog[eng], 1)
        for w in writes:
            self.lastw[w] = tok
            self.readers[w] = {}
        for r in reads:
            self.readers.setdefault(r, {})[tok[0]] = tok
        self.ops[eng].append((list(waits.values()), rec, inc))
        return tok

    def flush(self, name, final_waits=()):
        nc = self.nc
        ops = self.ops
        with nc.Block() as block:
            def mk(ename):
                def body(e):
                    for waits, rec, inc in ops[ename]:
                        for sem, val in waits:
                            e.wait_ge(sem, val)
                        inst = getattr(e, rec[0])(*rec[1], **rec[2])
                        inst.then_inc(inc[0], inc[1])
                    if ename == "sp":
                        for sem, val in final_waits:
                            e.wait_ge(sem, val)
                return body
            block.sync(mk("sp"))
            block.tensor(mk("pe"))
            block.scalar(mk("act"))
            block.vector(mk("dve"))
            block.gpsimd(mk("pool"))
        self.reset()


def build(S, dbg=False, stop=9):
    assert S % 1024 == 0
    NT = S // 128
    NQB = S // 512
    CH = 1024
    NCK = S // CH
    TPC = CH // 128

    nc = bass.Bass("TRN2", target_bir_lowering=False)

    def din(name, shape):
        return nc.dram_tensor(name, list(shape), F32, kind="ExternalInput").ap()

    x_d = din("x", [S, D])
    p_d = din("p", [S, PLE])
    cst_d = din("cst", [128, CW])
    aug_d = din("aug", [4, 2, 3, S])
    gmix_d = din("g_mix", [D])
    win_d = din("w_in", [D, 3072])
    lam_d = din("lam4", [4, HD])
    gsb_d = din("g_sb_out", [512])
    gdf_d = din("g_df_out", [128])
    wout_d = din("w_out", [D, D])
    gffn_d = din("g_ffn", [D])
    wr_d = din("w_router", [D, 20])
    br_d = din("b_router", [20])
    weg_d = din("w_expert_gate", [NEXP, D, DEXP])
    weu_d = din("w_expert_up", [NEXP, D, DEXP])
    wed_d = din("w_expert_down", [NEXP, DEXP, D])
    gple_d = din("g_ple", [D])
    wpg_d = din("w_ple_gate", [D, D])
    wpp_d = din("w_ple_proj", [PLE, D])
    gfin_d = din("g_final", [D])
    out_d = nc.dram_tensor("out", [S, D], F32, kind="ExternalOutput").ap()
    if dbg:
        dbg_o = nc.dram_tensor("dbg_o", [128, NCH, S], BF16, kind="ExternalOutput").ap()
        dbg_x1 = nc.dram_tensor("dbg_x1", [S, D], F32, kind="ExternalOutput").ap()

    with ExitStack() as es:
        fw = FW(nc, es)

        def sb(name, shape, dt, stack=es):
            return stack.enter_context(nc.sbuf_tensor(name, list(shape), dt))

        def pst(name, shape, dt, stack):
            return stack.enter_context(nc.psum_tensor(name, list(shape), dt))

        oT = sb("oT", [128, NCH, S], BF16)
        cb = sb("cb", [128, CW], BF16)
        identf = sb("identf", [128, 128], F32)
        gcols = sb("gcols", [128, 5, NCH], F32)
        lamt = sb("lamt", [128, 4, HD], F32)
        lamw = sb("lamw", [128, 8], F32)
        neglam = sb("neglam", [128, 1], F32)

        ident = cb[:, C_ID:C_ID + 128]
        negU = cb[:, C_NEGU:C_NEGU + 128]
        ones = cb[:, C_ONES:C_ONES + 128]
        negones = cb[:, C_NEGONES:C_NEGONES + 128]

        with ExitStack() as esA:
            hT = sb("hT", [128, NCH, S], BF16, esA)
            with ExitStack() as es0:
                xt = [sb(f"xt{i}", [128, D], F32, es0) for i in range(2)]
                xn = [sb(f"xn{i}", [128, D], BF16, es0) for i in range(2)]
                junk = sb("junk0", [128, D], BF16, es0)
                ssq = sb("ssq0", [128, NT], F32, es0)
                lnv = sb("lnv0", [128, NT], F32, es0)
                rstd = sb("rstd0", [128, NT], F32, es0)
                tp = [pst(f"tp{i}", [128, D], BF16, es0) for i in range(2)]

                fw.emit("pool", lambda e: e.dma_start(out=cb[:], in_=cst_d[:, :]), writes=["cb"], dma="cst")
                fw.emit("sp", lambda e: e.dma_start(out=identf[:], in_=cst_d[:, C_ID:C_ID + 128]), writes=["identf"], dma="cst2")
                gst = sb("gst", [8, 5, 128], F32, es0)
                gps = pst("gps", [128, 5, 8], F32, es0)
                for k, (gd, nr) in enumerate([(gmix_d, 8), (gffn_d, 8), (gple_d, 8), (gsb_d, 4), (gdf_d, 1)]):
                    fw.emit("sp", lambda e, k=k, gd=gd, nr=nr: e.dma_start(out=gst[0:nr, k, :], in_=gd.rearrange("(c p) -> c p", p=128)),
                            writes=[("gst", k)], dma="gc%d" % k)
                    fw.emit("pe", lambda e, k=k, nr=nr: e.transpose(out=gps[:, k, 0:nr], in_=gst[0:nr, k, :], identity=identf[0:nr, 0:nr]),
                            reads=[("gst", k), "identf"], writes=["gps"])
                    fw.emit("dve", lambda e, k=k, nr=nr: e.tensor_copy(out=gcols[:, k, 0:nr], in_=gps[:, k, 0:nr]),
                            reads=["gps"], writes=[("gcols", k)])
                fw.emit("dve", lambda e: e.tensor_scalar(out=gcols[:, 4, 0:1], in0=gcols[:, 4, 0:1], scalar1=float(1.0 - LAMBDA_INIT),
                                                          scalar2=None, op0=ALU.mult),
                        reads=[("gcols", 4)], writes=[("gcols", 4)])
                if True:
                    fw.emit("sp", lambda e: e.dma_start(out=lamt[:].rearrange("p a b -> p (a b)"),
                                                         in_=lam_d.rearrange("a b -> (a b)").partition_broadcast(128)),
                            writes=["lamt"], dma="lam")
                    fw.emit("dve", lambda e: e.tensor_tensor(out=lamt[:, 0, :], in0=lamt[:, 0, :], in1=lamt[:, 1, :], op=ALU.mult),
                            reads=["lamt"], writes=["lamt"])
                    fw.emit("dve", lambda e: e.tensor_tensor(out=lamt[:, 2, :], in0=lamt[:, 2, :], in1=lamt[:, 3, :], op=ALU.mult),
                            reads=["lamt"], writes=["lamt"])
                    fw.emit("dve", lambda e: e.reduce_sum(out=lamw[:, 0:1], in_=lamt[:, 0, :], axis=AX.X), reads=["lamt"], writes=["lamw"])
                    fw.emit("dve", lambda e: e.reduce_sum(out=lamw[:, 1:2], in_=lamt[:, 2, :], axis=AX.X), reads=["lamt"], writes=["lamw"])
                    fw.emit("act", lambda e: e.activation(out=lamw[:, 2:4], in_=lamw[:, 0:2], func=AF.Exp), reads=["lamw"], writes=["lamw"])
                    fw.emit("dve", lambda e: e.tensor_tensor(out=lamw[:, 4:5], in0=lamw[:, 3:4], in1=lamw[:, 2:3], op=ALU.subtract),
                            reads=["lamw"], writes=["lamw"])
                    fw.emit("dve", lambda e: e.tensor_scalar(out=neglam[:], in0=lamw[:, 4:5], scalar1=float(-LAMBDA_INIT), scalar2=None, op0=ALU.add),
                            reads=["lamw"], writes=["neglam"])

                for i in range(NT):
                    b = i % 2
                    fw.emit("sp", lambda e, i=i, b=b: e.dma_start(out=xt[b][:], in_=x_d[i * 128:(i + 1) * 128, :]),
                            writes=[("xt", b)], dma="xt%d" % b)
                    fw.emit("act", lambda e, i=i, b=b: e.activation(out=junk[:], in_=xt[b][:], func=AF.Square, accum_out=ssq[:, i:i + 1]),
                            reads=[("xt", b)], writes=["junk", ("ssq", i)])
                    fw.emit("act", lambda e, i=i: e.activation(out=lnv[:, i:i + 1], in_=ssq[:, i:i + 1], func=AF.Ln, scale=1.0 / D, bias=EPS),
                            reads=[("ssq", i)], writes=[("lnv", i)])
                    fw.emit("act", lambda e, i=i: e.activation(out=rstd[:, i:i + 1], in_=lnv[:, i:i + 1], func=AF.Exp, scale=-0.5),
                            reads=[("lnv", i)], writes=[("rstd", i)])
                    fw.emit("dve", lambda e, i=i, b=b: e.tensor_scalar(out=xn[b][:], in0=xt[b][:], scalar1=rstd[:, i:i + 1], scalar2=None, op0=ALU.mult),
                            reads=[("xt", b), ("rstd", i)], writes=[("xn", b)])
                    for c in range(NCH):
                        fw.emit("pe", lambda e, b=b, c=c: e.transpose(out=tp[b][:, c * 128:(c + 1) * 128], in_=xn[b][:, c * 128:(c + 1) * 128], identity=ident),
                                reads=[("xn", b), "cb"], writes=[("tp", b)])
                    for c in range(NCH):
                        if True:
                            fw.emit("dve", lambda e, i=i, b=b, c=c: e.tensor_scalar(out=hT[:, c, i * 128:(i + 1) * 128], in0=tp[b][:, c * 128:(c + 1) * 128],
                                                                                   scalar1=gcols[:, 0, c:c + 1], scalar2=None, op0=ALU.mult),
                                    reads=[("tp", b), ("gcols", 0)], writes=[("hT", i, c)])
                        else:
                            fw.emit("act", lambda e, i=i, b=b, c=c: e.activation(out=hT[:, c, i * 128:(i + 1) * 128], in_=tp[b][:, c * 128:(c + 1) * 128],
                                                                                func=AF.Copy, scale=gcols[:, 0, c:c + 1]),
                                    reads=[("tp", b), ("gcols", 0)], writes=[("hT", i, c)])
                if stop == 0:
                    fw.emit("sp", lambda e: e.dma_start(out=dbg_o[:, :, :], in_=hT[:]), reads=[("hT", i, c) for i in range(NT) for c in range(NCH)], dma="dbgo")
                    fw.flush("p0", final_waits=[(fw.sems["dbgo"], 16)])
                    return nc
                fw.flush("p0")

            with ExitStack() as esa:
                QA = [sb(f"QA{i}", [128, S], BF16, esa) for i in range(2)]
                KA = [sb(f"KA{i}", [128, S], BF16, esa) for i in range(2)]
                V = sb("V", [128, NT, 128], BF16, esa)
                wsl = sb("wsl", [128, NCH, 3, 128], BF16, esa)
                E2 = [sb(f"E{i}", [128, 1024], BF16, esa) for i in range(2)]
                SP2 = [sb(f"SP{i}", [128, 1024], BF16, esa) for i in range(2)]
                SC = [sb(f"SC{i}", [128, 512], BF16, esa) for i in range(2)]
                W2 = [sb(f"W{i}", [128, 1024], BF16, esa) for i in range(3)]
                W = [w[:, 0:512] for w in W2]
                OS = [sb(f"OS{i}", [128, 512], F32, esa) for i in range(2)]
                SH = [sb(f"SH{i}", [128, 512], BF16, esa) for i in range(2)]
                R1 = E2[0][:].bitcast(F32)
                R2 = E2[1][:].bitcast(F32)
                ZZ = [pst(f"ZZ{i}", [128, 1024], F32, esa) for i in range(3)]
                Zv = [ZZ[j // 2][:, (j % 2) * 512:(j % 2 + 1) * 512] for j in range(6)]
                Z = Zv[0:4]
                D1 = Zv[4]
                D2 = Zv[5]
                O1 = pst("O1", [128, 512], F32, esa)
                O2 = pst("O2", [128, 512], F32, esa)
                OB = [O1, O2]
                win_v = win_d.rearrange("(c p) n -> p c n", p=128)

                fw.emit("pool", lambda e: e.memset(QA[0][64:128, :], 0.0), writes=[("QA", 0)])
                fw.emit("pool", lambda e: e.memset(QA[1][0:64, :], 0.0), writes=[("QA", 1)])

                for g in range(8):
                    is_sb = g < 4
                    if is_sb:
                        offs = (128 * g, 512 + 128 * g, 1024 + 128 * g)
                    else:
                        h = g - 4
                        offs = (1536 + 128 * h, 2048 + 128 * h, 2560 + 128 * h)
                    def load_wsl(gg):
                        o3 = (128 * gg, 512 + 128 * gg, 1024 + 128 * gg) if gg < 4 else (1536 + 128 * (gg - 4), 2048 + 128 * (gg - 4), 2560 + 128 * (gg - 4))
                        for j in range(3):
                            fw.emit("pool", lambda e, j=j, o=o3[j]: e.dma_start(out=wsl[:, :, j, :], in_=win_v[:, :, o:o + 128]),
                                    writes=[("wsl", j)], dma="wsl%d" % j)
                    if g == 0:
                        load_wsl(0)
                    if g == 4:
                        for m2 in range(2):
                            fw.emit("pool", lambda e, m2=m2: e.memset(QA[m2][64:128, :], 0.0), writes=[("QA", m2)])
                            fw.emit("pool", lambda e, m2=m2: e.memset(KA[m2][64:128, :], 0.0), writes=[("KA", m2)])
                    if not is_sb:
                        for m2 in range(2):
                            fw.emit("pool", lambda e, m2=m2, h=h: e.dma_start(out=QA[m2][64:67, :], in_=aug_d[h, 0, :, :]), writes=[("QA", m2)], dma="augq%d" % m2)
                            fw.emit("pool", lambda e, m2=m2, h=h: e.dma_start(out=KA[m2][64:67, :], in_=aug_d[h, 1, :, :]), writes=[("KA", m2)], dma="augk%d" % m2)
                    def sb_inproj_pieces(tb):
                        cols = slice(tb * 512, (tb + 1) * 512)
                        pk = ("OB", 1)
                        pieces = []

                        def mmK(c_lo, c_hi, j):
                            def f():
                                for c in range(c_lo, c_hi):
                                    fw.emit("pe", lambda e, c=c: e.matmul(O2[:, :], lhsT=wsl[:, c, j, :], rhs=hT[:, c, cols], start=(c == 0), stop=(c == NCH - 1)),
                                            reads=[("wsl", j)], writes=[pk])
                            return f

                        def evK():
                            fw.emit("dve", lambda e: e.tensor_copy(out=KA[0][:, cols], in_=O2[:, :]), reads=[pk], writes=[("KA", 0)])

                        def evQ():
                            fw.emit("dve", lambda e: e.tensor_scalar(out=QA[0][0:64, cols], in0=O2[0:64, :], scalar1=float(SCALE), scalar2=None, op0=ALU.mult),
                                    reads=[pk], writes=[("QA", 0)])
                            fw.emit("dve", lambda e: e.tensor_scalar(out=QA[1][64:128, cols], in0=O2[64:128, :], scalar1=float(SCALE), scalar2=None, op0=ALU.mult),
                                    reads=[pk], writes=[("QA", 1)])

                        def mmV(q):
                            def f():
                                tt = tb * 4 + q
                                for c in range(NCH):
                                    fw.emit("pe", lambda e, c=c: e.matmul(O2[:, q * 128:(q + 1) * 128], lhsT=hT[:, c, tt * 128:(tt + 1) * 128],
                                                                         rhs=wsl[:, c, 2, :], start=(c == 0), stop=(c == NCH - 1)),
                                            reads=[("wsl", 2)], writes=[pk])
                            return f

                        def evV():
                            fw.emit("dve", lambda e: e.tensor_copy(out=V[:, tb * 4:(tb + 1) * 4, :].rearrange("p a b -> p (a b)"), in_=O2[:, :]),
                                    reads=[pk], writes=["V"])

                        def seq(*fs):
                            def f():
                                for x in fs:
                                    x()
                            return f
                        pieces.append(mmK(0, 4, 1))
                        pieces.append(seq(mmK(4, 8, 1), evK))
                        pieces.append(mmK(0, 4, 0))
                        pieces.append(seq(mmK(4, 8, 0), evQ))
                        pieces.append(mmV(0)); pieces.append(mmV(1)); pieces.append(mmV(2))
                        pieces.append(seq(mmV(3), evV))
                        return pieces

                    if is_sb:
                        for u in sb_inproj_pieces(0):
                            u()
                    if not is_sb:
                        inb = Zv
                        ib = 0
                        ish = 0
                        for j in (1, 0):
                            for tb in range(NQB):
                                cols = slice(tb * 512, (tb + 1) * 512)
                                if is_sb:
                                    ps = inb[ib % 6]; pk = ("zb", ib % 6); ib += 1
                                    for c in range(NCH):
                                        fw.emit("pe", lambda e, ps=ps, j=j, c=c, cols=cols: e.matmul(ps[:, :], lhsT=wsl[:, c, j, :], rhs=hT[:, c, cols],
                                                                                                    start=(c == 0), stop=(c == NCH - 1)),
                                                reads=[("wsl", j)], writes=[pk])
                                    if j == 0:
                                        fw.emit("dve", lambda e, ps=ps, cols=cols: e.tensor_scalar(out=QA[0][0:64, cols], in0=ps[0:64, :], scalar1=float(SCALE),
                                                                                                  scalar2=None, op0=ALU.mult),
                                                reads=[pk], writes=[("QA", 0)])
                                        fw.emit("dve", lambda e, ps=ps, cols=cols: e.tensor_scalar(out=QA[1][64:128, cols], in0=ps[64:128, :], scalar1=float(SCALE),
                                                                                                  scalar2=None, op0=ALU.mult),
                                                reads=[pk], writes=[("QA", 1)])
                                    else:
                                        fw.emit("act", lambda e, ps=ps, cols=cols: e.activation(out=KA[0][:, cols], in_=ps[:, :], func=AF.Copy),
                                                reads=[pk], writes=[("KA", 0)])
                                else:
                                    ps = inb[ib % 6]; pk = ("zb", ib % 6); ib += 1
                                    for c in range(NCH):
                                        fw.emit("pe", lambda e, ps=ps, j=j, c=c, cols=cols: e.matmul(ps[:, :], lhsT=wsl[:, c, j, :], rhs=hT[:, c, cols],
                                                                                                    start=(c == 0), stop=(c == NCH - 1)),
                                                reads=[("wsl", j)], writes=[pk])
                                    dst = QA if j == 0 else KA
                                    dkey = "QA" if j == 0 else "KA"
                                    sh = SH[ish % 2]; shk = ("SH", ish % 2); ish += 1
                                    if j == 0:
                                        fw.emit("dve", lambda e, ps=ps, cols=cols: e.tensor_scalar(out=QA[0][0:64, cols], in0=ps[0:64, :], scalar1=float(SCALE),
                                                                                                  scalar2=None, op0=ALU.mult),
                                                reads=[pk], writes=[("QA", 0)])
                                        fw.emit("dve", lambda e, ps=ps, sh=sh: e.tensor_scalar(out=sh[64:128, :], in0=ps[64:128, :], scalar1=float(SCALE),
                                                                                              scalar2=None, op0=ALU.mult),
                                                reads=[pk], writes=[shk])
                                    else:
                                        fw.emit("act", lambda e, ps=ps, cols=cols: e.activation(out=KA[0][0:64, cols], in_=ps[0:64, :], func=AF.Copy),
                                                reads=[pk], writes=[("KA", 0)])
                                        fw.emit("act", lambda e, ps=ps, sh=sh: e.activation(out=sh[64:128, :], in_=ps[64:128, :], func=AF.Copy),
                                                reads=[pk], writes=[shk])
                                    fw.emit("sp", lambda e, dst=dst, sh=sh, cols=cols: e.dma_start(out=dst[1][0:64, cols], in_=sh[64:128, :]),
                                            reads=[shk], writes=[(dkey, 1)], dma="sh%d" % ((ish - 1) % 2))
                        for t4 in range(NT // 4):
                            ps = inb[ib % 6]; pk = ("zb", ib % 6); ib += 1
                            for q in range(4):
                                tt = t4 * 4 + q
                                for c in range(NCH):
                                    fw.emit("pe", lambda e, ps=ps, q=q, c=c, tt=tt: e.matmul(ps[:, q * 128:(q + 1) * 128], lhsT=hT[:, c, tt * 128:(tt + 1) * 128],
                                                                                            rhs=wsl[:, c, 2, :], start=(c == 0), stop=(c == NCH - 1)),
                                            reads=[("wsl", 2)], writes=[pk])
                            if t4 % 2 == 0:
                                fw.emit("dve", lambda e, ps=ps, t4=t4: e.tensor_copy(out=V[:, t4 * 4:(t4 + 1) * 4, :].rearrange("p a b -> p (a b)"), in_=ps[:, :]),
                                        reads=[pk], writes=["V"])
                            else:
                                fw.emit("act", lambda e, ps=ps, t4=t4: e.activation(out=V[:, t4 * 4:(t4 + 1) * 4, :].rearrange("p a b -> p (a b)"), in_=ps[:, :], func=AF.Copy),
                                        reads=[pk], writes=["V"])
                    if not is_sb and g + 1 < 8:
                        load_wsl(g + 1)
                    zk = [("zb", 0), ("zb", 1), ("zb", 2), ("zb", 3)]

                    if is_sb:
                        tasks = []
                        for qb in range(NQB):
                            for hh in range(2):
                                nkb = 4 * (qb + 1)
                                kbs = list(reversed(range(nkb)))
                                for n in range(nkb // 2):
                                    kA, kB = kbs[2 * n], kbs[2 * n + 1]
                                    tasks.append(dict(qb=qb, hh=hh, kA=kA, kB=kB, first=(n == 0), last=(n == nkb // 2 - 1),
                                                      lA=kA - 4 * qb, lB=kB - 4 * qb))
                        NTK = len(tasks)

                        def pairv(tile, c0):
                            if c0 == 0:
                                return tile[:, :]
                            return tile[:, :].rearrange("p (h x) -> p h x", h=2)[:, :, c0:512]

                        def sb_s1(i):
                            t = tasks[i]
                            hh = t["hh"]; q0 = 512 * t["qb"]
                            c0 = 128 * max(t["lB"], 0)
                            zz = ZZ[i % 3]
                            zkA, zkB = ("zb", 2 * (i % 3)), ("zb", 2 * (i % 3) + 1)
                            for half, kb, zkk in ((0, t["kA"], zkA), (1, t["kB"], zkB)):
                                fw.emit("pe", lambda e, half=half, kb=kb: e.matmul(zz[:, half * 512 + c0:(half + 1) * 512], lhsT=KA[0][:, kb * 128:(kb + 1) * 128],
                                                                                  rhs=QA[hh][:, q0 + c0:q0 + 512], start=True, stop=True),
                                        reads=[("KA", 0), ("QA", hh)], writes=[zkk])
                            if t["lA"] >= 0:
                                fw.emit("pe", lambda e: e.matmul(zz[:, c0:c0 + 256], lhsT=ident, rhs=cb[:, C_FULL:C_FULL + 256], start=False, stop=True, skip_group_check=True),
                                        reads=["cb"], writes=[zkA])
                            if t["lB"] >= 0:
                                fw.emit("pe", lambda e: e.matmul(zz[:, 512 + c0:512 + c0 + 128], lhsT=ident, rhs=cb[:, C_TRI:C_TRI + 128], start=False, stop=True, skip_group_check=True),
                                        reads=["cb"], writes=[zkB])
                            fw.emit("act", lambda e: e.activation(out=pairv(E2[i % 2], c0), in_=pairv(zz, c0), func=AF.Exp),
                                    reads=[zkA, zkB], writes=[("E", i % 2)])
                            fw.emit("act", lambda e: e.activation(out=pairv(SP2[i % 2], c0), in_=pairv(E2[i % 2], c0), func=AF.Ln, bias=1.0),
                                    reads=[("E", i % 2)], writes=[("SP", i % 2)])

                        def sb_s2(i):
                            t = tasks[i]
                            c0 = 128 * max(t["lB"], 0)
                            zz = ZZ[i % 3]
                            sp = SP2[i % 2]
                            zkA, zkB = ("zb", 2 * (i % 3)), ("zb", 2 * (i % 3) + 1)
                            fw.emit("pe", lambda e: e.matmul(zz[:, c0:512], lhsT=negU, rhs=sp[:, c0:512], start=False, stop=True, skip_group_check=True),
                                    reads=[("SP", i % 2), "cb"], writes=[zkA])
                            if not t["first"]:
                                fw.emit("pe", lambda e: e.matmul(zz[:, c0:512], lhsT=negones, rhs=SC[1][:, c0:512], start=False, stop=True, skip_group_check=True),
                                        reads=[("SC", 1), "cb"], writes=[zkA])
                            if t["first"]:
                                fw.emit("pool", lambda e: e.memset(SC[0][:, 0:256], 0.0), writes=[("SC", 0)])
                                fw.emit("pool", lambda e: e.memset(SC[1][:, 0:256], 0.0), writes=[("SC", 1)])
                                fw.emit("dve", lambda e: e.tensor_copy(out=SC[0][:, c0:512], in_=sp[:, c0:512]),
                                        reads=[("SP", i % 2)], writes=[("SC", 0)])
                            else:
                                fw.emit("dve", lambda e: e.tensor_tensor(out=SC[0][:, c0:512], in0=SC[1][:, c0:512], in1=sp[:, c0:512], op=ALU.add),
                                        reads=[("SP", i % 2), ("SC", 1)], writes=[("SC", 0)])
                            fw.emit("pe", lambda e: e.matmul(zz[:, 512 + c0:1024], lhsT=negU, rhs=sp[:, 512 + c0:1024], start=False, stop=True, skip_group_check=True),
                                    reads=[("SP", i % 2), "cb"], writes=[zkB])
                            fw.emit("pe", lambda e: e.matmul(zz[:, 512 + c0:1024], lhsT=negones, rhs=SC[0][:, c0:512], start=False, stop=True, skip_group_check=True),
                                    reads=[("SC", 0), "cb"], writes=[zkB])
                            if not t["last"]:
                                fw.emit("dve", lambda e: e.tensor_tensor(out=SC[1][:, c0:512], in0=SC[0][:, c0:512], in1=sp[:, 512 + c0:1024], op=ALU.add),
                                        reads=[("SP", i % 2), ("SC", 0)], writes=[("SC", 1)])
                            fw.emit("act", lambda e: e.activation(out=pairv(W2[i % 3], c0), in_=pairv(zz, c0), func=AF.Exp),
                                    reads=[zkA, zkB], writes=[("W", i % 3)])

                        def sb_s3(i):
                            t = tasks[i]
                            hh = t["hh"]; qb = t["qb"]
                            c0 = 128 * max(t["lB"], 0)
                            for half, kb in ((0, t["kA"]), (1, t["kB"])):
                                fw.emit("pe", lambda e, half=half, kb=kb: e.matmul(O1[:, c0:512], lhsT=V[:, kb, :], rhs=W2[i % 3][:, half * 512 + c0:(half + 1) * 512],
                                                                                  start=(t["first"] and half == 0), stop=(t["last"] and half == 1), skip_group_check=True),
                                        reads=[("W", i % 3), "V"], writes=[("OB", 0)])
                            if t["last"]:
                                hb = 64 * hh
                                fw.emit("dve", lambda e: e.tensor_copy(out=oT[hb:hb + 64, g, qb * 512:(qb + 1) * 512], in_=O1[hb:hb + 64, :]),
                                        reads=[("OB", 0)], writes=[("oT", g, qb, hh)])

                        pending = []
                        last_qb = -1
                        for i in range(NTK + 2):
                            if i < NTK:
                                if tasks[i]["qb"] != last_qb:
                                    last_qb = tasks[i]["qb"]
                                    assert not pending
                                    if last_qb + 1 < NQB:
                                        pending = sb_inproj_pieces(last_qb + 1)
                                sb_s1(i)
                            if 0 <= i - 1 < NTK:
                                sb_s2(i - 1)
                            if 0 <= i - 2 < NTK:
                                sb_s3(i - 2)
                            if i < NTK:
                                for _ in range(2 if tasks[i]["qb"] == 0 else 1):
                                    if pending:
                                        pending.pop(0)()
                                        if not pending and tasks[i]["qb"] == NQB - 2 and g + 1 < 8:
                                            load_wsl(g + 1)
                    else:
                        h = g - 4
                        tasks = []
                        for qb in range(NQB):
                            for c in range(2):
                                nkb = 4 * (qb + 1)
                                for kb in range(nkb):
                                    tasks.append(dict(qb=qb, c=c, kb=kb, first=(kb == 0), last=(kb == nkb - 1), kbl=kb - 4 * qb))
                        NTK = len(tasks)

                        def df_s1(i):
                            t = tasks[i]
                            m2 = t["c"]; q0 = 512 * t["qb"]; kb = t["kb"]
                            c0 = 128 * max(t["kbl"], 0)
                            z = Z[i % 4]
                            fw.emit("pe", lambda e: e.matmul(z[:, c0:512], lhsT=KA[m2][:, kb * 128:(kb + 1) * 128], rhs=QA[m2][:, q0 + c0:q0 + 512],
                                                             start=True, stop=True),
                                    reads=[("KA", m2), ("QA", m2)], writes=[zk[i % 4]])
                            if t["kbl"] >= 0:
                                fw.emit("pe", lambda e: e.matmul(z[:, c0:c0 + 128], lhsT=ident, rhs=cb[:, C_TRI2:C_TRI2 + 128], start=False, stop=True, skip_group_check=True),
                                        reads=["cb"], writes=[zk[i % 4]])
                            cblk = -SLOPES[h] * 128.0 * (4 * t["qb"] - kb)
                            fw.emit("act", lambda e: e.activation(out=W[i % 3][:, c0:512], in_=z[:, c0:512], func=AF.Exp, bias=float(cblk)),
                                    reads=[zk[i % 4]], writes=[("W", i % 3)])

                        def df_s2(i):
                            t = tasks[i]
                            kb = t["kb"]; qb = t["qb"]; m2 = t["c"]
                            c0 = 128 * max(t["kbl"], 0)
                            OO = OB[m2]
                            DD = D1 if m2 == 0 else D2
                            fw.emit("pe", lambda e: e.matmul(OO[:, c0:512], lhsT=V[:, kb, :], rhs=W[i % 3][:, c0:512], start=t["first"], stop=t["last"]),
                                    reads=[("W", i % 3), "V"], writes=[("OB", m2)])
                            fw.emit("pe", lambda e: e.matmul(DD[:, c0:512], lhsT=ones, rhs=W[i % 3][:, c0:512], start=t["first"], stop=t["last"]),
                                    reads=[("W", i % 3), "cb"], writes=[("zb", 4 + m2)])
                            if t["last"] and m2 == 1:
                                fw.emit("act", lambda e: e.activation(out=OS[0][:], in_=O1[:, :], func=AF.Copy), reads=[("OB", 0)], writes=[("OS", 0)])
                                fw.emit("dve", lambda e: e.reciprocal(out=R1, in_=D1), reads=[("zb", 4)], writes=[("E", 0)])
                                fw.emit("act", lambda e: e.activation(out=OS[1][:], in_=O2[:, :], func=AF.Copy), reads=[("OB", 1)], writes=[("OS", 1)])
                                fw.emit("dve", lambda e: e.reciprocal(out=R2, in_=D2), reads=[("zb", 5)], writes=[("E", 1)])
                                fw.emit("pool", lambda e: e.tensor_tensor(out=R1, in0=OS[0][:], in1=R1, op=ALU.mult),
                                        reads=[("OS", 0), ("E", 0)], writes=[("E", 0)])
                                fw.emit("dve", lambda e: e.scalar_tensor_tensor(out=R2, in0=R2, scalar=neglam[:, 0:1], in1=OS[1][:], op0=ALU.mult, op1=ALU.mult),
                                        reads=[("OS", 1), ("E", 1), "neglam"], writes=[("E", 1)])
                                fw.emit("pool", lambda e: e.tensor_tensor(out=oT[:, g, qb * 512:(qb + 1) * 512], in0=R1, in1=R2, op=ALU.add),
                                        reads=[("E", 0), ("E", 1)], writes=[("oT", g, qb, 0)])

                        for i in range(NTK + 2):
                            if i < NTK:
                                df_s1(i)
                            if 0 <= i - 2 < NTK:
                                df_s2(i - 2)
                if dbg:
                    fw.emit("sp", lambda e: e.dma_start(out=dbg_o[:, :, :], in_=oT[:]), reads=[("oT", g, qb, hh) for g in range(8) for qb in range(NQB) for hh in range(2)], dma="dbgo")
                if stop == 1:
                    fw.flush("pa", final_waits=[(fw.sems["dbgo"], 16)])
                    return nc
                fw.flush("pa", final_waits=[(fw.sems["dbgo"], 16)] if dbg else ())

        with ExitStack() as esb:
            X1 = sb("X1", [128, TPC, D], F32, esb)
            wmisc = sb("wmisc", [128, NCH, D], BF16, esb)
            wpp = sb("wpp", [128, 2, D], BF16, esb)
            wg = [sb(f"wg{i}", [128, NCH, DEXP], BF16, esb) for i in range(2)]
            wu = [sb(f"wu{i}", [128, NCH, DEXP], BF16, esb) for i in range(2)]
            wd = [sb(f"wd{i}", [128, 4, D], BF16, esb) for i in range(2)]
            hid = [sb("hid0", [128, 4, 512], BF16, esb)] * 2
            sg = [sb(f"sg{i}", [128, 512], BF16, esb) for i in range(2)]
            xn32 = sb("xn32", [128, D], F32, esb)
            h32 = sb("h32", [128, NCH, 128], F32, esb)
            wr32 = sb("wr32", [128, NCH, 20], F32, esb)
            brb = sb("brb", [128, 20], F32, esb)
            gfb = sb("gfb", [128, D], F32, esb)
            sq2 = sb("sq2", [128, 1024], BF16, esb)
            sq = [sq2[:, 0:512], sq2[:, 512:1024]]
            sqf = sq2[:].bitcast(F32)
            rs_t = sb("rs", [128, 512], F32, esb)
            rs = rs_t[:]
            junkb = sb("junkb", [128, D], BF16, esb)
            st = sb("stat", [128, 3, TPC, 4], F32, esb)
            lg = sb("lg", [128, TPC, 20], F32, esb)
            rt = sb("rt", [128, TPC, 64], F32, esb)
            gates = sb("gates", [128, TPC, NEXP], F32, esb)
            pl = [sb(f"pl{i}", [128, PLE], F32, esb) for i in range(2)]
            plb = sb("plb", [128, PLE], BF16, esb)
            pT = sb("pT", [128, 2, 128], BF16, esb)
            PB = [pst(f"PB{i}", [128, 512], F32, esb) for i in range(8)]

            wout_v = wout_d.rearrange("(c p) n -> p c n", p=128)
            wpg_v = wpg_d.rearrange("(c p) n -> p c n", p=128)
            wpp_v = wpp_d.rearrange("(c p) n -> p c n", p=128)

            fw.emit("sp", lambda e: e.dma_start(out=wr32[:], in_=wr_d.rearrange("(c p) n -> p c n", p=128)), writes=["wr32"], dma="wr32")
            fw.emit("sp", lambda e: e.dma_start(out=brb[:], in_=br_d.partition_broadcast(128)), writes=["brb"], dma="brb")
            fw.emit("sp", lambda e: e.dma_start(out=gfb[:], in_=gfin_d.partition_broadcast(128)), writes=["gfb"], dma="gfb")
            fw.emit("pool", lambda e: e.dma_start(out=wpp[:], in_=wpp_v), writes=["wpp"], dma="wpp")

            def load_expert(e_idx, b):
                for c in range(0, NCH, 4):
                    fw.emit("pool", lambda e, c=c: e.dma_start(out=wg[b][:, c:c + 4, :], in_=weg_d[e_idx].rearrange("(c p) n -> p c n", p=128)[:, c:c + 4, :]),
                            writes=[("wg", b, c)], dma="wg%d_%d" % (b, c))
                    fw.emit("pool", lambda e, c=c: e.dma_start(out=wu[b][:, c:c + 4, :], in_=weu_d[e_idx].rearrange("(c p) n -> p c n", p=128)[:, c:c + 4, :]),
                            writes=[("wu", b, c)], dma="wu%d_%d" % (b, c))
                for c in range(0, 4, 2):
                    fw.emit("pool", lambda e, c=c: e.dma_start(out=wd[b][:, c:c + 2, :], in_=wed_d[e_idx].rearrange("(c p) n -> p c n", p=128)[:, c:c + 2, :]),
                            writes=[("wd", b, c)], dma="wd%d_%d" % (b, c))

            def rstd_batch(kind):
                fw.emit("act", lambda e: e.activation(out=st[:, kind, :, 1], in_=st[:, kind, :, 0], func=AF.Ln, scale=1.0 / D, bias=EPS),
                        reads=[("st", kind, t) for t in range(TPC)], writes=[("stln", kind)])
                fw.emit("act", lambda e: e.activation(out=st[:, kind, :, 2], in_=st[:, kind, :, 1], func=AF.Exp, scale=-0.5),
                        reads=[("stln", kind)], writes=[("strs", kind)])

            def norm_T(kind, t, gk, tok0, want32):
                fw.emit("dve", lambda e: e.tensor_scalar(out=xn32[:], in0=X1[:, t, :], scalar1=st[:, kind, t, 2:3], scalar2=None, op0=ALU.mult),
                        reads=[("X1", t), ("strs", kind)], writes=["xn32"])
                for c in range(NCH):
                    bk = 2 + c // 4
                    fw.emit("pe", lambda e, c=c, bk=bk: e.transpose(out=PB[bk][:, (c % 4) * 128:(c % 4 + 1) * 128], in_=xn32[:, c * 128:(c + 1) * 128], identity=identf[:]),
                            reads=["xn32", "identf"], writes=[("PB", bk)])
                for c in range(NCH):
                    bk = 2 + c // 4
                    src = PB[bk][:, (c % 4) * 128:(c % 4 + 1) * 128]
                    if want32:
                        fw.emit("dve", lambda e, c=c, src=src: e.tensor_scalar(out=h32[:, c, :], in0=src, scalar1=gcols[:, gk, c:c + 1], scalar2=None, op0=ALU.mult),
                                reads=[("PB", bk), ("gcols", gk)], writes=[("h32", c)])
                    else:
                        if True:
                            fw.emit("dve", lambda e, c=c, src=src: e.tensor_scalar(out=oT[:, c, tok0:tok0 + 128], in0=src, scalar1=gcols[:, gk, c:c + 1], scalar2=None, op0=ALU.mult),
                                    reads=[("PB", bk), ("gcols", gk)], writes=[("oTt", tok0)])
                        else:
                            fw.emit("act", lambda e, c=c, src=src: e.activation(out=oT[:, c, tok0:tok0 + 128], in_=src, func=AF.Copy, scale=gcols[:, gk, c:c + 1]),
                                    reads=[("PB", bk), ("gcols", gk)], writes=[("oTt", tok0)])
                if want32:
                    fw.emit("dve", lambda e: e.tensor_copy(out=oT[:, :, tok0:tok0 + 128], in_=h32[:]),
                            reads=[("h32", c) for c in range(NCH)], writes=[("oTt", tok0)])

            def onorm(t0):
                for blk in range(CH // 512):
                    c0 = t0 + blk * 512
                    for grp in range(5):
                        chunks = [0, 1, 2, 3] if grp == 0 else [3 + grp]
                        nfeat = 512.0 if grp == 0 else 128.0
                        for n, c in enumerate(chunks):
                            sqb = sq[n % 2]
                            fw.emit("act", lambda e, c=c, sqb=sqb: e.activation(out=sqb, in_=oT[:, c, c0:c0 + 512], func=AF.Square),
                                    reads=[("oTb", c, c0)], writes=[("sq", n % 2)])
                            fw.emit("pe", lambda e, sqb=sqb, n=n: e.matmul(PB[7][:, :], lhsT=ones, rhs=sqb, start=(n == 0), stop=(n == len(chunks) - 1)),
                                    reads=[("sq", n % 2), "cb"], writes=[("PB", 7)])
                        fw.emit("act", lambda e, nfeat=nfeat: e.activation(out=rs, in_=PB[7][:, :], func=AF.Ln, scale=1.0 / nfeat, bias=EPS),
                                reads=[("PB", 7)], writes=["rs"])
                        fw.emit("act", lambda e: e.activation(out=rs, in_=rs, func=AF.Exp, scale=-0.5), reads=["rs"], writes=["rs"])
                        for c in chunks:
                            gcol = gcols[:, 3, c:c + 1] if grp == 0 else gcols[:, 4, 0:1]
                            fw.emit("dve", lambda e, c=c, gcol=gcol: e.scalar_tensor_tensor(out=oT[:, c, c0:c0 + 512], in0=oT[:, c, c0:c0 + 512], scalar=gcol, in1=rs,
                                                                                           op0=ALU.mult, op1=ALU.mult),
                                    reads=["rs", ("oTb", c, c0), ("gcols", 3), ("gcols", 4)], writes=[("oTb", c, c0)])

            for ck in range(NCK):
                t0 = ck * CH
                fw.emit("pool", lambda e: e.dma_start(out=wmisc[:], in_=wout_v), writes=["wmisc"], dma="wmisc")
                load_expert(0, 0)
                if ck == 0:
                    onorm(t0)
                for t in range(TPC):
                    tok0 = t0 + t * 128
                    c0 = t0 + (t // 4) * 512
                    b = t % 2
                    fw.emit("sp", lambda e, t=t, tok0=tok0: e.dma_start(out=X1[:, t, :], in_=x_d[tok0:tok0 + 128, :]), writes=[("X1", t)], dma="xl%d" % t)
                    for nh in range(2):
                        for c in range(NCH):
                            fw.emit("pe", lambda e, nh=nh, c=c, tok0=tok0: e.matmul(PB[nh][:, :], lhsT=oT[:, c, tok0:tok0 + 128], rhs=wmisc[:, c, nh * 512:(nh + 1) * 512],
                                                                                   start=(c == 0), stop=(c == NCH - 1)),
                                    reads=[("oTb", c, c0), "wmisc", ("oTt", tok0)], writes=[("PB", nh)])
                        fw.emit("dve", lambda e, nh=nh, t=t, b=b: e.tensor_tensor(out=X1[:, t, nh * 512:(nh + 1) * 512], in0=PB[nh][:, :], in1=X1[:, t, nh * 512:(nh + 1) * 512], op=ALU.add),
                                reads=[("PB", nh), ("X1", t)], writes=[("X1", t)])
                    fw.emit("act", lambda e, t=t: e.activation(out=junkb[:], in_=X1[:, t, :], func=AF.Square, accum_out=st[:, 0, t, 0:1]),
                            reads=[("X1", t)], writes=["junkb", ("st", 0, t)])
                    if dbg:
                        fw.emit("sp", lambda e, t=t, tok0=tok0: e.dma_start(out=dbg_x1[tok0:tok0 + 128, :], in_=X1[:, t, :]), reads=[("X1", t)], dma="dbgx%d" % t)
                rstd_batch(0)
                for t in range(TPC):
                    tok0 = t0 + t * 128
                    norm_T(0, t, 1, tok0, True)
                    for c in range(NCH):
                        fw.emit("pe", lambda e, c=c: e.matmul(PB[7][:, 0:20], lhsT=h32[:, c, :], rhs=wr32[:, c, :], start=(c == 0), stop=(c == NCH - 1)),
                                reads=[("h32", cc) for cc in range(NCH)] + ["wr32"], writes=[("PB", 7)])
                    fw.emit("dve", lambda e, t=t: e.tensor_tensor(out=lg[:, t, :], in0=PB[7][:, 0:20], in1=brb[:], op=ALU.add),
                            reads=[("PB", 7), "brb"], writes=["lg"])
                G = lg[:, :, 0:4]
                EL = lg[:, :, 4:20]
                gmax = rt[:, :, 0:1]
                goh = rt[:, :, 1:5]
                gex = rt[:, :, 5:9]
                gsum = rt[:, :, 9:10]
                gw = rt[:, :, 10:11]
                msk = rt[:, :, 16:32]
                m1 = rt[:, :, 11:12]
                m2 = rt[:, :, 12:13]
                oh1 = rt[:, :, 32:48]
                oh2 = rt[:, :, 48:64]
                dlt = rt[:, :, 13:14]
                w1 = rt[:, :, 14:15]
                w2 = rt[:, :, 15:16]
                BIG = 1.0e4

                def dv(fn, r=("lg", "rt"), w=("rt",)):
                    fw.emit("dve", fn, reads=list(r), writes=list(w))

                dv(lambda e: e.tensor_reduce(out=gmax, in_=G, axis=AX.X, op=ALU.max))
                dv(lambda e: e.tensor_tensor(out=goh, in0=G, in1=gmax.to_broadcast([128, TPC, 4]), op=ALU.is_equal))
                dv(lambda e: e.tensor_tensor(out=gex, in0=G, in1=gmax.to_broadcast([128, TPC, 4]), op=ALU.subtract))
                fw.emit("act", lambda e: e.activation(out=gex, in_=gex, func=AF.Exp), reads=["rt"], writes=["rt"])
                dv(lambda e: e.tensor_reduce(out=gsum, in_=gex, axis=AX.X, op=ALU.add))
                dv(lambda e: e.reciprocal(out=gw, in_=gsum))
                dv(lambda e: e.tensor_scalar(out=gex, in0=goh, scalar1=-1.0, scalar2=BIG, op0=ALU.add, op1=ALU.mult))
                dv(lambda e: e.tensor_tensor(out=msk.rearrange("p t (g k) -> p t g k", k=4), in0=EL.rearrange("p t (g k) -> p t g k", k=4),
                                             in1=gex.unsqueeze(3).to_broadcast([128, TPC, 4, 4]), op=ALU.add))
                dv(lambda e: e.tensor_reduce(out=m1, in_=msk, axis=AX.X, op=ALU.max))
                dv(lambda e: e.tensor_tensor(out=oh1, in0=msk, in1=m1.to_broadcast([128, TPC, 16]), op=ALU.is_equal))
                dv(lambda e: e.scalar_tensor_tensor(out=msk, in0=oh1, scalar=-BIG, in1=msk, op0=ALU.mult, op1=ALU.add))
                dv(lambda e: e.tensor_reduce(out=m2, in_=msk, axis=AX.X, op=ALU.max))
                dv(lambda e: e.tensor_tensor(out=oh2, in0=msk, in1=m2.to_broadcast([128, TPC, 16]), op=ALU.is_equal))
                dv(lambda e: e.tensor_tensor(out=dlt, in0=m2, in1=m1, op=ALU.subtract))
                fw.emit("act", lambda e: e.activation(out=dlt, in_=dlt, func=AF.Exp), reads=["rt"], writes=["rt"])
                dv(lambda e: e.tensor_scalar(out=w1, in0=dlt, scalar1=1.0, scalar2=None, op0=ALU.add))
                dv(lambda e: e.reciprocal(out=w1, in_=w1))
                dv(lambda e: e.tensor_tensor(out=w2, in0=dlt, in1=w1, op=ALU.mult))
                dv(lambda e: e.tensor_tensor(out=w1, in0=w1, in1=gw, op=ALU.mult))
                dv(lambda e: e.tensor_tensor(out=w2, in0=w2, in1=gw, op=ALU.mult))
                dv(lambda e: e.tensor_tensor(out=oh1, in0=oh1, in1=w1.to_broadcast([128, TPC, 16]), op=ALU.mult))
                dv(lambda e: e.tensor_tensor(out=oh2, in0=oh2, in1=w2.to_broadcast([128, TPC, 16]), op=ALU.mult))
                dv(lambda e: e.tensor_tensor(out=gates[:], in0=oh1, in1=oh2, op=ALU.add), w=("gates",))

                for ex in range(NEXP):
                    b = ex % 2
                    if ex + 1 < NEXP:
                        load_expert(ex + 1, (ex + 1) % 2)
                    if ex == 4 and ck + 1 < NCK:
                        onorm(t0 + CH)
                    for tb in range(CH // 512):
                        hb_ = hid[tb % 2]
                        c0 = t0 + tb * 512
                        for jc in range(4):
                            gp = PB[jc % 2]; up = PB[2 + jc % 2]
                            for c in range(NCH):
                                fw.emit("pe", lambda e, gp=gp, c=c, jc=jc: e.matmul(gp[:, :], lhsT=wg[b][:, c, jc * 128:(jc + 1) * 128], rhs=oT[:, c, c0:c0 + 512],
                                                                                   start=(c == 0), stop=(c == NCH - 1)),
                                        reads=[("wg", b, 4 * (c // 4))] + [("oTt", c0 + 128 * q) for q in range(4)], writes=[("PB", jc % 2)])
                            for c in range(NCH):
                                fw.emit("pe", lambda e, up=up, c=c, jc=jc: e.matmul(up[:, :], lhsT=wu[b][:, c, jc * 128:(jc + 1) * 128], rhs=oT[:, c, c0:c0 + 512],
                                                                                   start=(c == 0), stop=(c == NCH - 1)),
                                        reads=[("wu", b, 4 * (c // 4))] + [("oTt", c0 + 128 * q) for q in range(4)], writes=[("PB", 2 + jc % 2)])
                            fw.emit("act", lambda e, gp=gp, jc=jc: e.activation(out=sg[jc % 2][:], in_=gp[:, :], func=AF.Silu),
                                    reads=[("PB", jc % 2)], writes=[("sg", jc % 2)])
                            fw.emit("dve", lambda e, up=up, jc=jc, hb_=hb_: e.tensor_tensor(out=hb_[:, jc, :], in0=up[:, :], in1=sg[jc % 2][:], op=ALU.mult),
                                    reads=[("PB", 2 + jc % 2), ("sg", jc % 2)], writes=[("hid", tb % 2, jc)])
                        for q in range(4):
                            t = tb * 4 + q
                            for nh in range(2):
                                yp = PB[4 + (2 * q + nh) % 3]
                                yk = ("PB", 4 + (2 * q + nh) % 3)
                                for jc in range(4):
                                    fw.emit("pe", lambda e, yp=yp, jc=jc, q=q, nh=nh, hb_=hb_: e.matmul(yp[:, :], lhsT=hb_[:, jc, q * 128:(q + 1) * 128],
                                                                                                       rhs=wd[b][:, jc, nh * 512:(nh + 1) * 512], start=(jc == 0), stop=(jc == 3)),
                                            reads=[("hid", tb % 2, jc), ("wd", b, 2 * (jc // 2))], writes=[yk])
                                fw.emit("dve", lambda e, yp=yp, t=t, nh=nh: e.scalar_tensor_tensor(out=X1[:, t, nh * 512:(nh + 1) * 512], in0=yp[:, :], scalar=gates[:, t, ex:ex + 1],
                                                                                                  in1=X1[:, t, nh * 512:(nh + 1) * 512], op0=ALU.mult, op1=ALU.add),
                                        reads=[yk, "gates", ("X1", t)], writes=[("X1", t)])
                fw.emit("pool", lambda e: e.dma_start(out=wmisc[:], in_=wpg_v), writes=["wmisc"], dma="wmisc")
                for t in range(TPC):
                    fw.emit("act", lambda e, t=t: e.activation(out=junkb[:], in_=X1[:, t, :], func=AF.Square, accum_out=st[:, 1, t, 0:1]),
                            reads=[("X1", t)], writes=["junkb", ("st", 1, t)])
                rstd_batch(1)
                for t in range(TPC):
                    tok0 = t0 + t * 128
                    b = t % 2
                    norm_T(1, t, 2, tok0, False)
                    fw.emit("sp", lambda e, b=b, tok0=tok0: e.dma_start(out=pl[b][:], in_=p_d[tok0:tok0 + 128, :]), writes=[("pl", b)], dma="pl%d" % b)
                    fw.emit("pool", lambda e, b=b: e.tensor_copy(out=plb[:], in_=pl[b][:]), reads=[("pl", b)], writes=["plb"])
                    tpv = PB[6][:, 0:128].bitcast(BF16)
                    for c2 in range(2):
                        fw.emit("pe", lambda e, c2=c2, tpv=tpv: e.transpose(out=tpv[:, c2 * 128:(c2 + 1) * 128], in_=plb[:, c2 * 128:(c2 + 1) * 128], identity=ident),
                                reads=["plb", "cb"], writes=[("PB", 6)])
                    fw.emit("dve", lambda e, tpv=tpv: e.tensor_copy(out=pT[:].rearrange("p a b -> p (a b)"), in_=tpv), reads=[("PB", 6)], writes=["pT"])
                    for nh in range(2):
                        for c in range(NCH):
                            fw.emit("pe", lambda e, nh=nh, c=c, tok0=tok0: e.matmul(PB[nh][:, :], lhsT=oT[:, c, tok0:tok0 + 128], rhs=wmisc[:, c, nh * 512:(nh + 1) * 512],
                                                                                   start=(c == 0), stop=(c == NCH - 1)),
                                    reads=[("oTt", tok0), "wmisc"], writes=[("PB", nh)])
                        for c2 in range(2):
                            fw.emit("pe", lambda e, nh=nh, c2=c2: e.matmul(PB[4 + nh][:, :], lhsT=pT[:, c2, :], rhs=wpp[:, c2, nh * 512:(nh + 1) * 512],
                                                                          start=(c2 == 0), stop=(c2 == 1)),
                                    reads=["pT", "wpp"], writes=[("PB", 4 + nh)])
                        sgv = h32[:].rearrange("p a b -> p (a b)")[:, nh * 512:(nh + 1) * 512]
                        sgk = [("h32", 4 * nh + cc) for cc in range(4)]
                        fw.emit("act", lambda e, nh=nh, sgv=sgv: e.activation(out=sgv, in_=PB[nh][:, :], func=AF.Sigmoid),
                                reads=[("PB", nh)], writes=sgk)
                        tmpb = rs if nh == 0 else sqf
                        tmpk = ["rs"] if nh == 0 else [("sq", 0), ("sq", 1)]
                        fw.emit("dve", lambda e, nh=nh, tmpb=tmpb, sgv=sgv: e.tensor_tensor(out=tmpb, in0=PB[4 + nh][:, :], in1=sgv, op=ALU.mult),
                                reads=[("PB", 4 + nh)] + sgk, writes=tmpk)
                        fw.emit("pool", lambda e, nh=nh, t=t, tmpb=tmpb: e.tensor_tensor(out=X1[:, t, nh * 512:(nh + 1) * 512], in0=X1[:, t, nh * 512:(nh + 1) * 512],
                                                                                        in1=tmpb, op=ALU.add),
                                reads=tmpk + [("X1", t)], writes=[("X1", t)])
                    fw.emit("act", lambda e, t=t: e.activation(out=junkb[:], in_=X1[:, t, :], func=AF.Square, accum_out=st[:, 2, t, 0:1]),
                            reads=[("X1", t)], writes=["junkb", ("st", 2, t)])
                rstd_batch(2)
                for t in range(TPC):
                    tok0 = t0 + t * 128
                    b = t % 2
                    fw.emit("dve", lambda e, t=t, b=b: e.scalar_tensor_tensor(out=X1[:, t, :], in0=X1[:, t, :], scalar=st[:, 2, t, 2:3], in1=gfb[:], op0=ALU.mult, op1=ALU.mult),
                            reads=[("X1", t), ("strs", 2), "gfb"], writes=[("X1", t)])
                    fw.emit("sp", lambda e, t=t, tok0=tok0: e.dma_start(out=out_d[tok0:tok0 + 128, :], in_=X1[:, t, :]), reads=[("X1", t)], dma="out%d" % t)
            finals = [(fw.sems[n], fw.dma_cnt[n]) for n in fw.sems if n.startswith("out") or n.startswith("dbg")]
            fw.flush("pb", final_waits=finals)
    return nc


_NC_CACHE = {}


def _prep_inputs(inputs, S):
    f = lambda a: np.ascontiguousarray(np.asarray(a, dtype=np.float32))
    shared = {
        "cst": make_consts(),
        "aug": make_aug(S),
        "g_mix": f(inputs["g_mix"][0]),
        "w_in": f(inputs["w_in"][0]),
        "lam4": f(np.stack([inputs["lambda_q1"][0], inputs["lambda_k1"][0], inputs["lambda_q2"][0], inputs["lambda_k2"][0]], 0)),
        "g_sb_out": f(inputs["g_sb_out"][0]),
        "g_df_out": f(inputs["g_df_out"][0]),
        "w_out": f(inputs["w_out"][0]),
        "g_ffn": f(inputs["g_ffn"][0]),
        "w_router": f(np.concatenate([inputs["w_router_group"][0], inputs["w_router_expert"][0]], axis=1)),
        "b_router": f(np.concatenate([inputs["b_router_group"][0], inputs["b_router_expert"][0]], axis=0)),
        "w_expert_gate": f(inputs["w_expert_gate"][0]),
        "w_expert_up": f(inputs["w_expert_up"][0]),
        "w_expert_down": f(inputs["w_expert_down"][0]),
        "g_ple": f(inputs["g_ple"][0]),
        "w_ple_gate": f(inputs["w_ple_gate"][0]),
        "w_ple_proj": f(inputs["w_ple_proj"][0]),
        "g_final": f(inputs["g_final"]),
    }
    return shared


def kernel(**inputs):
    x = np.asarray(inputs["x"], dtype=np.float32)
    p = np.asarray(inputs["p"], dtype=np.float32)
    B, S, _ = x.shape
    if S not in _NC_CACHE:
        _NC_CACHE[S] = build(S)
    nc = _NC_CACHE[S]
    shared = _prep_inputs(inputs, S)
    in_maps = []
    for b in range(B):
        m = dict(shared)
        m["x"] = np.ascontiguousarray(x[b])
        m["p"] = np.ascontiguousarray(p[0, b])
        in_maps.append(m)
    res = run_bass_kernel_spmd(nc, in_maps, core_ids=list(range(B)))
    return np.stack([np.asarray(r["out"], dtype=np.float32) for r in res.results], axis=0)
```

```python
import math
from contextlib import ExitStack

import numpy as np
import concourse.bass as bass
import concourse.mybir as mybir
from concourse.bass_utils import run_bass_kernel_spmd

F32 = mybir.dt.float32
BF16 = mybir.dt.bfloat16
AF = mybir.ActivationFunctionType
ALU = mybir.AluOpType
AX = mybir.AxisListType

D = 1024
NCH = 8
HD = 64
NEXP = 16
DEXP = 512
PLE = 256
EPS = 1e-6
SCALE = HD ** -0.5
SLOPES = [2.0 ** (-8.0 * (h + 1) / 4) for h in range(4)]
LAMBDA_INIT = 0.8 - 0.6 * math.exp(-0.3 * 0)
SAME_ENGINE_SYNC = True

C_ID, C_NEGU, C_ONES, C_NEGONES = 0, 128, 256, 384
C_KAUG = 512
C_QAUG = 1024
C_FULL = 3072
C_TRI = 3200
C_TRI2 = 3328
CW = 3456
MASK_BIG = 30000.0


def make_consts():
    c = np.zeros((128, CW), np.float32)
    j = np.arange(128)[:, None]
    s = np.arange(128)[None, :]
    c[:, C_ID:C_ID + 128] = (j == s)
    c[:, C_NEGU:C_NEGU + 128] = -(j >= s).astype(np.float32)
    c[:, C_ONES:C_ONES + 128] = 1.0
    c[:, C_NEGONES:C_NEGONES + 128] = -1.0
    c[:, C_FULL:C_FULL + 128] = -MASK_BIG
    c[:, C_TRI:C_TRI + 128] = np.where(s <= j, -MASK_BIG, 0.0)
    c[:, C_TRI2:C_TRI2 + 128] = np.where(s < j, -MASK_BIG, 0.0)
    tl = np.arange(512)
    for h in range(4):
        sl = SLOPES[h]
        k = c[:, C_KAUG + 128 * h:C_KAUG + 128 * (h + 1)]
        k[0, :] = sl * np.arange(128)
        k[1, :] = 1.0
        k[2, :] = 1.0
        q = c[:, C_QAUG + 512 * h:C_QAUG + 512 * (h + 1)]
        q[0, :] = 1.0
        q[1, :] = -sl * (tl % 128)
        q[2, :] = -sl * 128.0 * (tl // 128)
    return c


def make_aug(S):
    a = np.zeros((4, 2, 3, S), np.float32)
    t = np.arange(S)
    for h in range(4):
        sl = SLOPES[h]
        a[h, 0, 0] = 1.0
        a[h, 0, 1] = -sl * (t % 128)
        a[h, 0, 2] = -sl * 128.0 * ((t // 128) % 4)
        a[h, 1, 0] = sl * (t % 128)
        a[h, 1, 1] = 1.0
        a[h, 1, 2] = 1.0
    return a


class _Rec:
    def __getattr__(self, name):
        def f(*a, **k):
            return (name, a, k)
        return f


class FW:
    def __init__(self, nc, es):
        self.nc = nc
        self.es = es
        self.engs = {"pe": nc.tensor, "act": nc.scalar, "dve": nc.vector, "pool": nc.gpsimd, "sp": nc.sync}
        self.prog = {e: es.enter_context(nc.semaphore("prog_" + e)) for e in self.engs}
        self.cnt = {e: 0 for e in self.engs}
        self.waited = {e: {} for e in self.engs}
        self.dma_cnt = {}
        self.sems = {}
        self.reset()

    def reset(self):
        self.ops = {e: [] for e in self.engs}
        self.lastw = {}
        self.readers = {}

    def dsem(self, name):
        if name not in self.sems:
            self.sems[name] = self.es.enter_context(self.nc.semaphore("d_" + name))
            self.dma_cnt[name] = 0
        return name

    def emit(self, eng, fn, reads=(), writes=(), dma=None):
        rec = fn(_Rec())
        deps = []
        for r in reads:
            t = self.lastw.get(r)
            if t is not None:
                deps.append(t)
        for w in writes:
            t = self.lastw.get(w)
            if t is not None:
                deps.append(t)
            deps.extend(self.readers.get(w, {}).values())
        waits = {}
        for (skey, sem, val, teng) in deps:
            if teng == eng and (eng == "pe" or not SAME_ENGINE_SYNC):
                continue
            if self.waited[eng].get(skey, -1) >= val:
                continue
            if waits.get(skey, (None, -1))[1] < val:
                waits[skey] = (sem, val)
        for skey, (sem, val) in waits.items():
            self.waited[eng][skey] = val
        if dma is not None:
            self.dsem(dma)
            self.dma_cnt[dma] += 16
            tok = ("d_" + dma, self.sems[dma], self.dma_cnt[dma], None)
            inc = (self.sems[dma], 16)
        else:
            self.cnt[eng] += 1
            tok = ("p_" + eng, self.prog[eng], self.cnt[eng], eng)
            inc = (self.prog[eng], 1)
        for w in writes:
            self.lastw[w] = tok
            self.readers[w] = {}
        for r in reads:
            self.readers.setdefault(r, {})[tok[0]] = tok
        self.ops[eng].append((list(waits.values()), rec, inc))
        return tok

    def flush(self, name, final_waits=()):
        nc = self.nc
        ops = self.ops
        with nc.Block() as block:
            def mk(ename):
                def body(e):
                    for waits, rec, inc in ops[ename]:
                        for sem, val in waits:
                            e.wait_ge(sem, val)
                        inst = getattr(e, rec[0])(*rec[1], **rec[2])
                        inst.then_inc(inc[0], inc[1])
                    if ename == "sp":
                        for sem, val in final_waits:
                            e.wait_ge(sem, val)
                return body
            block.sync(mk("sp"))
            block.tensor(mk("pe"))
            block.scalar(mk("act"))
            block.vector(mk("dve"))
            block.gpsimd(mk("pool"))
        self.reset()


def build(S, dbg=False, stop=9):
    assert S % 1024 == 0
    NT = S // 128
    NQB = S // 512
    CH = 1024
    NCK = S // CH
    TPC = CH // 128

    nc = bass.Bass("TRN2", target_bir_lowering=False)

    def din(name, shape):
        return nc.dram_tensor(name, list(shape), F32, kind="ExternalInput").ap()

    x_d = din("x", [S, D])
    p_d = din("p", [S, PLE])
    cst_d = din("cst", [128, CW])
    aug_d = din("aug", [4, 2, 3, S])
    gmix_d = din("g_mix", [D])
    win_d = din("w_in", [D, 3072])
    lam_d = din("lam4", [4, HD])
    gsb_d = din("g_sb_out", [512])
    gdf_d = din("g_df_out", [128])
    wout_d = din("w_out", [D, D])
    gffn_d = din("g_ffn", [D])
    wr_d = din("w_router", [D, 20])
    br_d = din("b_router", [20])
    weg_d = din("w_expert_gate", [NEXP, D, DEXP])
    weu_d = din("w_expert_up", [NEXP, D, DEXP])
    wed_d = din("w_expert_down", [NEXP, DEXP, D])
    gple_d = din("g_ple", [D])
    wpg_d = din("w_ple_gate", [D, D])
    wpp_d = din("w_ple_proj", [PLE, D])
    gfin_d = din("g_final", [D])
    out_d = nc.dram_tensor("out", [S, D], F32, kind="ExternalOutput").ap()
    if dbg:
        dbg_o = nc.dram_tensor("dbg_o", [128, NCH, S], BF16, kind="ExternalOutput").ap()
        dbg_x1 = nc.dram_tensor("dbg_x1", [S, D], F32, kind="ExternalOutput").ap()

    with ExitStack() as es:
        fw = FW(nc, es)

        def sb(name, shape, dt, stack=es):
            return stack.enter_context(nc.sbuf_tensor(name, list(shape), dt))

        def pst(name, shape, dt, stack):
            return stack.enter_context(nc.psum_tensor(name, list(shape), dt))

        oT = sb("oT", [128, NCH, S], BF16)
        cb = sb("cb", [128, CW], BF16)
        identf = sb("identf", [128, 128], F32)
        gcols = sb("gcols", [128, 5, NCH], F32)
        lamt = sb("lamt", [128, 4, HD], F32)
        lamw = sb("lamw", [128, 8], F32)
        neglam = sb("neglam", [128, 1], F32)

        ident = cb[:, C_ID:C_ID + 128]
        negU = cb[:, C_NEGU:C_NEGU + 128]
        ones = cb[:, C_ONES:C_ONES + 128]
        negones = cb[:, C_NEGONES:C_NEGONES + 128]

        with ExitStack() as esA:
            hT = sb("hT", [128, NCH, S], BF16, esA)
            with ExitStack() as es0:
                xt = [sb(f"xt{i}", [128, D], F32, es0) for i in range(2)]
                xn = [sb(f"xn{i}", [128, D], BF16, es0) for i in range(2)]
                junk = sb("junk0", [128, D], BF16, es0)
                ssq = sb("ssq0", [128, NT], F32, es0)
                lnv = sb("lnv0", [128, NT], F32, es0)
                rstd = sb("rstd0", [128, NT], F32, es0)
                tp = [pst(f"tp{i}", [128, D], BF16, es0) for i in range(2)]

                fw.emit("pool", lambda e: e.dma_start(out=cb[:], in_=cst_d[:, :]), writes=["cb"], dma="cst")
                fw.emit("sp", lambda e: e.dma_start(out=identf[:], in_=cst_d[:, C_ID:C_ID + 128]), writes=["identf"], dma="cst2")
                gst = sb("gst", [8, 5, 128], F32, es0)
                gps = pst("gps", [128, 5, 8], F32, es0)
                for k, (gd, nr) in enumerate([(gmix_d, 8), (gffn_d, 8), (gple_d, 8), (gsb_d, 4), (gdf_d, 1)]):
                    fw.emit("sp", lambda e, k=k, gd=gd, nr=nr: e.dma_start(out=gst[0:nr, k, :], in_=gd.rearrange("(c p) -> c p", p=128)),
                            writes=[("gst", k)], dma="gc%d" % k)
                    fw.emit("pe", lambda e, k=k, nr=nr: e.transpose(out=gps[:, k, 0:nr], in_=gst[0:nr, k, :], identity=identf[0:nr, 0:nr]),
                            reads=[("gst", k), "identf"], writes=["gps"])
                    fw.emit("dve", lambda e, k=k, nr=nr: e.tensor_copy(out=gcols[:, k, 0:nr], in_=gps[:, k, 0:nr]),
                            reads=["gps"], writes=[("gcols", k)])
                fw.emit("dve", lambda e: e.tensor_scalar(out=gcols[:, 4, 0:1], in0=gcols[:, 4, 0:1], scalar1=float(1.0 - LAMBDA_INIT),
                                                          scalar2=None, op0=ALU.mult),
                        reads=[("gcols", 4)], writes=[("gcols", 4)])
                if True:
                    fw.emit("sp", lambda e: e.dma_start(out=lamt[:].rearrange("p a b -> p (a b)"),
                                                         in_=lam_d.rearrange("a b -> (a b)").partition_broadcast(128)),
                            writes=["lamt"], dma="lam")
                    fw.emit("dve", lambda e: e.tensor_tensor(out=lamt[:, 0, :], in0=lamt[:, 0, :], in1=lamt[:, 1, :], op=ALU.mult),
                            reads=["lamt"], writes=["lamt"])
                    fw.emit("dve", lambda e: e.tensor_tensor(out=lamt[:, 2, :], in0=lamt[:, 2, :], in1=lamt[:, 3, :], op=ALU.mult),
                            reads=["lamt"], writes=["lamt"])
                    fw.emit("dve", lambda e: e.reduce_sum(out=lamw[:, 0:1], in_=lamt[:, 0, :], axis=AX.X), reads=["lamt"], writes=["lamw"])
                    fw.emit("dve", lambda e: e.reduce_sum(out=lamw[:, 1:2], in_=lamt[:, 2, :], axis=AX.X), reads=["lamt"], writes=["lamw"])
                    fw.emit("act", lambda e: e.activation(out=lamw[:, 2:4], in_=lamw[:, 0:2], func=AF.Exp), reads=["lamw"], writes=["lamw"])
                    fw.emit("dve", lambda e: e.tensor_tensor(out=lamw[:, 4:5], in0=lamw[:, 3:4], in1=lamw[:, 2:3], op=ALU.subtract),
                            reads=["lamw"], writes=["lamw"])
                    fw.emit("dve", lambda e: e.tensor_scalar(out=neglam[:], in0=lamw[:, 4:5], scalar1=float(-LAMBDA_INIT), scalar2=None, op0=ALU.add),
                            reads=["lamw"], writes=["neglam"])

                for i in range(NT):
                    b = i % 2
                    fw.emit("sp", lambda e, i=i, b=b: e.dma_start(out=xt[b][:], in_=x_d[i * 128:(i + 1) * 128, :]),
                            writes=[("xt", b)], dma="xt%d" % b)
                    fw.emit("act", lambda e, i=i, b=b: e.activation(out=junk[:], in_=xt[b][:], func=AF.Square, accum_out=ssq[:, i:i + 1]),
                            reads=[("xt", b)], writes=["junk", ("ssq", i)])
                    fw.emit("act", lambda e, i=i: e.activation(out=lnv[:, i:i + 1], in_=ssq[:, i:i + 1], func=AF.Ln, scale=1.0 / D, bias=EPS),
                            reads=[("ssq", i)], writes=[("lnv", i)])
                    fw.emit("act", lambda e, i=i: e.activation(out=rstd[:, i:i + 1], in_=lnv[:, i:i + 1], func=AF.Exp, scale=-0.5),
                            reads=[("lnv", i)], writes=[("rstd", i)])
                    fw.emit("dve", lambda e, i=i, b=b: e.tensor_scalar(out=xn[b][:], in0=xt[b][:], scalar1=rstd[:, i:i + 1], scalar2=None, op0=ALU.mult),
                            reads=[("xt", b), ("rstd", i)], writes=[("xn", b)])
                    for c in range(NCH):
                        fw.emit("pe", lambda e, b=b, c=c: e.transpose(out=tp[b][:, c * 128:(c + 1) * 128], in_=xn[b][:, c * 128:(c + 1) * 128], identity=ident),
                                reads=[("xn", b), "cb"], writes=[("tp", b)])
                    for c in range(NCH):
                        if True:
                            fw.emit("dve", lambda e, i=i, b=b, c=c: e.tensor_scalar(out=hT[:, c, i * 128:(i + 1) * 128], in0=tp[b][:, c * 128:(c + 1) * 128],
                                                                                   scalar1=gcols[:, 0, c:c + 1], scalar2=None, op0=ALU.mult),
                                    reads=[("tp", b), ("gcols", 0)], writes=[("hT", i, c)])
                        else:
                            fw.emit("act", lambda e, i=i, b=b, c=c: e.activation(out=hT[:, c, i * 128:(i + 1) * 128], in_=tp[b][:, c * 128:(c + 1) * 128],
                                                                                func=AF.Copy, scale=gcols[:, 0, c:c + 1]),
                                    reads=[("tp", b), ("gcols", 0)], writes=[("hT", i, c)])
                if stop == 0:
                    fw.emit("sp", lambda e: e.dma_start(out=dbg_o[:, :, :], in_=hT[:]), reads=[("hT", i, c) for i in range(NT) for c in range(NCH)], dma="dbgo")
                    fw.flush("p0", final_waits=[(fw.sems["dbgo"], 16)])
                    return nc
                fw.flush("p0")

            with ExitStack() as esa:
                QA = [sb(f"QA{i}", [128, S], BF16, esa) for i in range(2)]
                KA = [sb(f"KA{i}", [128, S], BF16, esa) for i in range(2)]
                V = sb("V", [128, NT, 128], BF16, esa)
                wsl = sb("wsl", [128, NCH, 3, 128], BF16, esa)
                E2 = [sb(f"E{i}", [128, 1024], BF16, esa) for i in range(2)]
                SP2 = [sb(f"SP{i}", [128, 1024], BF16, esa) for i in range(2)]
                SC = [sb(f"SC{i}", [128, 512], BF16, esa) for i in range(2)]
                W2 = [sb(f"W{i}", [128, 1024], BF16, esa) for i in range(3)]
                W = [w[:, 0:512] for w in W2]
                OS = [sb(f"OS{i}", [128, 512], F32, esa) for i in range(2)]
                SH = [sb(f"SH{i}", [128, 512], BF16, esa) for i in range(2)]
                R1 = E2[0][:].bitcast(F32)
                R2 = E2[1][:].bitcast(F32)
                ZZ = [pst(f"ZZ{i}", [128, 1024], F32, esa) for i in range(3)]
                Zv = [ZZ[j // 2][:, (j % 2) * 512:(j % 2 + 1) * 512] for j in range(6)]
                Z = Zv[0:4]
                D1 = Zv[4]
                D2 = Zv[5]
                O1 = pst("O1", [128, 512], F32, esa)
                O2 = pst("O2", [128, 512], F32, esa)
                OB = [O1, O2]
                win_v = win_d.rearrange("(c p) n -> p c n", p=128)

                fw.emit("pool", lambda e: e.memset(QA[0][64:128, :], 0.0), writes=[("QA", 0)])
                fw.emit("pool", lambda e: e.memset(QA[1][0:64, :], 0.0), writes=[("QA", 1)])

                for g in range(8):
                    is_sb = g < 4
                    if is_sb:
                        offs = (128 * g, 512 + 128 * g, 1024 + 128 * g)
                    else:
                        h = g - 4
                        offs = (1536 + 128 * h, 2048 + 128 * h, 2560 + 128 * h)
                    def load_wsl(gg):
                        o3 = (128 * gg, 512 + 128 * gg, 1024 + 128 * gg) if gg < 4 else (1536 + 128 * (gg - 4), 2048 + 128 * (gg - 4), 2560 + 128 * (gg - 4))
                        for j in range(3):
                            fw.emit("pool", lambda e, j=j, o=o3[j]: e.dma_start(out=wsl[:, :, j, :], in_=win_v[:, :, o:o + 128]),
                                    writes=[("wsl", j)], dma="wsl%d" % j)
                    if g == 0:
                        load_wsl(0)
                    if g == 4:
                        for m2 in range(2):
                            fw.emit("pool", lambda e, m2=m2: e.memset(QA[m2][64:128, :], 0.0), writes=[("QA", m2)])
                            fw.emit("pool", lambda e, m2=m2: e.memset(KA[m2][64:128, :], 0.0), writes=[("KA", m2)])
                    if not is_sb:
                        for m2 in range(2):
                            fw.emit("pool", lambda e, m2=m2, h=h: e.dma_start(out=QA[m2][64:67, :], in_=aug_d[h, 0, :, :]), writes=[("QA", m2)], dma="augq%d" % m2)
                            fw.emit("pool", lambda e, m2=m2, h=h: e.dma_start(out=KA[m2][64:67, :], in_=aug_d[h, 1, :, :]), writes=[("KA", m2)], dma="augk%d" % m2)
                    def sb_inproj_pieces(tb):
                        cols = slice(tb * 512, (tb + 1) * 512)
                        pk = ("OB", 1)
                        pieces = []

                        def mmK(c_lo, c_hi, j):
                            def f():
                                for c in range(c_lo, c_hi):
                                    fw.emit("pe", lambda e, c=c: e.matmul(O2[:, :], lhsT=wsl[:, c, j, :], rhs=hT[:, c, cols], start=(c == 0), stop=(c == NCH - 1)),
                                            reads=[("wsl", j)], writes=[pk])
                            return f

                        def evK():
                            fw.emit("dve", lambda e: e.tensor_copy(out=KA[0][:, cols], in_=O2[:, :]), reads=[pk], writes=[("KA", 0)])

                        def evQ():
                            fw.emit("dve", lambda e: e.tensor_scalar(out=QA[0][0:64, cols], in0=O2[0:64, :], scalar1=float(SCALE), scalar2=None, op0=ALU.mult),
                                    reads=[pk], writes=[("QA", 0)])
                            fw.emit("dve", lambda e: e.tensor_scalar(out=QA[1][64:128, cols], in0=O2[64:128, :], scalar1=float(SCALE), scalar2=None, op0=ALU.mult),
                                    reads=[pk], writes=[("QA", 1)])

                        def mmV(q):
                            def f():
                                tt = tb * 4 + q
                                for c in range(NCH):
                                    fw.emit("pe", lambda e, c=c: e.matmul(O2[:, q * 128:(q + 1) * 128], lhsT=hT[:, c, tt * 128:(tt + 1) * 128],
                                                                         rhs=wsl[:, c, 2, :], start=(c == 0), stop=(c == NCH - 1)),
                                            reads=[("wsl", 2)], writes=[pk])
                            return f

                        def evV():
                            fw.emit("dve", lambda e: e.tensor_copy(out=V[:, tb * 4:(tb + 1) * 4, :].rearrange("p a b -> p (a b)"), in_=O2[:, :]),
                                    reads=[pk], writes=["V"])

                        def seq(*fs):
                            def f():
                                for x in fs:
                                    x()
                            return f
                        pieces.append(mmK(0, 4, 1))
                        pieces.append(seq(mmK(4, 8, 1), evK))
                        pieces.append(mmK(0, 4, 0))
                        pieces.append(seq(mmK(4, 8, 0), evQ))
                        pieces.append(mmV(0)); pieces.append(mmV(1)); pieces.append(mmV(2))
                        pieces.append(seq(mmV(3), evV))
                        return pieces

                    if is_sb:
                        for u in sb_inproj_pieces(0):
                            u()
                    if not is_sb:
                        inb = Zv
                        ib = 0
                        ish = 0
                        for j in (1, 0):
                            for tb in range(NQB):
                                cols = slice(tb * 512, (tb + 1) * 512)
                                if is_sb:
                                    ps = inb[ib % 6]; pk = ("zb", ib % 6); ib += 1
                                    for c in range(NCH):
                                        fw.emit("pe", lambda e, ps=ps, j=j, c=c, cols=cols: e.matmul(ps[:, :], lhsT=wsl[:, c, j, :], rhs=hT[:, c, cols],
                                                                                                    start=(c == 0), stop=(c == NCH - 1)),
                                                reads=[("wsl", j)], writes=[pk])
                                    if j == 0:
                                        fw.emit("dve", lambda e, ps=ps, cols=cols: e.tensor_scalar(out=QA[0][0:64, cols], in0=ps[0:64, :], scalar1=float(SCALE),
                                                                                                  scalar2=None, op0=ALU.mult),
                                                reads=[pk], writes=[("QA", 0)])
                                        fw.emit("dve", lambda e, ps=ps, cols=cols: e.tensor_scalar(out=QA[1][64:128, cols], in0=ps[64:128, :], scalar1=float(SCALE),
                                                                                                  scalar2=None, op0=ALU.mult),
                                                reads=[pk], writes=[("QA", 1)])
                                    else:
                                        fw.emit("act", lambda e, ps=ps, cols=cols: e.activation(out=KA[0][:, cols], in_=ps[:, :], func=AF.Copy),
                                                reads=[pk], writes=[("KA", 0)])
                                else:
                                    ps = inb[ib % 6]; pk = ("zb", ib % 6); ib += 1
                                    for c in range(NCH):
                                        fw.emit("pe", lambda e, ps=ps, j=j, c=c, cols=cols: e.matmul(ps[:, :], lhsT=wsl[:, c, j, :], rhs=hT[:, c, cols],
                                                                                                    start=(c == 0), stop=(c == NCH - 1)),
                                                reads=[("wsl", j)], writes=[pk])
                                    dst = QA if j == 0 else KA
                                    dkey = "QA" if j == 0 else "KA"
                                    sh = SH[ish % 2]; shk = ("SH", ish % 2); ish += 1
                                    if j == 0:
                                        fw.emit("dve", lambda e, ps=ps, cols=cols: e.tensor_scalar(out=QA[0][0:64, cols], in0=ps[0:64, :], scalar1=float(SCALE),
                                                                                                  scalar2=None, op0=ALU.mult),
                                                reads=[pk], writes=[("QA", 0)])
                                        fw.emit("dve", lambda e, ps=ps, sh=sh: e.tensor_scalar(out=sh[64:128, :], in0=ps[64:128, :], scalar1=float(SCALE),
                                                                                              scalar2=None, op0=ALU.mult),
                                                reads=[pk], writes=[shk])
                                    else:
                                        fw.emit("act", lambda e, ps=ps, cols=cols: e.activation(out=KA[0][0:64, cols], in_=ps[0:64, :], func=AF.Copy),
                                                reads=[pk], writes=[("KA", 0)])
                                        fw.emit("act", lambda e, ps=ps, sh=sh: e.activation(out=sh[64:128, :], in_=ps[64:128, :], func=AF.Copy),
                                                reads=[pk], writes=[shk])
                                    fw.emit("sp", lambda e, dst=dst, sh=sh, cols=cols: e.dma_start(out=dst[1][0:64, cols], in_=sh[64:128, :]),
                                            reads=[shk], writes=[(dkey, 1)], dma="sh%d" % ((ish - 1) % 2))
                        for t4 in range(NT // 4):
                            ps = inb[ib % 6]; pk = ("zb", ib % 6); ib += 1
                            for q in range(4):
                                tt = t4 * 4 + q
                                for c in range(NCH):
                                    fw.emit("pe", lambda e, ps=ps, q=q, c=c, tt=tt: e.matmul(ps[:, q * 128:(q + 1) * 128], lhsT=hT[:, c, tt * 128:(tt + 1) * 128],
                                                                                            rhs=wsl[:, c, 2, :], start=(c == 0), stop=(c == NCH - 1)),
                                            reads=[("wsl", 2)], writes=[pk])
                            if t4 % 2 == 0:
                                fw.emit("dve", lambda e, ps=ps, t4=t4: e.tensor_copy(out=V[:, t4 * 4:(t4 + 1) * 4, :].rearrange("p a b -> p (a b)"), in_=ps[:, :]),
                                        reads=[pk], writes=["V"])
                            else:
                                fw.emit("act", lambda e, ps=ps, t4=t4: e.activation(out=V[:, t4 * 4:(t4 + 1) * 4, :].rearrange("p a b -> p (a b)"), in_=ps[:, :], func=AF.Copy),
                                        reads=[pk], writes=["V"])
                    if not is_sb and g + 1 < 8:
                        load_wsl(g + 1)
                    zk = [("zb", 0), ("zb", 1), ("zb", 2), ("zb", 3)]

                    if is_sb:
                        tasks = []
                        for qb in range(NQB):
                            for hh in range(2):
                                nkb = 4 * (qb + 1)
                                kbs = list(reversed(range(nkb)))
                                for n in range(nkb // 2):
                                    kA, kB = kbs[2 * n], kbs[2 * n + 1]
                                    tasks.append(dict(qb=qb, hh=hh, kA=kA, kB=kB, first=(n == 0), last=(n == nkb // 2 - 1),
                                                      lA=kA - 4 * qb, lB=kB - 4 * qb))
                        NTK = len(tasks)

                        def pairv(tile, c0):
                            if c0 == 0:
                                return tile[:, :]
                            return tile[:, :].rearrange("p (h x) -> p h x", h=2)[:, :, c0:512]

                        def sb_s1(i):
                            t = tasks[i]
                            hh = t["hh"]; q0 = 512 * t["qb"]
                            c0 = 128 * max(t["lB"], 0)
                            zz = ZZ[i % 3]
                            zkA, zkB = ("zb", 2 * (i % 3)), ("zb", 2 * (i % 3) + 1)
                            for half, kb, zkk in ((0, t["kA"], zkA), (1, t["kB"], zkB)):
                                fw.emit("pe", lambda e, half=half, kb=kb: e.matmul(zz[:, half * 512 + c0:(half + 1) * 512], lhsT=KA[0][:, kb * 128:(kb + 1) * 128],
                                                                                  rhs=QA[hh][:, q0 + c0:q0 + 512], start=True, stop=True),
                                        reads=[("KA", 0), ("QA", hh)], writes=[zkk])
                            if t["lA"] >= 0:
                                fw.emit("pe", lambda e: e.matmul(zz[:, c0:c0 + 256], lhsT=ident, rhs=cb[:, C_FULL:C_FULL + 256], start=False, stop=True, skip_group_check=True),
                                        reads=["cb"], writes=[zkA])
                            if t["lB"] >= 0:
                                fw.emit("pe", lambda e: e.matmul(zz[:, 512 + c0:512 + c0 + 128], lhsT=ident, rhs=cb[:, C_TRI:C_TRI + 128], start=False, stop=True, skip_group_check=True),
                                        reads=["cb"], writes=[zkB])
                            fw.emit("act", lambda e: e.activation(out=pairv(E2[i % 2], c0), in_=pairv(zz, c0), func=AF.Exp),
                                    reads=[zkA, zkB], writes=[("E", i % 2)])
                            fw.emit("act", lambda e: e.activation(out=pairv(SP2[i % 2], c0), in_=pairv(E2[i % 2], c0), func=AF.Ln, bias=1.0),
                                    reads=[("E", i % 2)], writes=[("SP", i % 2)])

                        def sb_s2(i):
                            t = tasks[i]
                            c0 = 128 * max(t["lB"], 0)
                            zz = ZZ[i % 3]
                            sp = SP2[i % 2]
                            zkA, zkB = ("zb", 2 * (i % 3)), ("zb", 2 * (i % 3) + 1)
                            fw.emit("pe", lambda e: e.matmul(zz[:, c0:512], lhsT=negU, rhs=sp[:, c0:512], start=False, stop=True, skip_group_check=True),
                                    reads=[("SP", i % 2), "cb"], writes=[zkA])
                            if not t["first"]:
                                fw.emit("pe", lambda e: e.matmul(zz[:, c0:512], lhsT=negones, rhs=SC[1][:, c0:512], start=False, stop=True, skip_group_check=True),
                                        reads=[("SC", 1), "cb"], writes=[zkA])
                            if t["first"]:
                                fw.emit("pool", lambda e: e.memset(SC[0][:, 0:256], 0.0), writes=[("SC", 0)])
                                fw.emit("pool", lambda e: e.memset(SC[1][:, 0:256], 0.0), writes=[("SC", 1)])
                                fw.emit("dve", lambda e: e.tensor_copy(out=SC[0][:, c0:512], in_=sp[:, c0:512]),
                                        reads=[("SP", i % 2)], writes=[("SC", 0)])
                            else:
                                fw.emit("dve", lambda e: e.tensor_tensor(out=SC[0][:, c0:512], in0=SC[1][:, c0:512], in1=sp[:, c0:512], op=ALU.add),
                                        reads=[("SP", i % 2), ("SC", 1)], writes=[("SC", 0)])
                            fw.emit("pe", lambda e: e.matmul(zz[:, 512 + c0:1024], lhsT=negU, rhs=sp[:, 512 + c0:1024], start=False, stop=True, skip_group_check=True),
                                    reads=[("SP", i % 2), "cb"], writes=[zkB])
                            fw.emit("pe", lambda e: e.matmul(zz[:, 512 + c0:1024], lhsT=negones, rhs=SC[0][:, c0:512], start=False, stop=True, skip_group_check=True),
                                    reads=[("SC", 0), "cb"], writes=[zkB])
                            if not t["last"]:
                                fw.emit("dve", lambda e: e.tensor_tensor(out=SC[1][:, c0:512], in0=SC[0][:, c0:512], in1=sp[:, 512 + c0:1024], op=ALU.add),
                                        reads=[("SP", i % 2), ("SC", 0)], writes=[("SC", 1)])
                            fw.emit("act", lambda e: e.activation(out=pairv(W2[i % 3], c0), in_=pairv(zz, c0), func=AF.Exp),
                                    reads=[zkA, zkB], writes=[("W", i % 3)])

                        def sb_s3(i):
                            t = tasks[i]
                            hh = t["hh"]; qb = t["qb"]
                            c0 = 128 * max(t["lB"], 0)
                            for half, kb in ((0, t["kA"]), (1, t["kB"])):
                                fw.emit("pe", lambda e, half=half, kb=kb: e.matmul(O1[:, c0:512], lhsT=V[:, kb, :], rhs=W2[i % 3][:, half * 512 + c0:(half + 1) * 512],
                                                                                  start=(t["first"] and half == 0), stop=(t["last"] and half == 1), skip_group_check=True),
                                        reads=[("W", i % 3), "V"], writes=[("OB", 0)])
                            if t["last"]:
                                hb = 64 * hh
                                fw.emit("dve", lambda e: e.tensor_copy(out=oT[hb:hb + 64, g, qb * 512:(qb + 1) * 512], in_=O1[hb:hb + 64, :]),
                                        reads=[("OB", 0)], writes=[("oT", g, qb, hh)])

                        pending = []
                        last_qb = -1
                        for i in range(NTK + 2):
                            if i < NTK:
                                if tasks[i]["qb"] != last_qb:
                                    last_qb = tasks[i]["qb"]
                                    assert not pending
                                    if last_qb + 1 < NQB:
                                        pending = sb_inproj_pieces(last_qb + 1)
                                sb_s1(i)
                            if 0 <= i - 1 < NTK:
                                sb_s2(i - 1)
                            if 0 <= i - 2 < NTK:
                                sb_s3(i - 2)
                            if i < NTK:
                                for _ in range(2 if tasks[i]["qb"] == 0 else 1):
                                    if pending:
                                        pending.pop(0)()
                                        if not pending and tasks[i]["qb"] == NQB - 2 and g + 1 < 8:
                                            load_wsl(g + 1)
                    else:
                        h = g - 4
                        tasks = []
                        for qb in range(NQB):
                            for c in range(2):
                                nkb = 4 * (qb + 1)
                                for kb in range(nkb):
                                    tasks.append(dict(qb=qb, c=c, kb=kb, first=(kb == 0), last=(kb == nkb - 1), kbl=kb - 4 * qb))
                        NTK = len(tasks)

                        def df_s1(i):
                            t = tasks[i]
                            m2 = t["c"]; q0 = 512 * t["qb"]; kb = t["kb"]
                            c0 = 128 * max(t["kbl"], 0)
                            z = Z[i % 4]
                            fw.emit("pe", lambda e: e.matmul(z[:, c0:512], lhsT=KA[m2][:, kb * 128:(kb + 1) * 128], rhs=QA[m2][:, q0 + c0:q0 + 512],
                                                             start=True, stop=True),
                                    reads=[("KA", m2), ("QA", m2)], writes=[zk[i % 4]])
                            if t["kbl"] >= 0:
                                fw.emit("pe", lambda e: e.matmul(z[:, c0:c0 + 128], lhsT=ident, rhs=cb[:, C_TRI2:C_TRI2 + 128], start=False, stop=True, skip_group_check=True),
                                        reads=["cb"], writes=[zk[i % 4]])
                            cblk = -SLOPES[h] * 128.0 * (4 * t["qb"] - kb)
                            fw.emit("act", lambda e: e.activation(out=W[i % 3][:, c0:512], in_=z[:, c0:512], func=AF.Exp, bias=float(cblk)),
                                    reads=[zk[i % 4]], writes=[("W", i % 3)])

                        def df_s2(i):
                            t = tasks[i]
                            kb = t["kb"]; qb = t["qb"]; m2 = t["c"]
                            c0 = 128 * max(t["kbl"], 0)
                            OO = OB[m2]
                            DD = D1 if m2 == 0 else D2
                            fw.emit("pe", lambda e: e.matmul(OO[:, c0:512], lhsT=V[:, kb, :], rhs=W[i % 3][:, c0:512], start=t["first"], stop=t["last"]),
                                    reads=[("W", i % 3), "V"], writes=[("OB", m2)])
                            fw.emit("pe", lambda e: e.matmul(DD[:, c0:512], lhsT=ones, rhs=W[i % 3][:, c0:512], start=t["first"], stop=t["last"]),
                                    reads=[("W", i % 3), "cb"], writes=[("zb", 4 + m2)])
                            if t["last"] and m2 == 1:
                                fw.emit("act", lambda e: e.activation(out=OS[0][:], in_=O1[:, :], func=AF.Copy), reads=[("OB", 0)], writes=[("OS", 0)])
                                fw.emit("dve", lambda e: e.reciprocal(out=R1, in_=D1), reads=[("zb", 4)], writes=[("E", 0)])
                                fw.emit("act", lambda e: e.activation(out=OS[1][:], in_=O2[:, :], func=AF.Copy), reads=[("OB", 1)], writes=[("OS", 1)])
                                fw.emit("dve", lambda e: e.reciprocal(out=R2, in_=D2), reads=[("zb", 5)], writes=[("E", 1)])
                                fw.emit("pool", lambda e: e.tensor_tensor(out=R1, in0=OS[0][:], in1=R1, op=ALU.mult),
                                        reads=[("OS", 0), ("E", 0)], writes=[("E", 0)])
                                fw.emit("dve", lambda e: e.scalar_tensor_tensor(out=R2, in0=R2, scalar=neglam[:, 0:1], in1=OS[1][:], op0=ALU.mult, op1=ALU.mult),
                                        reads=[("OS", 1), ("E", 1), "neglam"], writes=[("E", 1)])
                                fw.emit("pool", lambda e: e.tensor_tensor(out=oT[:, g, qb * 512:(qb + 1) * 512], in0=R1, in1=R2, op=ALU.add),
                                        reads=[("E", 0), ("E", 1)], writes=[("oT", g, qb, 0)])

                        for i in range(NTK + 2):
                            if i < NTK:
                                df_s1(i)
                            if 0 <= i - 2 < NTK:
                                df_s2(i - 2)
                if dbg:
                    fw.emit("sp", lambda e: e.dma_start(out=dbg_o[:, :, :], in_=oT[:]), reads=[("oT", g, qb, hh) for g in range(8) for qb in range(NQB) for hh in range(2)], dma="dbgo")
                if stop == 1:
                    fw.flush("pa", final_waits=[(fw.sems["dbgo"], 16)])
                    return nc
                fw.flush("pa", final_waits=[(fw.sems["dbgo"], 16)] if dbg else ())

        with ExitStack() as esb:
            X1 = sb("X1", [128, TPC, D], F32, esb)
            wmisc = sb("wmisc", [128, NCH, D], BF16, esb)
            wpp = sb("wpp", [128, 2, D], BF16, esb)
            wg = [sb(f"wg{i}", [128, NCH, DEXP], BF16, esb) for i in range(2)]
            wu = [sb(f"wu{i}", [128, NCH, DEXP], BF16, esb) for i in range(2)]
            wd = [sb(f"wd{i}", [128, 4, D], BF16, esb) for i in range(2)]
            hid = [sb("hid0", [128, 4, 512], BF16, esb)] * 2
            sg = [sb(f"sg{i}", [128, 512], BF16, esb) for i in range(2)]
            xn32 = sb("xn32", [128, D], F32, esb)
            h32 = sb("h32", [128, NCH, 128], F32, esb)
            wr32 = sb("wr32", [128, NCH, 20], F32, esb)
            brb = sb("brb", [128, 20], F32, esb)
            gfb = sb("gfb", [128, D], F32, esb)
            sq2 = sb("sq2", [128, 1024], BF16, esb)
            sq = [sq2[:, 0:512], sq2[:, 512:1024]]
            sqf = sq2[:].bitcast(F32)
            rs_t = sb("rs", [128, 512], F32, esb)
            rs = rs_t[:]
            junkb = sb("junkb", [128, D], BF16, esb)
            st = sb("stat", [128, 3, TPC, 4], F32, esb)
            lg = sb("lg", [128, TPC, 20], F32, esb)
            rt = sb("rt", [128, TPC, 64], F32, esb)
            gates = sb("gates", [128, TPC, NEXP], F32, esb)
            pl = [sb(f"pl{i}", [128, PLE], F32, esb) for i in range(2)]
            plb = sb("plb", [128, PLE], BF16, esb)
            pT = sb("pT", [128, 2, 128], BF16, esb)
            PB = [pst(f"PB{i}", [128, 512], F32, esb) for i in range(8)]

            wout_v = wout_d.rearrange("(c p) n -> p c n", p=128)
            wpg_v = wpg_d.rearrange("(c p) n -> p c n", p=128)
            wpp_v = wpp_d.rearrange("(c p) n -> p c n", p=128)

            fw.emit("sp", lambda e: e.dma_start(out=wr32[:], in_=wr_d.rearrange("(c p) n -> p c n", p=128)), writes=["wr32"], dma="wr32")
            fw.emit("sp", lambda e: e.dma_start(out=brb[:], in_=br_d.partition_broadcast(128)), writes=["brb"], dma="brb")
            fw.emit("sp", lambda e: e.dma_start(out=gfb[:], in_=gfin_d.partition_broadcast(128)), writes=["gfb"], dma="gfb")
            fw.emit("pool", lambda e: e.dma_start(out=wpp[:], in_=wpp_v), writes=["wpp"], dma="wpp")

            def load_expert(e_idx, b):
                for c in range(0, NCH, 4):
                    fw.emit("pool", lambda e, c=c: e.dma_start(out=wg[b][:, c:c + 4, :], in_=weg_d[e_idx].rearrange("(c p) n -> p c n", p=128)[:, c:c + 4, :]),
                            writes=[("wg", b, c)], dma="wg%d_%d" % (b, c))
                    fw.emit("pool", lambda e, c=c: e.dma_start(out=wu[b][:, c:c + 4, :], in_=weu_d[e_idx].rearrange("(c p) n -> p c n", p=128)[:, c:c + 4, :]),
                            writes=[("wu", b, c)], dma="wu%d_%d" % (b, c))
                for c in range(0, 4, 2):
                    fw.emit("pool", lambda e, c=c: e.dma_start(out=wd[b][:, c:c + 2, :], in_=wed_d[e_idx].rearrange("(c p) n -> p c n", p=128)[:, c:c + 2, :]),
                            writes=[("wd", b, c)], dma="wd%d_%d" % (b, c))

            def rstd_batch(kind):
                fw.emit("act", lambda e: e.activation(out=st[:, kind, :, 1], in_=st[:, kind, :, 0], func=AF.Ln, scale=1.0 / D, bias=EPS),
                        reads=[("st", kind, t) for t in range(TPC)], writes=[("stln", kind)])
                fw.emit("act", lambda e: e.activation(out=st[:, kind, :, 2], in_=st[:, kind, :, 1], func=AF.Exp, scale=-0.5),
                        reads=[("stln", kind)], writes=[("strs", kind)])

            def norm_T(kind, t, gk, tok0, want32):
                fw.emit("dve", lambda e: e.tensor_scalar(out=xn32[:], in0=X1[:, t, :], scalar1=st[:, kind, t, 2:3], scalar2=None, op0=ALU.mult),
                        reads=[("X1", t), ("strs", kind)], writes=["xn32"])
                for c in range(NCH):
                    bk = 2 + c // 4
                    fw.emit("pe", lambda e, c=c, bk=bk: e.transpose(out=PB[bk][:, (c % 4) * 128:(c % 4 + 1) * 128], in_=xn32[:, c * 128:(c + 1) * 128], identity=identf[:]),
                            reads=["xn32", "identf"], writes=[("PB", bk)])
                for c in range(NCH):
                    bk = 2 + c // 4
                    src = PB[bk][:, (c % 4) * 128:(c % 4 + 1) * 128]
                    if want32:
                        fw.emit("dve", lambda e, c=c, src=src: e.tensor_scalar(out=h32[:, c, :], in0=src, scalar1=gcols[:, gk, c:c + 1], scalar2=None, op0=ALU.mult),
                                reads=[("PB", bk), ("gcols", gk)], writes=[("h32", c)])
                    else:
                        if True:
                            fw.emit("dve", lambda e, c=c, src=src: e.tensor_scalar(out=oT[:, c, tok0:tok0 + 128], in0=src, scalar1=gcols[:, gk, c:c + 1], scalar2=None, op0=ALU.mult),
                                    reads=[("PB", bk), ("gcols", gk)], writes=[("oTt", tok0)])
                        else:
                            fw.emit("act", lambda e, c=c, src=src: e.activation(out=oT[:, c, tok0:tok0 + 128], in_=src, func=AF.Copy, scale=gcols[:, gk, c:c + 1]),
                                    reads=[("PB", bk), ("gcols", gk)], writes=[("oTt", tok0)])
                if want32:
                    fw.emit("dve", lambda e: e.tensor_copy(out=oT[:, :, tok0:tok0 + 128], in_=h32[:]),
                            reads=[("h32", c) for c in range(NCH)], writes=[("oTt", tok0)])

            def onorm(t0):
                for blk in range(CH // 512):
                    c0 = t0 + blk * 512
                    for grp in range(5):
                        chunks = [0, 1, 2, 3] if grp == 0 else [3 + grp]
                        nfeat = 512.0 if grp == 0 else 128.0
                        for n, c in enumerate(chunks):
                            sqb = sq[n % 2]
                            fw.emit("act", lambda e, c=c, sqb=sqb: e.activation(out=sqb, in_=oT[:, c, c0:c0 + 512], func=AF.Square),
                                    reads=[("oTb", c, c0)], writes=[("sq", n % 2)])
                            fw.emit("pe", lambda e, sqb=sqb, n=n: e.matmul(PB[7][:, :], lhsT=ones, rhs=sqb, start=(n == 0), stop=(n == len(chunks) - 1)),
                                    reads=[("sq", n % 2), "cb"], writes=[("PB", 7)])
                        fw.emit("act", lambda e, nfeat=nfeat: e.activation(out=rs, in_=PB[7][:, :], func=AF.Ln, scale=1.0 / nfeat, bias=EPS),
                                reads=[("PB", 7)], writes=["rs"])
                        fw.emit("act", lambda e: e.activation(out=rs, in_=rs, func=AF.Exp, scale=-0.5), reads=["rs"], writes=["rs"])
                        for c in chunks:
                            gcol = gcols[:, 3, c:c + 1] if grp == 0 else gcols[:, 4, 0:1]
                            fw.emit("dve", lambda e, c=c, gcol=gcol: e.scalar_tensor_tensor(out=oT[:, c, c0:c0 + 512], in0=oT[:, c, c0:c0 + 512], scalar=gcol, in1=rs,
                                                                                           op0=ALU.mult, op1=ALU.mult),
                                    reads=["rs", ("oTb", c, c0), ("gcols", 3), ("gcols", 4)], writes=[("oTb", c, c0)])

            for ck in range(NCK):
                t0 = ck * CH
                for hc in range(2):
                    fw.emit("pool", lambda e, hc=hc: e.dma_start(out=wmisc[:, 4 * hc:4 * hc + 4, :], in_=wout_v[:, 4 * hc:4 * hc + 4, :]),
                            writes=[("wmisc", hc)], dma="wmisc%d" % hc)
                load_expert(0, 0)
                if ck == 0:
                    onorm(t0)
                for t in range(TPC):
                    tok0 = t0 + t * 128
                    c0 = t0 + (t // 4) * 512
                    b = t % 2
                    fw.emit("sp", lambda e, t=t, tok0=tok0: e.dma_start(out=X1[:, t, :], in_=x_d[tok0:tok0 + 128, :]), writes=[("X1", t)], dma="xl%d" % t)
                    for nh in range(2):
                        bk = 2 * (t % 2) + nh
                        for c in range(NCH):
                            fw.emit("pe", lambda e, nh=nh, c=c, tok0=tok0, bk=bk: e.matmul(PB[bk][:, :], lhsT=oT[:, c, tok0:tok0 + 128], rhs=wmisc[:, c, nh * 512:(nh + 1) * 512],
                                                                                          start=(c == 0), stop=(c == NCH - 1)),
                                    reads=[("oTb", c, c0), ("wmisc", c // 4), ("oTt", tok0)], writes=[("PB", bk)])
                        fw.emit("dve", lambda e, nh=nh, t=t, b=b, bk=bk: e.tensor_tensor(out=X1[:, t, nh * 512:(nh + 1) * 512], in0=PB[bk][:, :], in1=X1[:, t, nh * 512:(nh + 1) * 512], op=ALU.add),
                                reads=[("PB", bk), ("X1", t)], writes=[("X1", t)])
                    fw.emit("act", lambda e, t=t: e.activation(out=junkb[:], in_=X1[:, t, :], func=AF.Square, accum_out=st[:, 0, t, 0:1]),
                            reads=[("X1", t)], writes=["junkb", ("st", 0, t)])
                    if dbg:
                        fw.emit("sp", lambda e, t=t, tok0=tok0: e.dma_start(out=dbg_x1[tok0:tok0 + 128, :], in_=X1[:, t, :]), reads=[("X1", t)], dma="dbgx%d" % t)
                rstd_batch(0)
                for t in range(TPC):
                    tok0 = t0 + t * 128
                    norm_T(0, t, 1, tok0, True)
                    for c in range(NCH):
                        fw.emit("pe", lambda e, c=c: e.matmul(PB[7][:, 0:20], lhsT=h32[:, c, :], rhs=wr32[:, c, :], start=(c == 0), stop=(c == NCH - 1)),
                                reads=[("h32", cc) for cc in range(NCH)] + ["wr32"], writes=[("PB", 7)])
                    fw.emit("dve", lambda e, t=t: e.tensor_tensor(out=lg[:, t, :], in0=PB[7][:, 0:20], in1=brb[:], op=ALU.add),
                            reads=[("PB", 7), "brb"], writes=["lg"])
                G = lg[:, :, 0:4]
                EL = lg[:, :, 4:20]
                gmax = rt[:, :, 0:1]
                goh = rt[:, :, 1:5]
                gex = rt[:, :, 5:9]
                gsum = rt[:, :, 9:10]
                gw = rt[:, :, 10:11]
                msk = rt[:, :, 16:32]
                m1 = rt[:, :, 11:12]
                m2 = rt[:, :, 12:13]
                oh1 = rt[:, :, 32:48]
                oh2 = rt[:, :, 48:64]
                dlt = rt[:, :, 13:14]
                w1 = rt[:, :, 14:15]
                w2 = rt[:, :, 15:16]
                BIG = 1.0e4

                def dv(fn, r=("lg", "rt"), w=("rt",)):
                    fw.emit("dve", fn, reads=list(r), writes=list(w))

                dv(lambda e: e.tensor_reduce(out=gmax, in_=G, axis=AX.X, op=ALU.max))
                dv(lambda e: e.tensor_tensor(out=goh, in0=G, in1=gmax.to_broadcast([128, TPC, 4]), op=ALU.is_equal))
                dv(lambda e: e.tensor_tensor(out=gex, in0=G, in1=gmax.to_broadcast([128, TPC, 4]), op=ALU.subtract))
                fw.emit("act", lambda e: e.activation(out=gex, in_=gex, func=AF.Exp), reads=["rt"], writes=["rt"])
                dv(lambda e: e.tensor_reduce(out=gsum, in_=gex, axis=AX.X, op=ALU.add))
                dv(lambda e: e.reciprocal(out=gw, in_=gsum))
                dv(lambda e: e.tensor_scalar(out=gex, in0=goh, scalar1=-1.0, scalar2=BIG, op0=ALU.add, op1=ALU.mult))
                dv(lambda e: e.tensor_tensor(out=msk.rearrange("p t (g k) -> p t g k", k=4), in0=EL.rearrange("p t (g k) -> p t g k", k=4),
                                             in1=gex.unsqueeze(3).to_broadcast([128, TPC, 4, 4]), op=ALU.add))
                dv(lambda e: e.tensor_reduce(out=m1, in_=msk, axis=AX.X, op=ALU.max))
                dv(lambda e: e.tensor_tensor(out=oh1, in0=msk, in1=m1.to_broadcast([128, TPC, 16]), op=ALU.is_equal))
                dv(lambda e: e.scalar_tensor_tensor(out=msk, in0=oh1, scalar=-BIG, in1=msk, op0=ALU.mult, op1=ALU.add))
                dv(lambda e: e.tensor_reduce(out=m2, in_=msk, axis=AX.X, op=ALU.max))
                dv(lambda e: e.tensor_tensor(out=oh2, in0=msk, in1=m2.to_broadcast([128, TPC, 16]), op=ALU.is_equal))
                dv(lambda e: e.tensor_tensor(out=dlt, in0=m2, in1=m1, op=ALU.subtract))
                fw.emit("act", lambda e: e.activation(out=dlt, in_=dlt, func=AF.Exp), reads=["rt"], writes=["rt"])
                dv(lambda e: e.tensor_scalar(out=w1, in0=dlt, scalar1=1.0, scalar2=None, op0=ALU.add))
                dv(lambda e: e.reciprocal(out=w1, in_=w1))
                dv(lambda e: e.tensor_tensor(out=w2, in0=dlt, in1=w1, op=ALU.mult))
                dv(lambda e: e.tensor_tensor(out=w1, in0=w1, in1=gw, op=ALU.mult))
                dv(lambda e: e.tensor_tensor(out=w2, in0=w2, in1=gw, op=ALU.mult))
                dv(lambda e: e.tensor_tensor(out=oh1, in0=oh1, in1=w1.to_broadcast([128, TPC, 16]), op=ALU.mult))
                dv(lambda e: e.tensor_tensor(out=oh2, in0=oh2, in1=w2.to_broadcast([128, TPC, 16]), op=ALU.mult))
                dv(lambda e: e.tensor_tensor(out=gates[:], in0=oh1, in1=oh2, op=ALU.add), w=("gates",))

                for ex in range(NEXP):
                    b = ex % 2
                    if ex + 1 < NEXP:
                        load_expert(ex + 1, (ex + 1) % 2)
                    if ex == 4 and ck + 1 < NCK:
                        onorm(t0 + CH)
                    for tb in range(CH // 512):
                        hb_ = hid[tb % 2]
                        c0 = t0 + tb * 512
                        for jc in range(4):
                            gp = PB[jc % 2]; up = PB[2 + jc % 2]
                            for c in range(NCH):
                                fw.emit("pe", lambda e, gp=gp, c=c, jc=jc: e.matmul(gp[:, :], lhsT=wg[b][:, c, jc * 128:(jc + 1) * 128], rhs=oT[:, c, c0:c0 + 512],
                                                                                   start=(c == 0), stop=(c == NCH - 1)),
                                        reads=[("wg", b, 4 * (c // 4))] + [("oTt", c0 + 128 * q) for q in range(4)], writes=[("PB", jc % 2)])
                            for c in range(NCH):
                                fw.emit("pe", lambda e, up=up, c=c, jc=jc: e.matmul(up[:, :], lhsT=wu[b][:, c, jc * 128:(jc + 1) * 128], rhs=oT[:, c, c0:c0 + 512],
                                                                                   start=(c == 0), stop=(c == NCH - 1)),
                                        reads=[("wu", b, 4 * (c // 4))] + [("oTt", c0 + 128 * q) for q in range(4)], writes=[("PB", 2 + jc % 2)])
                            fw.emit("act", lambda e, gp=gp, jc=jc: e.activation(out=sg[jc % 2][:], in_=gp[:, :], func=AF.Silu),
                                    reads=[("PB", jc % 2)], writes=[("sg", jc % 2)])
                            fw.emit("dve", lambda e, up=up, jc=jc, hb_=hb_: e.tensor_tensor(out=hb_[:, jc, :], in0=up[:, :], in1=sg[jc % 2][:], op=ALU.mult),
                                    reads=[("PB", 2 + jc % 2), ("sg", jc % 2)], writes=[("hid", tb % 2, jc)])
                        for q in range(4):
                            t = tb * 4 + q
                            for nh in range(2):
                                yp = PB[4 + (2 * q + nh) % 3]
                                yk = ("PB", 4 + (2 * q + nh) % 3)
                                for jc in range(4):
                                    fw.emit("pe", lambda e, yp=yp, jc=jc, q=q, nh=nh, hb_=hb_: e.matmul(yp[:, :], lhsT=hb_[:, jc, q * 128:(q + 1) * 128],
                                                                                                       rhs=wd[b][:, jc, nh * 512:(nh + 1) * 512], start=(jc == 0), stop=(jc == 3)),
                                            reads=[("hid", tb % 2, jc), ("wd", b, 2 * (jc // 2))], writes=[yk])
                                fw.emit("dve", lambda e, yp=yp, t=t, nh=nh: e.scalar_tensor_tensor(out=X1[:, t, nh * 512:(nh + 1) * 512], in0=yp[:, :], scalar=gates[:, t, ex:ex + 1],
                                                                                                  in1=X1[:, t, nh * 512:(nh + 1) * 512], op0=ALU.mult, op1=ALU.add),
                                        reads=[yk, "gates", ("X1", t)], writes=[("X1", t)])
                for hc in range(2):
                    fw.emit("pool", lambda e, hc=hc: e.dma_start(out=wmisc[:, 4 * hc:4 * hc + 4, :], in_=wpg_v[:, 4 * hc:4 * hc + 4, :]),
                            writes=[("wmisc", hc)], dma="wmisc%d" % hc)
                for t in range(TPC):
                    fw.emit("act", lambda e, t=t: e.activation(out=junkb[:], in_=X1[:, t, :], func=AF.Square, accum_out=st[:, 1, t, 0:1]),
                            reads=[("X1", t)], writes=["junkb", ("st", 1, t)])
                rstd_batch(1)
                for t in range(TPC):
                    tok0 = t0 + t * 128
                    b = t % 2
                    norm_T(1, t, 2, tok0, False)
                    fw.emit("sp", lambda e, b=b, tok0=tok0: e.dma_start(out=pl[b][:], in_=p_d[tok0:tok0 + 128, :]), writes=[("pl", b)], dma="pl%d" % b)
                    fw.emit("pool", lambda e, b=b: e.tensor_copy(out=plb[:], in_=pl[b][:]), reads=[("pl", b)], writes=["plb"])
                    tpv = PB[6][:, 0:128].bitcast(BF16)
                    for c2 in range(2):
                        fw.emit("pe", lambda e, c2=c2, tpv=tpv: e.transpose(out=tpv[:, c2 * 128:(c2 + 1) * 128], in_=plb[:, c2 * 128:(c2 + 1) * 128], identity=ident),
                                reads=["plb", "cb"], writes=[("PB", 6)])
                    fw.emit("dve", lambda e, tpv=tpv: e.tensor_copy(out=pT[:].rearrange("p a b -> p (a b)"), in_=tpv), reads=[("PB", 6)], writes=["pT"])
                    for nh in range(2):
                        for c in range(NCH):
                            fw.emit("pe", lambda e, nh=nh, c=c, tok0=tok0: e.matmul(PB[nh][:, :], lhsT=oT[:, c, tok0:tok0 + 128], rhs=wmisc[:, c, nh * 512:(nh + 1) * 512],
                                                                                   start=(c == 0), stop=(c == NCH - 1)),
                                    reads=[("oTt", tok0), ("wmisc", c // 4)], writes=[("PB", nh)])
                        for c2 in range(2):
                            fw.emit("pe", lambda e, nh=nh, c2=c2: e.matmul(PB[4 + nh][:, :], lhsT=pT[:, c2, :], rhs=wpp[:, c2, nh * 512:(nh + 1) * 512],
                                                                          start=(c2 == 0), stop=(c2 == 1)),
                                    reads=["pT", "wpp"], writes=[("PB", 4 + nh)])
                        sgv = h32[:].rearrange("p a b -> p (a b)")[:, nh * 512:(nh + 1) * 512]
                        sgk = [("h32", 4 * nh + cc) for cc in range(4)]
                        fw.emit("act", lambda e, nh=nh, sgv=sgv: e.activation(out=sgv, in_=PB[nh][:, :], func=AF.Sigmoid),
                                reads=[("PB", nh)], writes=sgk)
                        tmpb = rs if nh == 0 else sqf
                        tmpk = ["rs"] if nh == 0 else [("sq", 0), ("sq", 1)]
                        fw.emit("dve", lambda e, nh=nh, tmpb=tmpb, sgv=sgv: e.tensor_tensor(out=tmpb, in0=PB[4 + nh][:, :], in1=sgv, op=ALU.mult),
                                reads=[("PB", 4 + nh)] + sgk, writes=tmpk)
                        fw.emit("pool", lambda e, nh=nh, t=t, tmpb=tmpb: e.tensor_tensor(out=X1[:, t, nh * 512:(nh + 1) * 512], in0=X1[:, t, nh * 512:(nh + 1) * 512],
                                                                                        in1=tmpb, op=ALU.add),
                                reads=tmpk + [("X1", t)], writes=[("X1", t)])
                    fw.emit("act", lambda e, t=t: e.activation(out=junkb[:], in_=X1[:, t, :], func=AF.Square, accum_out=st[:, 2, t, 0:1]),
                            reads=[("X1", t)], writes=["junkb", ("st", 2, t)])
                rstd_batch(2)
                for t in range(TPC):
                    tok0 = t0 + t * 128
                    b = t % 2
                    fw.emit("dve", lambda e, t=t, b=b: e.scalar_tensor_tensor(out=X1[:, t, :], in0=X1[:, t, :], scalar=st[:, 2, t, 2:3], in1=gfb[:], op0=ALU.mult, op1=ALU.mult),
                            reads=[("X1", t), ("strs", 2), "gfb"], writes=[("X1", t)])
                    fw.emit("sp", lambda e, t=t, tok0=tok0: e.dma_start(out=out_d[tok0:tok0 + 128, :], in_=X1[:, t, :]), reads=[("X1", t)], dma="out%d" % t)
            finals = [(fw.sems[n], fw.dma_cnt[n]) for n in fw.sems if n.startswith("out") or n.startswith("dbg")]
            fw.flush("pb", final_waits=finals)
    return nc


_NC_CACHE = {}


def _prep_inputs(inputs, S):
    f = lambda a: np.ascontiguousarray(np.asarray(a, dtype=np.float32))
    shared = {
        "cst": make_consts(),
        "aug": make_aug(S),
        "g_mix": f(inputs["g_mix"][0]),
        "w_in": f(inputs["w_in"][0]),
        "lam4": f(np.stack([inputs["lambda_q1"][0], inputs["lambda_k1"][0], inputs["lambda_q2"][0], inputs["lambda_k2"][0]], 0)),
        "g_sb_out": f(inputs["g_sb_out"][0]),
        "g_df_out": f(inputs["g_df_out"][0]),
        "w_out": f(inputs["w_out"][0]),
        "g_ffn": f(inputs["g_ffn"][0]),
        "w_router": f(np.concatenate([inputs["w_router_group"][0], inputs["w_router_expert"][0]], axis=1)),
        "b_router": f(np.concatenate([inputs["b_router_group"][0], inputs["b_router_expert"][0]], axis=0)),
        "w_expert_gate": f(inputs["w_expert_gate"][0]),
        "w_expert_up": f(inputs["w_expert_up"][0]),
        "w_expert_down": f(inputs["w_expert_down"][0]),
        "g_ple": f(inputs["g_ple"][0]),
        "w_ple_gate": f(inputs["w_ple_gate"][0]),
        "w_ple_proj": f(inputs["w_ple_proj"][0]),
        "g_final": f(inputs["g_final"]),
    }
    return shared


def kernel(**inputs):
    x = np.asarray(inputs["x"], dtype=np.float32)
    p = np.asarray(inputs["p"], dtype=np.float32)
    B, S, _ = x.shape
    if S not in _NC_CACHE:
        _NC_CACHE[S] = build(S)
    nc = _NC_CACHE[S]
    shared = _prep_inputs(inputs, S)
    in_maps = []
    for b in range(B):
        m = dict(shared)
        m["x"] = np.ascontiguousarray(x[b])
        m["p"] = np.ascontiguousarray(p[0, b])
        in_maps.append(m)
    res = run_bass_kernel_spmd(nc, in_maps, core_ids=list(range(B)))
    return np.stack([np.asarray(r["out"], dtype=np.float32) for r in res.results], axis=0)
```

```python
import math
from contextlib import ExitStack

import numpy as np
import concourse.bass as bass
import concourse.mybir as mybir
from concourse.bass_utils import run_bass_kernel_spmd

F32 = mybir.dt.float32
BF16 = mybir.dt.bfloat16
AF = mybir.ActivationFunctionType
ALU = mybir.AluOpType
AX = mybir.AxisListType

D = 1024
NCH = 8
HD = 64
NEXP = 16
DEXP = 512
PLE = 256
EPS = 1e-6
SCALE = HD ** -0.5
SLOPES = [2.0 ** (-8.0 * (h + 1) / 4) for h in range(4)]
LAMBDA_INIT = 0.8 - 0.6 * math.exp(-0.3 * 0)
SAME_ENGINE_SYNC = True

C_ID, C_NEGU, C_ONES, C_NEGONES = 0, 128, 256, 384
C_KAUG = 512
C_QAUG = 1024
C_FULL = 3072
C_TRI = 3200
C_TRI2 = 3328
CW = 3456
MASK_BIG = 30000.0


def make_consts():
    c = np.zeros((128, CW), np.float32)
    j = np.arange(128)[:, None]
    s = np.arange(128)[None, :]
    c[:, C_ID:C_ID + 128] = (j == s)
    c[:, C_NEGU:C_NEGU + 128] = -(j >= s).astype(np.float32)
    c[:, C_ONES:C_ONES + 128] = 1.0
    c[:, C_NEGONES:C_NEGONES + 128] = -1.0
    c[:, C_FULL:C_FULL + 128] = -MASK_BIG
    c[:, C_TRI:C_TRI + 128] = np.where(s <= j, -MASK_BIG, 0.0)
    c[:, C_TRI2:C_TRI2 + 128] = np.where(s < j, -MASK_BIG, 0.0)
    tl = np.arange(512)
    for h in range(4):
        sl = SLOPES[h]
        k = c[:, C_KAUG + 128 * h:C_KAUG + 128 * (h + 1)]
        k[0, :] = sl * np.arange(128)
        k[1, :] = 1.0
        k[2, :] = 1.0
        q = c[:, C_QAUG + 512 * h:C_QAUG + 512 * (h + 1)]
        q[0, :] = 1.0
        q[1, :] = -sl * (tl % 128)
        q[2, :] = -sl * 128.0 * (tl // 128)
    return c


def make_aug(S):
    a = np.zeros((4, 2, 3, S), np.float32)
    t = np.arange(S)
    for h in range(4):
        sl = SLOPES[h]
        a[h, 0, 0] = 1.0
        a[h, 0, 1] = -sl * (t % 128)
        a[h, 0, 2] = -sl * 128.0 * ((t // 128) % 4)
        a[h, 1, 0] = sl * (t % 128)
        a[h, 1, 1] = 1.0
        a[h, 1, 2] = 1.0
    return a


class _Rec:
    def __getattr__(self, name):
        def f(*a, **k):
            return (name, a, k)
        return f


class FW:
    def __init__(self, nc, es):
        self.nc = nc
        self.es = es
        self.engs = {"pe": nc.tensor, "act": nc.scalar, "dve": nc.vector, "pool": nc.gpsimd, "sp": nc.sync}
        self.prog = {e: es.enter_context(nc.semaphore("prog_" + e)) for e in self.engs}
        self.cnt = {e: 0 for e in self.engs}
        self.waited = {e: {} for e in self.engs}
        self.dma_cnt = {}
        self.sems = {}
        self.reset()

    def reset(self):
        self.ops = {e: [] for e in self.engs}
        self.lastw = {}
        self.readers = {}

    def dsem(self, name):
        if name not in self.sems:
            self.sems[name] = self.es.enter_context(self.nc.semaphore("d_" + name))
            self.dma_cnt[name] = 0
        return name

    def emit(self, eng, fn, reads=(), writes=(), dma=None):
        rec = fn(_Rec())
        deps = []
        for r in reads:
            t = self.lastw.get(r)
            if t is not None:
                deps.append(t)
        for w in writes:
            t = self.lastw.get(w)
            if t is not None:
                deps.append(t)
            deps.extend(self.readers.get(w, {}).values())
        waits = {}
        for (skey, sem, val, teng) in deps:
            if teng == eng and (eng == "pe" or not SAME_ENGINE_SYNC):
                continue
            if self.waited[eng].get(skey, -1) >= val:
                continue
            if waits.get(skey, (None, -1))[1] < val:
                waits[skey] = (sem, val)
        for skey, (sem, val) in waits.items():
            self.waited[eng][skey] = val
        if dma is not None:
            self.dsem(dma)
            self.dma_cnt[dma] += 16
            tok = ("d_" + dma, self.sems[dma], self.dma_cnt[dma], None)
            inc = (self.sems[dma], 16)
        else:
            self.cnt[eng] += 1
            tok = ("p_" + eng, self.prog[eng], self.cnt[eng], eng)
            inc = (self.prog[eng], 1)
        for w in writes:
            self.lastw[w] = tok
            self.readers[w] = {}
        for r in reads:
            self.readers.setdefault(r, {})[tok[0]] = tok
        self.ops[eng].append((list(waits.values()), rec, inc))
        return tok

    def flush(self, name, final_waits=()):
        nc = self.nc
        ops = self.ops
        with nc.Block() as block:
            def mk(ename):
                def body(e):
                    for waits, rec, inc in ops[ename]:
                        for sem, val in waits:
                            e.wait_ge(sem, val)
                        inst = getattr(e, rec[0])(*rec[1], **rec[2])
                        inst.then_inc(inc[0], inc[1])
                    if ename == "sp":
                        for sem, val in final_waits:
                            e.wait_ge(sem, val)
                return body
            block.sync(mk("sp"))
            block.tensor(mk("pe"))
            block.scalar(mk("act"))
            block.vector(mk("dve"))
            block.gpsimd(mk("pool"))
        self.reset()


def build(S, dbg=False, stop=9):
    assert S % 1024 == 0
    NT = S // 128
    NQB = S // 512
    CH = 1024
    NCK = S // CH
    TPC = CH // 128

    nc = bass.Bass("TRN2", target_bir_lowering=False)

    def din(name, shape):
        return nc.dram_tensor(name, list(shape), F32, kind="ExternalInput").ap()

    x_d = din("x", [S, D])
    p_d = din("p", [S, PLE])
    cst_d = din("cst", [128, CW])
    aug_d = din("aug", [4, 2, 3, S])
    gmix_d = din("g_mix", [D])
    win_d = din("w_in", [D, 3072])
    lam_d = din("lam4", [4, HD])
    gsb_d = din("g_sb_out", [512])
    gdf_d = din("g_df_out", [128])
    wout_d = din("w_out", [D, D])
    gffn_d = din("g_ffn", [D])
    wr_d = din("w_router", [D, 20])
    br_d = din("b_router", [20])
    weg_d = din("w_expert_gate", [NEXP, D, DEXP])
    weu_d = din("w_expert_up", [NEXP, D, DEXP])
    wed_d = din("w_expert_down", [NEXP, DEXP, D])
    gple_d = din("g_ple", [D])
    wpg_d = din("w_ple_gate", [D, D])
    wpp_d = din("w_ple_proj", [PLE, D])
    gfin_d = din("g_final", [D])
    out_d = nc.dram_tensor("out", [S, D], F32, kind="ExternalOutput").ap()
    if dbg:
        dbg_o = nc.dram_tensor("dbg_o", [128, NCH, S], BF16, kind="ExternalOutput").ap()
        dbg_x1 = nc.dram_tensor("dbg_x1", [S, D], F32, kind="ExternalOutput").ap()

    with ExitStack() as es:
        fw = FW(nc, es)

        def sb(name, shape, dt, stack=es):
            return stack.enter_context(nc.sbuf_tensor(name, list(shape), dt))

        def pst(name, shape, dt, stack):
            return stack.enter_context(nc.psum_tensor(name, list(shape), dt))

        oT = sb("oT", [128, NCH, S], BF16)
        cb = sb("cb", [128, CW], BF16)
        identf = sb("identf", [128, 128], F32)
        gcols = sb("gcols", [128, 5, NCH], F32)
        lamt = sb("lamt", [128, 4, HD], F32)
        lamw = sb("lamw", [128, 8], F32)
        neglam = sb("neglam", [128, 1], F32)

        ident = cb[:, C_ID:C_ID + 128]
        negU = cb[:, C_NEGU:C_NEGU + 128]
        ones = cb[:, C_ONES:C_ONES + 128]
        negones = cb[:, C_NEGONES:C_NEGONES + 128]

        with ExitStack() as esA:
            hT = sb("hT", [128, NCH, S], BF16, esA)
            with ExitStack() as es0:
                xt = [sb(f"xt{i}", [128, D], F32, es0) for i in range(2)]
                xn = [sb(f"xn{i}", [128, D], BF16, es0) for i in range(2)]
                junk = sb("junk0", [128, D], BF16, es0)
                ssq = sb("ssq0", [128, NT], F32, es0)
                lnv = sb("lnv0", [128, NT], F32, es0)
                rstd = sb("rstd0", [128, NT], F32, es0)
                tp = [pst(f"tp{i}", [128, D], BF16, es0) for i in range(2)]

                fw.emit("pool", lambda e: e.dma_start(out=cb[:], in_=cst_d[:, :]), writes=["cb"], dma="cst")
                fw.emit("sp", lambda e: e.dma_start(out=identf[:], in_=cst_d[:, C_ID:C_ID + 128]), writes=["identf"], dma="cst2")
                gst = sb("gst", [8, 5, 128], F32, es0)
                gps = pst("gps", [128, 5, 8], F32, es0)
                for k, (gd, nr) in enumerate([(gmix_d, 8), (gffn_d, 8), (gple_d, 8), (gsb_d, 4), (gdf_d, 1)]):
                    fw.emit("sp", lambda e, k=k, gd=gd, nr=nr: e.dma_start(out=gst[0:nr, k, :], in_=gd.rearrange("(c p) -> c p", p=128)),
                            writes=[("gst", k)], dma="gc%d" % k)
                    fw.emit("pe", lambda e, k=k, nr=nr: e.transpose(out=gps[:, k, 0:nr], in_=gst[0:nr, k, :], identity=identf[0:nr, 0:nr]),
                            reads=[("gst", k), "identf"], writes=["gps"])
                    fw.emit("dve", lambda e, k=k, nr=nr: e.tensor_copy(out=gcols[:, k, 0:nr], in_=gps[:, k, 0:nr]),
                            reads=["gps"], writes=[("gcols", k)])
                fw.emit("dve", lambda e: e.tensor_scalar(out=gcols[:, 4, 0:1], in0=gcols[:, 4, 0:1], scalar1=float(1.0 - LAMBDA_INIT),
                                                          scalar2=None, op0=ALU.mult),
                        reads=[("gcols", 4)], writes=[("gcols", 4)])
                if True:
                    fw.emit("sp", lambda e: e.dma_start(out=lamt[:].rearrange("p a b -> p (a b)"),
                                                         in_=lam_d.rearrange("a b -> (a b)").partition_broadcast(128)),
                            writes=["lamt"], dma="lam")
                    fw.emit("dve", lambda e: e.tensor_tensor(out=lamt[:, 0, :], in0=lamt[:, 0, :], in1=lamt[:, 1, :], op=ALU.mult),
                            reads=["lamt"], writes=["lamt"])
                    fw.emit("dve", lambda e: e.tensor_tensor(out=lamt[:, 2, :], in0=lamt[:, 2, :], in1=lamt[:, 3, :], op=ALU.mult),
                            reads=["lamt"], writes=["lamt"])
                    fw.emit("dve", lambda e: e.reduce_sum(out=lamw[:, 0:1], in_=lamt[:, 0, :], axis=AX.X), reads=["lamt"], writes=["lamw"])
                    fw.emit("dve", lambda e: e.reduce_sum(out=lamw[:, 1:2], in_=lamt[:, 2, :], axis=AX.X), reads=["lamt"], writes=["lamw"])
                    fw.emit("act", lambda e: e.activation(out=lamw[:, 2:4], in_=lamw[:, 0:2], func=AF.Exp), reads=["lamw"], writes=["lamw"])
                    fw.emit("dve", lambda e: e.tensor_tensor(out=lamw[:, 4:5], in0=lamw[:, 3:4], in1=lamw[:, 2:3], op=ALU.subtract),
                            reads=["lamw"], writes=["lamw"])
                    fw.emit("dve", lambda e: e.tensor_scalar(out=neglam[:], in0=lamw[:, 4:5], scalar1=float(-LAMBDA_INIT), scalar2=None, op0=ALU.add),
                            reads=["lamw"], writes=["neglam"])

                for i in range(NT):
                    b = i % 2
                    fw.emit("sp", lambda e, i=i, b=b: e.dma_start(out=xt[b][:], in_=x_d[i * 128:(i + 1) * 128, :]),
                            writes=[("xt", b)], dma="xt%d" % b)
                    fw.emit("act", lambda e, i=i, b=b: e.activation(out=junk[:], in_=xt[b][:], func=AF.Square, accum_out=ssq[:, i:i + 1]),
                            reads=[("xt", b)], writes=["junk", ("ssq", i)])
                    fw.emit("act", lambda e, i=i: e.activation(out=lnv[:, i:i + 1], in_=ssq[:, i:i + 1], func=AF.Ln, scale=1.0 / D, bias=EPS),
                            reads=[("ssq", i)], writes=[("lnv", i)])
                    fw.emit("act", lambda e, i=i: e.activation(out=rstd[:, i:i + 1], in_=lnv[:, i:i + 1], func=AF.Exp, scale=-0.5),
                            reads=[("lnv", i)], writes=[("rstd", i)])
                    fw.emit("dve", lambda e, i=i, b=b: e.tensor_scalar(out=xn[b][:], in0=xt[b][:], scalar1=rstd[:, i:i + 1], scalar2=None, op0=ALU.mult),
                            reads=[("xt", b), ("rstd", i)], writes=[("xn", b)])
                    for c in range(NCH):
                        fw.emit("pe", lambda e, b=b, c=c: e.transpose(out=tp[b][:, c * 128:(c + 1) * 128], in_=xn[b][:, c * 128:(c + 1) * 128], identity=ident),
                                reads=[("xn", b), "cb"], writes=[("tp", b)])
                    for c in range(NCH):
                        if True:
                            fw.emit("dve", lambda e, i=i, b=b, c=c: e.tensor_scalar(out=hT[:, c, i * 128:(i + 1) * 128], in0=tp[b][:, c * 128:(c + 1) * 128],
                                                                                   scalar1=gcols[:, 0, c:c + 1], scalar2=None, op0=ALU.mult),
                                    reads=[("tp", b), ("gcols", 0)], writes=[("hT", i, c)])
                        else:
                            fw.emit("act", lambda e, i=i, b=b, c=c: e.activation(out=hT[:, c, i * 128:(i + 1) * 128], in_=tp[b][:, c * 128:(c + 1) * 128],
                                                                                func=AF.Copy, scale=gcols[:, 0, c:c + 1]),
                                    reads=[("tp", b), ("gcols", 0)], writes=[("hT", i, c)])
                if stop == 0:
                    fw.emit("sp", lambda e: e.dma_start(out=dbg_o[:, :, :], in_=hT[:]), reads=[("hT", i, c) for i in range(NT) for c in range(NCH)], dma="dbgo")
                    fw.flush("p0", final_waits=[(fw.sems["dbgo"], 16)])
                    return nc
                fw.flush("p0")

            with ExitStack() as esa:
                QA = [sb(f"QA{i}", [128, S], BF16, esa) for i in range(2)]
                KA = [sb(f"KA{i}", [128, S], BF16, esa) for i in range(2)]
                V = sb("V", [128, NT, 128], BF16, esa)
                wsl = sb("wsl", [128, NCH, 3, 128], BF16, esa)
                E2 = [sb(f"E{i}", [128, 1024], BF16, esa) for i in range(2)]
                SP2 = [sb(f"SP{i}", [128, 1024], BF16, esa) for i in range(2)]
                SC = [sb(f"SC{i}", [128, 512], BF16, esa) for i in range(2)]
                W2 = [sb(f"W{i}", [128, 1024], BF16, esa) for i in range(3)]
                W = [w[:, 0:512] for w in W2]
                OS = [sb(f"OS{i}", [128, 512], F32, esa) for i in range(2)]
                SH = [sb(f"SH{i}", [128, 512], BF16, esa) for i in range(2)]
                R1 = E2[0][:].bitcast(F32)
                R2 = E2[1][:].bitcast(F32)
                ZZ = [pst(f"ZZ{i}", [128, 1024], F32, esa) for i in range(3)]
                Zv = [ZZ[j // 2][:, (j % 2) * 512:(j % 2 + 1) * 512] for j in range(6)]
                Z = Zv[0:4]
                D1 = Zv[4]
                D2 = Zv[5]
                O1 = pst("O1", [128, 512], F32, esa)
                O2 = pst("O2", [128, 512], F32, esa)
                OB = [O1, O2]
                win_v = win_d.rearrange("(c p) n -> p c n", p=128)

                fw.emit("pool", lambda e: e.memset(QA[0][64:128, :], 0.0), writes=[("QA", 0)])
                fw.emit("pool", lambda e: e.memset(QA[1][0:64, :], 0.0), writes=[("QA", 1)])

                for g in range(8):
                    is_sb = g < 4
                    if is_sb:
                        offs = (128 * g, 512 + 128 * g, 1024 + 128 * g)
                    else:
                        h = g - 4
                        offs = (1536 + 128 * h, 2048 + 128 * h, 2560 + 128 * h)
                    def load_wsl(gg):
                        o3 = (128 * gg, 512 + 128 * gg, 1024 + 128 * gg) if gg < 4 else (1536 + 128 * (gg - 4), 2048 + 128 * (gg - 4), 2560 + 128 * (gg - 4))
                        for j in range(3):
                            fw.emit("pool", lambda e, j=j, o=o3[j]: e.dma_start(out=wsl[:, :, j, :], in_=win_v[:, :, o:o + 128]),
                                    writes=[("wsl", j)], dma="wsl%d" % j)
                    if g == 0:
                        load_wsl(0)
                    if g == 4:
                        for m2 in range(2):
                            fw.emit("pool", lambda e, m2=m2: e.memset(QA[m2][64:128, :], 0.0), writes=[("QA", m2)])
                            fw.emit("pool", lambda e, m2=m2: e.memset(KA[m2][64:128, :], 0.0), writes=[("KA", m2)])
                    if not is_sb:
                        for m2 in range(2):
                            fw.emit("pool", lambda e, m2=m2, h=h: e.dma_start(out=QA[m2][64:67, :], in_=aug_d[h, 0, :, :]), writes=[("QA", m2)], dma="augq%d" % m2)
                            fw.emit("pool", lambda e, m2=m2, h=h: e.dma_start(out=KA[m2][64:67, :], in_=aug_d[h, 1, :, :]), writes=[("KA", m2)], dma="augk%d" % m2)
                    def sb_inproj_pieces(tb):
                        cols = slice(tb * 512, (tb + 1) * 512)
                        pk = ("OB", 1)
                        pieces = []

                        def mmK(c_lo, c_hi, j):
                            def f():
                                for c in range(c_lo, c_hi):
                                    fw.emit("pe", lambda e, c=c: e.matmul(O2[:, :], lhsT=wsl[:, c, j, :], rhs=hT[:, c, cols], start=(c == 0), stop=(c == NCH - 1)),
                                            reads=[("wsl", j)], writes=[pk])
                            return f

                        def evK():
                            fw.emit("dve", lambda e: e.tensor_copy(out=KA[0][:, cols], in_=O2[:, :]), reads=[pk], writes=[("KA", 0)])

                        def evQ():
                            fw.emit("dve", lambda e: e.tensor_scalar(out=QA[0][0:64, cols], in0=O2[0:64, :], scalar1=float(SCALE), scalar2=None, op0=ALU.mult),
                                    reads=[pk], writes=[("QA", 0)])
                            fw.emit("dve", lambda e: e.tensor_scalar(out=QA[1][64:128, cols], in0=O2[64:128, :], scalar1=float(SCALE), scalar2=None, op0=ALU.mult),
                                    reads=[pk], writes=[("QA", 1)])

                        def mmV(q):
                            def f():
                                tt = tb * 4 + q
                                for c in range(NCH):
                                    fw.emit("pe", lambda e, c=c: e.matmul(O2[:, q * 128:(q + 1) * 128], lhsT=hT[:, c, tt * 128:(tt + 1) * 128],
                                                                         rhs=wsl[:, c, 2, :], start=(c == 0), stop=(c == NCH - 1)),
                                            reads=[("wsl", 2)], writes=[pk])
                            return f

                        def evV():
                            fw.emit("dve", lambda e: e.tensor_copy(out=V[:, tb * 4:(tb + 1) * 4, :].rearrange("p a b -> p (a b)"), in_=O2[:, :]),
                                    reads=[pk], writes=["V"])

                        def seq(*fs):
                            def f():
                                for x in fs:
                                    x()
                            return f
                        pieces.append(mmK(0, 4, 1))
                        pieces.append(seq(mmK(4, 8, 1), evK))
                        pieces.append(mmK(0, 4, 0))
                        pieces.append(seq(mmK(4, 8, 0), evQ))
                        pieces.append(mmV(0)); pieces.append(mmV(1)); pieces.append(mmV(2))
                        pieces.append(seq(mmV(3), evV))
                        return pieces

                    if is_sb:
                        for u in sb_inproj_pieces(0):
                            u()
                    if not is_sb:
                        inb = Zv
                        ib = 0
                        ish = 0
                        for j in (1, 0):
                            for tb in range(NQB):
                                cols = slice(tb * 512, (tb + 1) * 512)
                                if is_sb:
                                    ps = inb[ib % 6]; pk = ("zb", ib % 6); ib += 1
                                    for c in range(NCH):
                                        fw.emit("pe", lambda e, ps=ps, j=j, c=c, cols=cols: e.matmul(ps[:, :], lhsT=wsl[:, c, j, :], rhs=hT[:, c, cols],
                                                                                                    start=(c == 0), stop=(c == NCH - 1)),
                                                reads=[("wsl", j)], writes=[pk])
                                    if j == 0:
                                        fw.emit("dve", lambda e, ps=ps, cols=cols: e.tensor_scalar(out=QA[0][0:64, cols], in0=ps[0:64, :], scalar1=float(SCALE),
                                                                                                  scalar2=None, op0=ALU.mult),
                                                reads=[pk], writes=[("QA", 0)])
                                        fw.emit("dve", lambda e, ps=ps, cols=cols: e.tensor_scalar(out=QA[1][64:128, cols], in0=ps[64:128, :], scalar1=float(SCALE),
                                                                                                  scalar2=None, op0=ALU.mult),
                                                reads=[pk], writes=[("QA", 1)])
                                    else:
                                        fw.emit("act", lambda e, ps=ps, cols=cols: e.activation(out=KA[0][:, cols], in_=ps[:, :], func=AF.Copy),
                                                reads=[pk], writes=[("KA", 0)])
                                else:
                                    ps = inb[ib % 6]; pk = ("zb", ib % 6); ib += 1
                                    for c in range(NCH):
                                        fw.emit("pe", lambda e, ps=ps, j=j, c=c, cols=cols: e.matmul(ps[:, :], lhsT=wsl[:, c, j, :], rhs=hT[:, c, cols],
                                                                                                    start=(c == 0), stop=(c == NCH - 1)),
                                                reads=[("wsl", j)], writes=[pk])
                                    dst = QA if j == 0 else KA
                                    dkey = "QA" if j == 0 else "KA"
                                    sh = SH[ish % 2]; shk = ("SH", ish % 2); ish += 1
                                    if j == 0:
                                        fw.emit("dve", lambda e, ps=ps, cols=cols: e.tensor_scalar(out=QA[0][0:64, cols], in0=ps[0:64, :], scalar1=float(SCALE),
                                                                                                  scalar2=None, op0=ALU.mult),
                                                reads=[pk], writes=[("QA", 0)])
                                        fw.emit("dve", lambda e, ps=ps, sh=sh: e.tensor_scalar(out=sh[64:128, :], in0=ps[64:128, :], scalar1=float(SCALE),
                                                                                              scalar2=None, op0=ALU.mult),
                                                reads=[pk], writes=[shk])
                                    else:
                                        fw.emit("act", lambda e, ps=ps, cols=cols: e.activation(out=KA[0][0:64, cols], in_=ps[0:64, :], func=AF.Copy),
                                                reads=[pk], writes=[("KA", 0)])
                                        fw.emit("act", lambda e, ps=ps, sh=sh: e.activation(out=sh[64:128, :], in_=ps[64:128, :], func=AF.Copy),
                                                reads=[pk], writes=[shk])
                                    fw.emit("sp", lambda e, dst=dst, sh=sh, cols=cols: e.dma_start(out=dst[1][0:64, cols], in_=sh[64:128, :]),
                                            reads=[shk], writes=[(dkey, 1)], dma="sh%d" % ((ish - 1) % 2))
                        for t4 in range(NT // 4):
                            ps = inb[ib % 6]; pk = ("zb", ib % 6); ib += 1
                            for q in range(4):
                                tt = t4 * 4 + q
                                for c in range(NCH):
                                    fw.emit("pe", lambda e, ps=ps, q=q, c=c, tt=tt: e.matmul(ps[:, q * 128:(q + 1) * 128], lhsT=hT[:, c, tt * 128:(tt + 1) * 128],
                                                                                            rhs=wsl[:, c, 2, :], start=(c == 0), stop=(c == NCH - 1)),
                                            reads=[("wsl", 2)], writes=[pk])
                            if t4 % 2 == 0:
                                fw.emit("dve", lambda e, ps=ps, t4=t4: e.tensor_copy(out=V[:, t4 * 4:(t4 + 1) * 4, :].rearrange("p a b -> p (a b)"), in_=ps[:, :]),
                                        reads=[pk], writes=["V"])
                            else:
                                fw.emit("act", lambda e, ps=ps, t4=t4: e.activation(out=V[:, t4 * 4:(t4 + 1) * 4, :].rearrange("p a b -> p (a b)"), in_=ps[:, :], func=AF.Copy),
                                        reads=[pk], writes=["V"])
                    if not is_sb and g + 1 < 8:
                        load_wsl(g + 1)
                    zk = [("zb", 0), ("zb", 1), ("zb", 2), ("zb", 3)]

                    if is_sb:
                        tasks = []
                        for qb in range(NQB):
                            for hh in range(2):
                                nkb = 4 * (qb + 1)
                                kbs = list(reversed(range(nkb)))
                                for n in range(nkb // 2):
                                    kA, kB = kbs[2 * n], kbs[2 * n + 1]
                                    tasks.append(dict(qb=qb, hh=hh, kA=kA, kB=kB, first=(n == 0), last=(n == nkb // 2 - 1),
                                                      lA=kA - 4 * qb, lB=kB - 4 * qb))
                        NTK = len(tasks)

                        def pairv(tile, c0):
                            if c0 == 0:
                                return tile[:, :]
                            return tile[:, :].rearrange("p (h x) -> p h x", h=2)[:, :, c0:512]

                        def sb_s1(i):
                            t = tasks[i]
                            hh = t["hh"]; q0 = 512 * t["qb"]
                            c0 = 128 * max(t["lB"], 0)
                            zz = ZZ[i % 3]
                            zkA, zkB = ("zb", 2 * (i % 3)), ("zb", 2 * (i % 3) + 1)
                            for half, kb, zkk in ((0, t["kA"], zkA), (1, t["kB"], zkB)):
                                fw.emit("pe", lambda e, half=half, kb=kb: e.matmul(zz[:, half * 512 + c0:(half + 1) * 512], lhsT=KA[0][:, kb * 128:(kb + 1) * 128],
                                                                                  rhs=QA[hh][:, q0 + c0:q0 + 512], start=True, stop=True),
                                        reads=[("KA", 0), ("QA", hh)], writes=[zkk])
                            if t["lA"] >= 0:
                                fw.emit("pe", lambda e: e.matmul(zz[:, c0:c0 + 256], lhsT=ident, rhs=cb[:, C_FULL:C_FULL + 256], start=False, stop=True, skip_group_check=True),
                                        reads=["cb"], writes=[zkA])
                            if t["lB"] >= 0:
                                fw.emit("pe", lambda e: e.matmul(zz[:, 512 + c0:512 + c0 + 128], lhsT=ident, rhs=cb[:, C_TRI:C_TRI + 128], start=False, stop=True, skip_group_check=True),
                                        reads=["cb"], writes=[zkB])
                            fw.emit("act", lambda e: e.activation(out=pairv(E2[i % 2], c0), in_=pairv(zz, c0), func=AF.Exp),
                                    reads=[zkA, zkB], writes=[("E", i % 2)])
                            fw.emit("act", lambda e: e.activation(out=pairv(SP2[i % 2], c0), in_=pairv(E2[i % 2], c0), func=AF.Ln, bias=1.0),
                                    reads=[("E", i % 2)], writes=[("SP", i % 2)])

                        def sb_s2(i):
                            t = tasks[i]
                            c0 = 128 * max(t["lB"], 0)
                            zz = ZZ[i % 3]
                            sp = SP2[i % 2]
                            zkA, zkB = ("zb", 2 * (i % 3)), ("zb", 2 * (i % 3) + 1)
                            fw.emit("pe", lambda e: e.matmul(zz[:, c0:512], lhsT=negU, rhs=sp[:, c0:512], start=False, stop=True, skip_group_check=True),
                                    reads=[("SP", i % 2), "cb"], writes=[zkA])
                            if not t["first"]:
                                fw.emit("pe", lambda e: e.matmul(zz[:, c0:512], lhsT=negones, rhs=SC[1][:, c0:512], start=False, stop=True, skip_group_check=True),
                                        reads=[("SC", 1), "cb"], writes=[zkA])
                            if t["first"]:
                                fw.emit("pool", lambda e: e.memset(SC[0][:, 0:256], 0.0), writes=[("SC", 0)])
                                fw.emit("pool", lambda e: e.memset(SC[1][:, 0:256], 0.0), writes=[("SC", 1)])
                                fw.emit("dve", lambda e: e.tensor_copy(out=SC[0][:, c0:512], in_=sp[:, c0:512]),
                                        reads=[("SP", i % 2)], writes=[("SC", 0)])
                            else:
                                fw.emit("dve", lambda e: e.tensor_tensor(out=SC[0][:, c0:512], in0=SC[1][:, c0:512], in1=sp[:, c0:512], op=ALU.add),
                                        reads=[("SP", i % 2), ("SC", 1)], writes=[("SC", 0)])
                            fw.emit("pe", lambda e: e.matmul(zz[:, 512 + c0:1024], lhsT=negU, rhs=sp[:, 512 + c0:1024], start=False, stop=True, skip_group_check=True),
                                    reads=[("SP", i % 2), "cb"], writes=[zkB])
                            fw.emit("pe", lambda e: e.matmul(zz[:, 512 + c0:1024], lhsT=negones, rhs=SC[0][:, c0:512], start=False, stop=True, skip_group_check=True),
                                    reads=[("SC", 0), "cb"], writes=[zkB])
                            if not t["last"]:
                                fw.emit("dve", lambda e: e.tensor_tensor(out=SC[1][:, c0:512], in0=SC[0][:, c0:512], in1=sp[:, 512 + c0:1024], op=ALU.add),
                                        reads=[("SP", i % 2), ("SC", 0)], writes=[("SC", 1)])
                            fw.emit("act", lambda e: e.activation(out=pairv(W2[i % 3], c0), in_=pairv(zz, c0), func=AF.Exp),
                                    reads=[zkA, zkB], writes=[("W", i % 3)])

                        def sb_s3(i):
                            t = tasks[i]
                            hh = t["hh"]; qb = t["qb"]
                            c0 = 128 * max(t["lB"], 0)
                            for half, kb in ((0, t["kA"]), (1, t["kB"])):
                                fw.emit("pe", lambda e, half=half, kb=kb: e.matmul(O1[:, c0:512], lhsT=V[:, kb, :], rhs=W2[i % 3][:, half * 512 + c0:(half + 1) * 512],
                                                                                  start=(t["first"] and half == 0), stop=(t["last"] and half == 1), skip_group_check=True),
                                        reads=[("W", i % 3), "V"], writes=[("OB", 0)])
                            if t["last"]:
                                hb = 64 * hh
                                fw.emit("dve", lambda e: e.tensor_copy(out=oT[hb:hb + 64, g, qb * 512:(qb + 1) * 512], in_=O1[hb:hb + 64, :]),
                                        reads=[("OB", 0)], writes=[("oT", g, qb, hh)])

                        pending = []
                        last_qb = -1
                        for i in range(NTK + 2):
                            if i < NTK:
                                if tasks[i]["qb"] != last_qb:
                                    last_qb = tasks[i]["qb"]
                                    assert not pending
                                    if last_qb + 1 < NQB:
                                        pending = sb_inproj_pieces(last_qb + 1)
                                sb_s1(i)
                            if 0 <= i - 1 < NTK:
                                sb_s2(i - 1)
                            if 0 <= i - 2 < NTK:
                                sb_s3(i - 2)
                            if i < NTK:
                                for _ in range(2 if tasks[i]["qb"] == 0 else 1):
                                    if pending:
                                        pending.pop(0)()
                                        if not pending and tasks[i]["qb"] == NQB - 2 and g + 1 < 8:
                                            load_wsl(g + 1)
                    else:
                        h = g - 4
                        tasks = []
                        for qb in range(NQB):
                            for c in range(2):
                                nkb = 4 * (qb + 1)
                                for kb in range(nkb):
                                    tasks.append(dict(qb=qb, c=c, kb=kb, first=(kb == 0), last=(kb == nkb - 1), kbl=kb - 4 * qb))
                        NTK = len(tasks)

                        def df_s1(i):
                            t = tasks[i]
                            m2 = t["c"]; q0 = 512 * t["qb"]; kb = t["kb"]
                            c0 = 128 * max(t["kbl"], 0)
                            z = Z[i % 4]
                            fw.emit("pe", lambda e: e.matmul(z[:, c0:512], lhsT=KA[m2][:, kb * 128:(kb + 1) * 128], rhs=QA[m2][:, q0 + c0:q0 + 512],
                                                             start=True, stop=True),
                                    reads=[("KA", m2), ("QA", m2)], writes=[zk[i % 4]])
                            if t["kbl"] >= 0:
                                fw.emit("pe", lambda e: e.matmul(z[:, c0:c0 + 128], lhsT=ident, rhs=cb[:, C_TRI2:C_TRI2 + 128], start=False, stop=True, skip_group_check=True),
                                        reads=["cb"], writes=[zk[i % 4]])
                            cblk = -SLOPES[h] * 128.0 * (4 * t["qb"] - kb)
                            fw.emit("act", lambda e: e.activation(out=W[i % 3][:, c0:512], in_=z[:, c0:512], func=AF.Exp, bias=float(cblk)),
                                    reads=[zk[i % 4]], writes=[("W", i % 3)])

                        def df_s2(i):
                            t = tasks[i]
                            kb = t["kb"]; qb = t["qb"]; m2 = t["c"]
                            c0 = 128 * max(t["kbl"], 0)
                            OO = OB[m2]
                            DD = D1 if m2 == 0 else D2
                            fw.emit("pe", lambda e: e.matmul(OO[:, c0:512], lhsT=V[:, kb, :], rhs=W[i % 3][:, c0:512], start=t["first"], stop=t["last"]),
                                    reads=[("W", i % 3), "V"], writes=[("OB", m2)])
                            fw.emit("pe", lambda e: e.matmul(DD[:, c0:512], lhsT=ones, rhs=W[i % 3][:, c0:512], start=t["first"], stop=t["last"]),
                                    reads=[("W", i % 3), "cb"], writes=[("zb", 4 + m2)])
                            if t["last"] and m2 == 1:
                                fw.emit("act", lambda e: e.activation(out=OS[0][:], in_=O1[:, :], func=AF.Copy), reads=[("OB", 0)], writes=[("OS", 0)])
                                fw.emit("dve", lambda e: e.reciprocal(out=R1, in_=D1), reads=[("zb", 4)], writes=[("E", 0)])
                                fw.emit("act", lambda e: e.activation(out=OS[1][:], in_=O2[:, :], func=AF.Copy), reads=[("OB", 1)], writes=[("OS", 1)])
                                fw.emit("dve", lambda e: e.reciprocal(out=R2, in_=D2), reads=[("zb", 5)], writes=[("E", 1)])
                                fw.emit("pool", lambda e: e.tensor_tensor(out=R1, in0=OS[0][:], in1=R1, op=ALU.mult),
                                        reads=[("OS", 0), ("E", 0)], writes=[("E", 0)])
                                fw.emit("dve", lambda e: e.scalar_tensor_tensor(out=R2, in0=R2, scalar=neglam[:, 0:1], in1=OS[1][:], op0=ALU.mult, op1=ALU.mult),
                                        reads=[("OS", 1), ("E", 1), "neglam"], writes=[("E", 1)])
                                fw.emit("pool", lambda e: e.tensor_tensor(out=oT[:, g, qb * 512:(qb + 1) * 512], in0=R1, in1=R2, op=ALU.add),
                                        reads=[("E", 0), ("E", 1)], writes=[("oT", g, qb, 0)])

                        for i in range(NTK + 2):
                            if i < NTK:
                                df_s1(i)
                            if 0 <= i - 2 < NTK:
                                df_s2(i - 2)
                if dbg:
                    fw.emit("sp", lambda e: e.dma_start(out=dbg_o[:, :, :], in_=oT[:]), reads=[("oT", g, qb, hh) for g in range(8) for qb in range(NQB) for hh in range(2)], dma="dbgo")
                if stop == 1:
                    fw.flush("pa", final_waits=[(fw.sems["dbgo"], 16)])
                    return nc
                fw.flush("pa", final_waits=[(fw.sems["dbgo"], 16)] if dbg else ())

        with ExitStack() as esb:
            X1 = sb("X1", [128, TPC, D], F32, esb)
            wmisc = sb("wmisc", [128, NCH, D], BF16, esb)
            wpp = sb("wpp", [128, 2, D], BF16, esb)
            wg = [sb(f"wg{i}", [128, NCH, DEXP], BF16, esb) for i in range(2)]
            wu = [sb(f"wu{i}", [128, NCH, DEXP], BF16, esb) for i in range(2)]
            wd = [sb(f"wd{i}", [128, 4, D], BF16, esb) for i in range(2)]
            hid = [sb("hid0", [128, 4, 512], BF16, esb)] * 2
            sg = [sb(f"sg{i}", [128, 512], BF16, esb) for i in range(2)]
            xn32 = sb("xn32", [128, D], F32, esb)
            h32 = sb("h32", [128, NCH, 128], F32, esb)
            wr32 = sb("wr32", [128, NCH, 20], F32, esb)
            brb = sb("brb", [128, 20], F32, esb)
            gfb = sb("gfb", [128, D], F32, esb)
            sq2 = sb("sq2", [128, 1024], BF16, esb)
            sq = [sq2[:, 0:512], sq2[:, 512:1024]]
            sqf = sq2[:].bitcast(F32)
            rs_t = sb("rs", [128, 512], F32, esb)
            rs = rs_t[:]
            junkb = sb("junkb", [128, D], BF16, esb)
            st = sb("stat", [128, 3, TPC, 4], F32, esb)
            lg = sb("lg", [128, TPC, 20], F32, esb)
            rt = sb("rt", [128, TPC, 64], F32, esb)
            gates = sb("gates", [128, TPC, NEXP], F32, esb)
            pl = [sb(f"pl{i}", [128, PLE], F32, esb) for i in range(2)]
            plb = sb("plb", [128, PLE], BF16, esb)
            pT = sb("pT", [128, 2, 128], BF16, esb)
            PB = [pst(f"PB{i}", [128, 512], F32, esb) for i in range(8)]

            wout_v = wout_d.rearrange("(c p) n -> p c n", p=128)
            wpg_v = wpg_d.rearrange("(c p) n -> p c n", p=128)
            wpp_v = wpp_d.rearrange("(c p) n -> p c n", p=128)

            fw.emit("sp", lambda e: e.dma_start(out=wr32[:], in_=wr_d.rearrange("(c p) n -> p c n", p=128)), writes=["wr32"], dma="wr32")
            fw.emit("sp", lambda e: e.dma_start(out=brb[:], in_=br_d.partition_broadcast(128)), writes=["brb"], dma="brb")
            fw.emit("sp", lambda e: e.dma_start(out=gfb[:], in_=gfin_d.partition_broadcast(128)), writes=["gfb"], dma="gfb")
            fw.emit("pool", lambda e: e.dma_start(out=wpp[:], in_=wpp_v), writes=["wpp"], dma="wpp")

            def load_expert(e_idx, b):
                for c in range(0, NCH, 4):
                    fw.emit("pool", lambda e, c=c: e.dma_start(out=wg[b][:, c:c + 4, :], in_=weg_d[e_idx].rearrange("(c p) n -> p c n", p=128)[:, c:c + 4, :]),
                            writes=[("wg", b, c)], dma="wg%d_%d" % (b, c))
                    fw.emit("pool", lambda e, c=c: e.dma_start(out=wu[b][:, c:c + 4, :], in_=weu_d[e_idx].rearrange("(c p) n -> p c n", p=128)[:, c:c + 4, :]),
                            writes=[("wu", b, c)], dma="wu%d_%d" % (b, c))
                for c in range(0, 4, 2):
                    fw.emit("pool", lambda e, c=c: e.dma_start(out=wd[b][:, c:c + 2, :], in_=wed_d[e_idx].rearrange("(c p) n -> p c n", p=128)[:, c:c + 2, :]),
                            writes=[("wd", b, c)], dma="wd%d_%d" % (b, c))

            def rstd_batch(kind):
                fw.emit("act", lambda e: e.activation(out=st[:, kind, :, 1], in_=st[:, kind, :, 0], func=AF.Ln, scale=1.0 / D, bias=EPS),
                        reads=[("st", kind, t) for t in range(TPC)], writes=[("stln", kind)])
                fw.emit("act", lambda e: e.activation(out=st[:, kind, :, 2], in_=st[:, kind, :, 1], func=AF.Exp, scale=-0.5),
                        reads=[("stln", kind)], writes=[("strs", kind)])

            def norm_T(kind, t, gk, tok0, want32):
                fw.emit("dve", lambda e: e.tensor_scalar(out=xn32[:], in0=X1[:, t, :], scalar1=st[:, kind, t, 2:3], scalar2=None, op0=ALU.mult),
                        reads=[("X1", t), ("strs", kind)], writes=["xn32"])
                for c in range(NCH):
                    bk = 2 + c // 4
                    fw.emit("pe", lambda e, c=c, bk=bk: e.transpose(out=PB[bk][:, (c % 4) * 128:(c % 4 + 1) * 128], in_=xn32[:, c * 128:(c + 1) * 128], identity=identf[:]),
                            reads=["xn32", "identf"], writes=[("PB", bk)])
                for k2 in range(2):
                    bk = 2 + k2
                    src = PB[bk][:, :].rearrange("p (c t) -> p c t", c=4)
                    gb = gcols[:, gk, 4 * k2:4 * k2 + 4].unsqueeze(2).to_broadcast([128, 4, 128])
                    if want32:
                        fw.emit("dve", lambda e, k2=k2, src=src, gb=gb: e.tensor_tensor(out=h32[:, 4 * k2:4 * k2 + 4, :], in0=src, in1=gb, op=ALU.mult),
                                reads=[("PB", bk), ("gcols", gk)], writes=[("h32", 4 * k2 + cc) for cc in range(4)])
                    else:
                        fw.emit("dve", lambda e, k2=k2, src=src, gb=gb: e.tensor_tensor(out=oT[:, 4 * k2:4 * k2 + 4, tok0:tok0 + 128], in0=src, in1=gb, op=ALU.mult),
                                reads=[("PB", bk), ("gcols", gk)], writes=[("oTt", tok0)])
                if want32:
                    fw.emit("dve", lambda e: e.tensor_copy(out=oT[:, :, tok0:tok0 + 128], in_=h32[:]),
                            reads=[("h32", c) for c in range(NCH)], writes=[("oTt", tok0)])

            def onorm(t0):
                for blk in range(CH // 512):
                    c0 = t0 + blk * 512
                    for grp in range(5):
                        chunks = [0, 1, 2, 3] if grp == 0 else [3 + grp]
                        nfeat = 512.0 if grp == 0 else 128.0
                        for n, c in enumerate(chunks):
                            sqb = sq[n % 2]
                            fw.emit("act", lambda e, c=c, sqb=sqb: e.activation(out=sqb, in_=oT[:, c, c0:c0 + 512], func=AF.Square),
                                    reads=[("oTb", c, c0)], writes=[("sq", n % 2)])
                            fw.emit("pe", lambda e, sqb=sqb, n=n: e.matmul(PB[7][:, :], lhsT=ones, rhs=sqb, start=(n == 0), stop=(n == len(chunks) - 1)),
                                    reads=[("sq", n % 2), "cb"], writes=[("PB", 7)])
                        fw.emit("act", lambda e, nfeat=nfeat: e.activation(out=rs, in_=PB[7][:, :], func=AF.Ln, scale=1.0 / nfeat, bias=EPS),
                                reads=[("PB", 7)], writes=["rs"])
                        fw.emit("act", lambda e: e.activation(out=rs, in_=rs, func=AF.Exp, scale=-0.5), reads=["rs"], writes=["rs"])
                        for c in chunks:
                            gcol = gcols[:, 3, c:c + 1] if grp == 0 else gcols[:, 4, 0:1]
                            fw.emit("dve", lambda e, c=c, gcol=gcol: e.scalar_tensor_tensor(out=oT[:, c, c0:c0 + 512], in0=oT[:, c, c0:c0 + 512], scalar=gcol, in1=rs,
                                                                                           op0=ALU.mult, op1=ALU.mult),
                                    reads=["rs", ("oTb", c, c0), ("gcols", 3), ("gcols", 4)], writes=[("oTb", c, c0)])

            for ck in range(NCK):
                t0 = ck * CH
                for hc in range(2):
                    fw.emit("pool", lambda e, hc=hc: e.dma_start(out=wmisc[:, 4 * hc:4 * hc + 4, :], in_=wout_v[:, 4 * hc:4 * hc + 4, :]),
                            writes=[("wmisc", hc)], dma="wmisc%d" % hc)
                load_expert(0, 0)
                if ck == 0:
                    onorm(t0)
                for t in range(TPC):
                    tok0 = t0 + t * 128
                    c0 = t0 + (t // 4) * 512
                    b = t % 2
                    fw.emit("sp", lambda e, t=t, tok0=tok0: e.dma_start(out=X1[:, t, :], in_=x_d[tok0:tok0 + 128, :]), writes=[("X1", t)], dma="xl%d" % t)
                    for nh in range(2):
                        bk = 2 * (t % 2) + nh
                        for c in range(NCH):
                            fw.emit("pe", lambda e, nh=nh, c=c, tok0=tok0, bk=bk: e.matmul(PB[bk][:, :], lhsT=oT[:, c, tok0:tok0 + 128], rhs=wmisc[:, c, nh * 512:(nh + 1) * 512],
                                                                                          start=(c == 0), stop=(c == NCH - 1)),
                                    reads=[("oTb", c, c0), ("wmisc", c // 4), ("oTt", tok0)], writes=[("PB", bk)])
                        fw.emit("dve", lambda e, nh=nh, t=t, b=b, bk=bk: e.tensor_tensor(out=X1[:, t, nh * 512:(nh + 1) * 512], in0=PB[bk][:, :], in1=X1[:, t, nh * 512:(nh + 1) * 512], op=ALU.add),
                                reads=[("PB", bk), ("X1", t)], writes=[("X1", t)])
                    fw.emit("act", lambda e, t=t: e.activation(out=junkb[:], in_=X1[:, t, :], func=AF.Square, accum_out=st[:, 0, t, 0:1]),
                            reads=[("X1", t)], writes=["junkb", ("st", 0, t)])
                    if dbg:
                        fw.emit("sp", lambda e, t=t, tok0=tok0: e.dma_start(out=dbg_x1[tok0:tok0 + 128, :], in_=X1[:, t, :]), reads=[("X1", t)], dma="dbgx%d" % t)
                rstd_batch(0)
                for t in range(TPC):
                    tok0 = t0 + t * 128
                    norm_T(0, t, 1, tok0, True)
                    for c in range(NCH):
                        fw.emit("pe", lambda e, c=c: e.matmul(PB[7][:, 0:20], lhsT=h32[:, c, :], rhs=wr32[:, c, :], start=(c == 0), stop=(c == NCH - 1)),
                                reads=[("h32", cc) for cc in range(NCH)] + ["wr32"], writes=[("PB", 7)])
                    fw.emit("dve", lambda e, t=t: e.tensor_tensor(out=lg[:, t, :], in0=PB[7][:, 0:20], in1=brb[:], op=ALU.add),
                            reads=[("PB", 7), "brb"], writes=["lg"])
                G = lg[:, :, 0:4]
                EL = lg[:, :, 4:20]
                gmax = rt[:, :, 0:1]
                goh = rt[:, :, 1:5]
                gex = rt[:, :, 5:9]
                gsum = rt[:, :, 9:10]
                gw = rt[:, :, 10:11]
                msk = rt[:, :, 16:32]
                m1 = rt[:, :, 11:12]
                m2 = rt[:, :, 12:13]
                oh1 = rt[:, :, 32:48]
                oh2 = rt[:, :, 48:64]
                dlt = rt[:, :, 13:14]
                w1 = rt[:, :, 14:15]
                w2 = rt[:, :, 15:16]
                BIG = 1.0e4

                def dv(fn, r=("lg", "rt"), w=("rt",)):
                    fw.emit("dve", fn, reads=list(r), writes=list(w))

                dv(lambda e: e.tensor_reduce(out=gmax, in_=G, axis=AX.X, op=ALU.max))
                dv(lambda e: e.tensor_tensor(out=goh, in0=G, in1=gmax.to_broadcast([128, TPC, 4]), op=ALU.is_equal))
                dv(lambda e: e.tensor_tensor(out=gex, in0=G, in1=gmax.to_broadcast([128, TPC, 4]), op=ALU.subtract))
                fw.emit("act", lambda e: e.activation(out=gex, in_=gex, func=AF.Exp), reads=["rt"], writes=["rt"])
                dv(lambda e: e.tensor_reduce(out=gsum, in_=gex, axis=AX.X, op=ALU.add))
                dv(lambda e: e.reciprocal(out=gw, in_=gsum))
                dv(lambda e: e.tensor_scalar(out=gex, in0=goh, scalar1=-1.0, scalar2=BIG, op0=ALU.add, op1=ALU.mult))
                dv(lambda e: e.tensor_tensor(out=msk.rearrange("p t (g k) -> p t g k", k=4), in0=EL.rearrange("p t (g k) -> p t g k", k=4),
                                             in1=gex.unsqueeze(3).to_broadcast([128, TPC, 4, 4]), op=ALU.add))
                dv(lambda e: e.tensor_reduce(out=m1, in_=msk, axis=AX.X, op=ALU.max))
                dv(lambda e: e.tensor_tensor(out=oh1, in0=msk, in1=m1.to_broadcast([128, TPC, 16]), op=ALU.is_equal))
                dv(lambda e: e.scalar_tensor_tensor(out=msk, in0=oh1, scalar=-BIG, in1=msk, op0=ALU.mult, op1=ALU.add))
                dv(lambda e: e.tensor_reduce(out=m2, in_=msk, axis=AX.X, op=ALU.max))
                dv(lambda e: e.tensor_tensor(out=oh2, in0=msk, in1=m2.to_broadcast([128, TPC, 16]), op=ALU.is_equal))
                dv(lambda e: e.tensor_tensor(out=dlt, in0=m2, in1=m1, op=ALU.subtract))
                fw.emit("act", lambda e: e.activation(out=dlt, in_=dlt, func=AF.Exp), reads=["rt"], writes=["rt"])
                dv(lambda e: e.tensor_scalar(out=w1, in0=dlt, scalar1=1.0, scalar2=None, op0=ALU.add))
                dv(lambda e: e.reciprocal(out=w1, in_=w1))
                dv(lambda e: e.tensor_tensor(out=w2, in0=dlt, in1=w1, op=ALU.mult))
                dv(lambda e: e.tensor_tensor(out=w1, in0=w1, in1=gw, op=ALU.mult))
                dv(lambda e: e.tensor_tensor(out=w2, in0=w2, in1=gw, op=ALU.mult))
                dv(lambda e: e.tensor_tensor(out=oh1, in0=oh1, in1=w1.to_broadcast([128, TPC, 16]), op=ALU.mult))
                dv(lambda e: e.tensor_tensor(out=oh2, in0=oh2, in1=w2.to_broadcast([128, TPC, 16]), op=ALU.mult))
                dv(lambda e: e.tensor_tensor(out=gates[:], in0=oh1, in1=oh2, op=ALU.add), w=("gates",))

                for ex in range(NEXP):
                    b = ex % 2
                    if ex + 1 < NEXP:
                        load_expert(ex + 1, (ex + 1) % 2)
                    if ex == 4 and ck + 1 < NCK:
                        onorm(t0 + CH)
                    for tb in range(CH // 512):
                        hb_ = hid[tb % 2]
                        c0 = t0 + tb * 512
                        for jc in range(4):
                            gp = PB[jc % 2]; up = PB[2 + jc % 2]
                            for c in range(NCH):
                                fw.emit("pe", lambda e, gp=gp, c=c, jc=jc: e.matmul(gp[:, :], lhsT=wg[b][:, c, jc * 128:(jc + 1) * 128], rhs=oT[:, c, c0:c0 + 512],
                                                                                   start=(c == 0), stop=(c == NCH - 1)),
                                        reads=[("wg", b, 4 * (c // 4))] + [("oTt", c0 + 128 * q) for q in range(4)], writes=[("PB", jc % 2)])
                            for c in range(NCH):
                                fw.emit("pe", lambda e, up=up, c=c, jc=jc: e.matmul(up[:, :], lhsT=wu[b][:, c, jc * 128:(jc + 1) * 128], rhs=oT[:, c, c0:c0 + 512],
                                                                                   start=(c == 0), stop=(c == NCH - 1)),
                                        reads=[("wu", b, 4 * (c // 4))] + [("oTt", c0 + 128 * q) for q in range(4)], writes=[("PB", 2 + jc % 2)])
                            fw.emit("act", lambda e, gp=gp, jc=jc: e.activation(out=sg[jc % 2][:], in_=gp[:, :], func=AF.Silu),
                                    reads=[("PB", jc % 2)], writes=[("sg", jc % 2)])
                            fw.emit("dve", lambda e, up=up, jc=jc, hb_=hb_: e.tensor_tensor(out=hb_[:, jc, :], in0=up[:, :], in1=sg[jc % 2][:], op=ALU.mult),
                                    reads=[("PB", 2 + jc % 2), ("sg", jc % 2)], writes=[("hid", tb % 2, jc)])
                        for q in range(4):
                            t = tb * 4 + q
                            for nh in range(2):
                                yp = PB[4 + (2 * q + nh) % 3]
                                yk = ("PB", 4 + (2 * q + nh) % 3)
                                for jc in range(4):
                                    fw.emit("pe", lambda e, yp=yp, jc=jc, q=q, nh=nh, hb_=hb_: e.matmul(yp[:, :], lhsT=hb_[:, jc, q * 128:(q + 1) * 128],
                                                                                                       rhs=wd[b][:, jc, nh * 512:(nh + 1) * 512], start=(jc == 0), stop=(jc == 3)),
                                            reads=[("hid", tb % 2, jc), ("wd", b, 2 * (jc // 2))], writes=[yk])
                                fw.emit("dve", lambda e, yp=yp, t=t, nh=nh: e.scalar_tensor_tensor(out=X1[:, t, nh * 512:(nh + 1) * 512], in0=yp[:, :], scalar=gates[:, t, ex:ex + 1],
                                                                                                  in1=X1[:, t, nh * 512:(nh + 1) * 512], op0=ALU.mult, op1=ALU.add),
                                        reads=[yk, "gates", ("X1", t)], writes=[("X1", t)])
                for hc in range(2):
                    fw.emit("pool", lambda e, hc=hc: e.dma_start(out=wmisc[:, 4 * hc:4 * hc + 4, :], in_=wpg_v[:, 4 * hc:4 * hc + 4, :]),
                            writes=[("wmisc", hc)], dma="wmisc%d" % hc)
                for t in range(TPC):
                    fw.emit("act", lambda e, t=t: e.activation(out=junkb[:], in_=X1[:, t, :], func=AF.Square, accum_out=st[:, 1, t, 0:1]),
                            reads=[("X1", t)], writes=["junkb", ("st", 1, t)])
                rstd_batch(1)
                for t in range(TPC):
                    tok0 = t0 + t * 128
                    b = t % 2
                    norm_T(1, t, 2, tok0, False)
                    fw.emit("sp", lambda e, b=b, tok0=tok0: e.dma_start(out=pl[b][:], in_=p_d[tok0:tok0 + 128, :]), writes=[("pl", b)], dma="pl%d" % b)
                    fw.emit("pool", lambda e, b=b: e.tensor_copy(out=plb[:], in_=pl[b][:]), reads=[("pl", b)], writes=["plb"])
                    tpv = PB[6][:, 0:128].bitcast(BF16)
                    for c2 in range(2):
                        fw.emit("pe", lambda e, c2=c2, tpv=tpv: e.transpose(out=tpv[:, c2 * 128:(c2 + 1) * 128], in_=plb[:, c2 * 128:(c2 + 1) * 128], identity=ident),
                                reads=["plb", "cb"], writes=[("PB", 6)])
                    fw.emit("dve", lambda e, tpv=tpv: e.tensor_copy(out=pT[:].rearrange("p a b -> p (a b)"), in_=tpv), reads=[("PB", 6)], writes=["pT"])
                    for nh in range(2):
                        for c in range(NCH):
                            fw.emit("pe", lambda e, nh=nh, c=c, tok0=tok0: e.matmul(PB[nh][:, :], lhsT=oT[:, c, tok0:tok0 + 128], rhs=wmisc[:, c, nh * 512:(nh + 1) * 512],
                                                                                   start=(c == 0), stop=(c == NCH - 1)),
                                    reads=[("oTt", tok0), ("wmisc", c // 4)], writes=[("PB", nh)])
                        for c2 in range(2):
                            fw.emit("pe", lambda e, nh=nh, c2=c2: e.matmul(PB[4 + nh][:, :], lhsT=pT[:, c2, :], rhs=wpp[:, c2, nh * 512:(nh + 1) * 512],
                                                                          start=(c2 == 0), stop=(c2 == 1)),
                                    reads=["pT", "wpp"], writes=[("PB", 4 + nh)])
                        sgv = h32[:].rearrange("p a b -> p (a b)")[:, nh * 512:(nh + 1) * 512]
                        sgk = [("h32", 4 * nh + cc) for cc in range(4)]
                        fw.emit("act", lambda e, nh=nh, sgv=sgv: e.activation(out=sgv, in_=PB[nh][:, :], func=AF.Sigmoid),
                                reads=[("PB", nh)], writes=sgk)
                        tmpb = rs if nh == 0 else sqf
                        tmpk = ["rs"] if nh == 0 else [("sq", 0), ("sq", 1)]
                        fw.emit("dve", lambda e, nh=nh, tmpb=tmpb, sgv=sgv: e.tensor_tensor(out=tmpb, in0=PB[4 + nh][:, :], in1=sgv, op=ALU.mult),
                                reads=[("PB", 4 + nh)] + sgk, writes=tmpk)
                        fw.emit("pool", lambda e, nh=nh, t=t, tmpb=tmpb: e.tensor_tensor(out=X1[:, t, nh * 512:(nh + 1) * 512], in0=X1[:, t, nh * 512:(nh + 1) * 512],
                                                                                        in1=tmpb, op=ALU.add),
                                reads=tmpk + [("X1", t)], writes=[("X1", t)])
                    fw.emit("act", lambda e, t=t: e.activation(out=junkb[:], in_=X1[:, t, :], func=AF.Square, accum_out=st[:, 2, t, 0:1]),
                            reads=[("X1", t)], writes=["junkb", ("st", 2, t)])
                rstd_batch(2)
                for t in range(TPC):
                    tok0 = t0 + t * 128
                    b = t % 2
                    fw.emit("dve", lambda e, t=t, b=b: e.scalar_tensor_tensor(out=X1[:, t, :], in0=X1[:, t, :], scalar=st[:, 2, t, 2:3], in1=gfb[:], op0=ALU.mult, op1=ALU.mult),
                            reads=[("X1", t), ("strs", 2), "gfb"], writes=[("X1", t)])
                    fw.emit("sp", lambda e, t=t, tok0=tok0: e.dma_start(out=out_d[tok0:tok0 + 128, :], in_=X1[:, t, :]), reads=[("X1", t)], dma="out%d" % t)
            finals = [(fw.sems[n], fw.dma_cnt[n]) for n in fw.sems if n.startswith("out") or n.startswith("dbg")]
            fw.flush("pb", final_waits=finals)
    return nc


_NC_CACHE = {}


def _prep_inputs(inputs, S):
    f = lambda a: np.ascontiguousarray(np.asarray(a, dtype=np.float32))
    shared = {
        "cst": make_consts(),
        "aug": make_aug(S),
        "g_mix": f(inputs["g_mix"][0]),
        "w_in": f(inputs["w_in"][0]),
        "lam4": f(np.stack([inputs["lambda_q1"][0], inputs["lambda_k1"][0], inputs["lambda_q2"][0], inputs["lambda_k2"][0]], 0)),
        "g_sb_out": f(inputs["g_sb_out"][0]),
        "g_df_out": f(inputs["g_df_out"][0]),
        "w_out": f(inputs["w_out"][0]),
        "g_ffn": f(inputs["g_ffn"][0]),
        "w_router": f(np.concatenate([inputs["w_router_group"][0], inputs["w_router_expert"][0]], axis=1)),
        "b_router": f(np.concatenate([inputs["b_router_group"][0], inputs["b_router_expert"][0]], axis=0)),
        "w_expert_gate": f(inputs["w_expert_gate"][0]),
        "w_expert_up": f(inputs["w_expert_up"][0]),
        "w_expert_down": f(inputs["w_expert_down"][0]),
        "g_ple": f(inputs["g_ple"][0]),
        "w_ple_gate": f(inputs["w_ple_gate"][0]),
        "w_ple_proj": f(inputs["w_ple_proj"][0]),
        "g_final": f(inputs["g_final"]),
    }
    return shared


def kernel(**inputs):
    x = np.asarray(inputs["x"], dtype=np.float32)
    p = np.asarray(inputs["p"], dtype=np.float32)
    B, S, _ = x.shape
    if S not in _NC_CACHE:
        _NC_CACHE[S] = build(S)
    nc = _NC_CACHE[S]
    shared = _prep_inputs(inputs, S)
    in_maps = []
    for b in range(B):
        m = dict(shared)
        m["x"] = np.ascontiguousarray(x[b])
        m["p"] = np.ascontiguousarray(p[0, b])
        in_maps.append(m)
    res = run_bass_kernel_spmd(nc, in_maps, core_ids=list(range(B)))
    return np.stack([np.asarray(r["out"], dtype=np.float32) for r in res.results], axis=0)
```

```python
import math
from contextlib import ExitStack

import numpy as np
import concourse.bass as bass
import concourse.mybir as mybir
from concourse.bass_utils import run_bass_kernel_spmd

F32 = mybir.dt.float32
BF16 = mybir.dt.bfloat16
AF = mybir.ActivationFunctionType
ALU = mybir.AluOpType
AX = mybir.AxisListType

D = 1024
NCH = 8
HD = 64
NEXP = 16
DEXP = 512
PLE = 256
EPS = 1e-6
SCALE = HD ** -0.5
SLOPES = [2.0 ** (-8.0 * (h + 1) / 4) for h in range(4)]
LAMBDA_INIT = 0.8 - 0.6 * math.exp(-0.3 * 0)
SAME_ENGINE_SYNC = True

C_ID, C_NEGU, C_ONES, C_NEGONES = 0, 128, 256, 384
C_KAUG = 512
C_QAUG = 1024
C_FULL = 3072
C_TRI = 3200
C_TRI2 = 3328
CW = 3456
MASK_BIG = 30000.0


def make_consts():
    c = np.zeros((128, CW), np.float32)
    j = np.arange(128)[:, None]
    s = np.arange(128)[None, :]
    c[:, C_ID:C_ID + 128] = (j == s)
    c[:, C_NEGU:C_NEGU + 128] = -(j >= s).astype(np.float32)
    c[:, C_ONES:C_ONES + 128] = 1.0
    c[:, C_NEGONES:C_NEGONES + 128] = -1.0
    c[:, C_FULL:C_FULL + 128] = -MASK_BIG
    c[:, C_TRI:C_TRI + 128] = np.where(s <= j, -MASK_BIG, 0.0)
    c[:, C_TRI2:C_TRI2 + 128] = np.where(s < j, -MASK_BIG, 0.0)
    tl = np.arange(512)
    for h in range(4):
        sl = SLOPES[h]
        k = c[:, C_KAUG + 128 * h:C_KAUG + 128 * (h + 1)]
        k[0, :] = sl * np.arange(128)
        k[1, :] = 1.0
        k[2, :] = 1.0
        q = c[:, C_QAUG + 512 * h:C_QAUG + 512 * (h + 1)]
        q[0, :] = 1.0
        q[1, :] = -sl * (tl % 128)
        q[2, :] = -sl * 128.0 * (tl // 128)
    return c


def make_aug(S):
    a = np.zeros((4, 2, 3, S), np.float32)
    t = np.arange(S)
    for h in range(4):
        sl = SLOPES[h]
        a[h, 0, 0] = 1.0
        a[h, 0, 1] = -sl * (t % 128)
        a[h, 0, 2] = -sl * 128.0 * ((t // 128) % 4)
        a[h, 1, 0] = sl * (t % 128)
        a[h, 1, 1] = 1.0
        a[h, 1, 2] = 1.0
    return a


class _Rec:
    def __getattr__(self, name):
        def f(*a, **k):
            return (name, a, k)
        return f


class FW:
    def __init__(self, nc, es):
        self.nc = nc
        self.es = es
        self.engs = {"pe": nc.tensor, "act": nc.scalar, "dve": nc.vector, "pool": nc.gpsimd, "sp": nc.sync}
        self.prog = {e: es.enter_context(nc.semaphore("prog_" + e)) for e in self.engs}
        self.cnt = {e: 0 for e in self.engs}
        self.waited = {e: {} for e in self.engs}
        self.dma_cnt = {}
        self.sems = {}
        self.reset()

    def reset(self):
        self.ops = {e: [] for e in self.engs}
        self.lastw = {}
        self.readers = {}

    def dsem(self, name):
        if name not in self.sems:
            self.sems[name] = self.es.enter_context(self.nc.semaphore("d_" + name))
            self.dma_cnt[name] = 0
        return name

    def emit(self, eng, fn, reads=(), writes=(), dma=None):
        rec = fn(_Rec())
        deps = []
        for r in reads:
            t = self.lastw.get(r)
            if t is not None:
                deps.append(t)
        for w in writes:
            t = self.lastw.get(w)
            if t is not None:
                deps.append(t)
            deps.extend(self.readers.get(w, {}).values())
        waits = {}
        for (skey, sem, val, teng) in deps:
            if teng == eng and (eng == "pe" or not SAME_ENGINE_SYNC):
                continue
            if self.waited[eng].get(skey, -1) >= val:
                continue
            if waits.get(skey, (None, -1))[1] < val:
                waits[skey] = (sem, val)
        for skey, (sem, val) in waits.items():
            self.waited[eng][skey] = val
        if dma is not None:
            self.dsem(dma)
            self.dma_cnt[dma] += 16
            tok = ("d_" + dma, self.sems[dma], self.dma_cnt[dma], None)
            inc = (self.sems[dma], 16)
        else:
            self.cnt[eng] += 1
            tok = ("p_" + eng, self.prog[eng], self.cnt[eng], eng)
            inc = (self.prog[eng], 1)
        for w in writes:
            self.lastw[w] = tok
            self.readers[w] = {}
        for r in reads:
            self.readers.setdefault(r, {})[tok[0]] = tok
        self.ops[eng].append((list(waits.values()), rec, inc))
        return tok

    def flush(self, name, final_waits=()):
        nc = self.nc
        ops = self.ops
        with nc.Block() as block:
            def mk(ename):
                def body(e):
                    for waits, rec, inc in ops[ename]:
                        for sem, val in waits:
                            e.wait_ge(sem, val)
                        inst = getattr(e, rec[0])(*rec[1], **rec[2])
                        inst.then_inc(inc[0], inc[1])
                    if ename == "sp":
                        for sem, val in final_waits:
                            e.wait_ge(sem, val)
                return body
            block.sync(mk("sp"))
            block.tensor(mk("pe"))
            block.scalar(mk("act"))
            block.vector(mk("dve"))
            block.gpsimd(mk("pool"))
        self.reset()


def build(S, dbg=False, stop=9):
    assert S % 1024 == 0
    NT = S // 128
    NQB = S // 512
    CH = 1024
    NCK = S // CH
    TPC = CH // 128

    nc = bass.Bass("TRN2", target_bir_lowering=False)

    def din(name, shape):
        return nc.dram_tensor(name, list(shape), F32, kind="ExternalInput").ap()

    x_d = din("x", [S, D])
    p_d = din("p", [S, PLE])
    cst_d = din("cst", [128, CW])
    aug_d = din("aug", [4, 2, 3, S])
    gmix_d = din("g_mix", [D])
    win_d = din("w_in", [D, 3072])
    lam_d = din("lam4", [4, HD])
    gsb_d = din("g_sb_out", [512])
    gdf_d = din("g_df_out", [128])
    wout_d = din("w_out", [D, D])
    gffn_d = din("g_ffn", [D])
    wr_d = din("w_router", [D, 20])
    br_d = din("b_router", [20])
    weg_d = din("w_expert_gate", [NEXP, D, DEXP])
    weu_d = din("w_expert_up", [NEXP, D, DEXP])
    wed_d = din("w_expert_down", [NEXP, DEXP, D])
    gple_d = din("g_ple", [D])
    wpg_d = din("w_ple_gate", [D, D])
    wpp_d = din("w_ple_proj", [PLE, D])
    gfin_d = din("g_final", [D])
    out_d = nc.dram_tensor("out", [S, D], F32, kind="ExternalOutput").ap()
    if dbg:
        dbg_o = nc.dram_tensor("dbg_o", [128, NCH, S], BF16, kind="ExternalOutput").ap()
        dbg_x1 = nc.dram_tensor("dbg_x1", [S, D], F32, kind="ExternalOutput").ap()

    with ExitStack() as es:
        fw = FW(nc, es)

        def sb(name, shape, dt, stack=es):
            return stack.enter_context(nc.sbuf_tensor(name, list(shape), dt))

        def pst(name, shape, dt, stack):
            return stack.enter_context(nc.psum_tensor(name, list(shape), dt))

        oT = sb("oT", [128, NCH, S], BF16)
        cb = sb("cb", [128, CW], BF16)
        identf = sb("identf", [128, 128], F32)
        gcols = sb("gcols", [128, 5, NCH], F32)
        lamt = sb("lamt", [128, 4, HD], F32)
        lamw = sb("lamw", [128, 8], F32)
        neglam = sb("neglam", [128, 1], F32)

        ident = cb[:, C_ID:C_ID + 128]
        negU = cb[:, C_NEGU:C_NEGU + 128]
        ones = cb[:, C_ONES:C_ONES + 128]
        negones = cb[:, C_NEGONES:C_NEGONES + 128]

        with ExitStack() as esA:
            hT = sb("hT", [128, NCH, S], BF16, esA)
            with ExitStack() as es0:
                xt = [sb(f"xt{i}", [128, D], F32, es0) for i in range(2)]
                xn = [sb(f"xn{i}", [128, D], BF16, es0) for i in range(2)]
                junk = sb("junk0", [128, D], BF16, es0)
                ssq = sb("ssq0", [128, NT], F32, es0)
                lnv = sb("lnv0", [128, NT], F32, es0)
                rstd = sb("rstd0", [128, NT], F32, es0)
                tp = [pst(f"tp{i}", [128, D], BF16, es0) for i in range(2)]

                fw.emit("pool", lambda e: e.dma_start(out=cb[:], in_=cst_d[:, :]), writes=["cb"], dma="cst")
                fw.emit("sp", lambda e: e.dma_start(out=identf[:], in_=cst_d[:, C_ID:C_ID + 128]), writes=["identf"], dma="cst2")
                gst = sb("gst", [8, 5, 128], F32, es0)
                gps = pst("gps", [128, 5, 8], F32, es0)
                for k, (gd, nr) in enumerate([(gmix_d, 8), (gffn_d, 8), (gple_d, 8), (gsb_d, 4), (gdf_d, 1)]):
                    fw.emit("sp", lambda e, k=k, gd=gd, nr=nr: e.dma_start(out=gst[0:nr, k, :], in_=gd.rearrange("(c p) -> c p", p=128)),
                            writes=[("gst", k)], dma="gc%d" % k)
                    fw.emit("pe", lambda e, k=k, nr=nr: e.transpose(out=gps[:, k, 0:nr], in_=gst[0:nr, k, :], identity=identf[0:nr, 0:nr]),
                            reads=[("gst", k), "identf"], writes=["gps"])
                    fw.emit("dve", lambda e, k=k, nr=nr: e.tensor_copy(out=gcols[:, k, 0:nr], in_=gps[:, k, 0:nr]),
                            reads=["gps"], writes=[("gcols", k)])
                fw.emit("dve", lambda e: e.tensor_scalar(out=gcols[:, 4, 0:1], in0=gcols[:, 4, 0:1], scalar1=float(1.0 - LAMBDA_INIT),
                                                          scalar2=None, op0=ALU.mult),
                        reads=[("gcols", 4)], writes=[("gcols", 4)])
                if True:
                    fw.emit("sp", lambda e: e.dma_start(out=lamt[:].rearrange("p a b -> p (a b)"),
                                                         in_=lam_d.rearrange("a b -> (a b)").partition_broadcast(128)),
                            writes=["lamt"], dma="lam")
                    fw.emit("dve", lambda e: e.tensor_tensor(out=lamt[:, 0, :], in0=lamt[:, 0, :], in1=lamt[:, 1, :], op=ALU.mult),
                            reads=["lamt"], writes=["lamt"])
                    fw.emit("dve", lambda e: e.tensor_tensor(out=lamt[:, 2, :], in0=lamt[:, 2, :], in1=lamt[:, 3, :], op=ALU.mult),
                            reads=["lamt"], writes=["lamt"])
                    fw.emit("dve", lambda e: e.reduce_sum(out=lamw[:, 0:1], in_=lamt[:, 0, :], axis=AX.X), reads=["lamt"], writes=["lamw"])
                    fw.emit("dve", lambda e: e.reduce_sum(out=lamw[:, 1:2], in_=lamt[:, 2, :], axis=AX.X), reads=["lamt"], writes=["lamw"])
                    fw.emit("act", lambda e: e.activation(out=lamw[:, 2:4], in_=lamw[:, 0:2], func=AF.Exp), reads=["lamw"], writes=["lamw"])
                    fw.emit("dve", lambda e: e.tensor_tensor(out=lamw[:, 4:5], in0=lamw[:, 3:4], in1=lamw[:, 2:3], op=ALU.subtract),
                            reads=["lamw"], writes=["lamw"])
                    fw.emit("dve", lambda e: e.tensor_scalar(out=neglam[:], in0=lamw[:, 4:5], scalar1=float(-LAMBDA_INIT), scalar2=None, op0=ALU.add),
                            reads=["lamw"], writes=["neglam"])

                for i in range(NT):
                    b = i % 2
                    fw.emit("sp", lambda e, i=i, b=b: e.dma_start(out=xt[b][:], in_=x_d[i * 128:(i + 1) * 128, :]),
                            writes=[("xt", b)], dma="xt%d" % b)
                    fw.emit("act", lambda e, i=i, b=b: e.activation(out=junk[:], in_=xt[b][:], func=AF.Square, accum_out=ssq[:, i:i + 1]),
                            reads=[("xt", b)], writes=["junk", ("ssq", i)])
                    fw.emit("act", lambda e, i=i: e.activation(out=lnv[:, i:i + 1], in_=ssq[:, i:i + 1], func=AF.Ln, scale=1.0 / D, bias=EPS),
                            reads=[("ssq", i)], writes=[("lnv", i)])
                    fw.emit("act", lambda e, i=i: e.activation(out=rstd[:, i:i + 1], in_=lnv[:, i:i + 1], func=AF.Exp, scale=-0.5),
                            reads=[("lnv", i)], writes=[("rstd", i)])
                    fw.emit("dve", lambda e, i=i, b=b: e.tensor_scalar(out=xn[b][:], in0=xt[b][:], scalar1=rstd[:, i:i + 1], scalar2=None, op0=ALU.mult),
                            reads=[("xt", b), ("rstd", i)], writes=[("xn", b)])
                    for c in range(NCH):
                        fw.emit("pe", lambda e, b=b, c=c: e.transpose(out=tp[b][:, c * 128:(c + 1) * 128], in_=xn[b][:, c * 128:(c + 1) * 128], identity=ident),
                                reads=[("xn", b), "cb"], writes=[("tp", b)])
                    fw.emit("dve", lambda e, i=i, b=b: e.tensor_tensor(out=hT[:, :, i * 128:(i + 1) * 128], in0=tp[b][:, :].rearrange("p (c t) -> p c t", c=NCH),
                                                                     in1=gcols[:, 0, 0:NCH].unsqueeze(2).to_broadcast([128, NCH, 128]), op=ALU.mult),
                            reads=[("tp", b), ("gcols", 0)], writes=[("hT", i, c) for c in range(NCH)])
                if stop == 0:
                    fw.emit("sp", lambda e: e.dma_start(out=dbg_o[:, :, :], in_=hT[:]), reads=[("hT", i, c) for i in range(NT) for c in range(NCH)], dma="dbgo")
                    fw.flush("p0", final_waits=[(fw.sems["dbgo"], 16)])
                    return nc
                fw.flush("p0")

            with ExitStack() as esa:
                QA = [sb(f"QA{i}", [128, S], BF16, esa) for i in range(2)]
                KA = [sb(f"KA{i}", [128, S], BF16, esa) for i in range(2)]
                V = sb("V", [128, NT, 128], BF16, esa)
                wsl = sb("wsl", [128, NCH, 3, 128], BF16, esa)
                E2 = [sb(f"E{i}", [128, 1024], BF16, esa) for i in range(2)]
                SP2 = [sb(f"SP{i}", [128, 1024], BF16, esa) for i in range(2)]
                SC = [sb(f"SC{i}", [128, 512], BF16, esa) for i in range(2)]
                W2 = [sb(f"W{i}", [128, 1024], BF16, esa) for i in range(3)]
                W = [w[:, 0:512] for w in W2]
                OS = [sb(f"OS{i}", [128, 512], F32, esa) for i in range(2)]
                SH = [sb(f"SH{i}", [128, 512], BF16, esa) for i in range(2)]
                R1 = E2[0][:].bitcast(F32)
                R2 = E2[1][:].bitcast(F32)
                ZZ = [pst(f"ZZ{i}", [128, 1024], F32, esa) for i in range(3)]
                Zv = [ZZ[j // 2][:, (j % 2) * 512:(j % 2 + 1) * 512] for j in range(6)]
                Z = Zv[0:4]
                D1 = Zv[4]
                D2 = Zv[5]
                O1 = pst("O1", [128, 512], F32, esa)
                O2 = pst("O2", [128, 512], F32, esa)
                OB = [O1, O2]
                win_v = win_d.rearrange("(c p) n -> p c n", p=128)

                fw.emit("pool", lambda e: e.memset(QA[0][64:128, :], 0.0), writes=[("QA", 0)])
                fw.emit("pool", lambda e: e.memset(QA[1][0:64, :], 0.0), writes=[("QA", 1)])

                for g in range(8):
                    is_sb = g < 4
                    if is_sb:
                        offs = (128 * g, 512 + 128 * g, 1024 + 128 * g)
                    else:
                        h = g - 4
                        offs = (1536 + 128 * h, 2048 + 128 * h, 2560 + 128 * h)
                    def load_wsl(gg):
                        o3 = (128 * gg, 512 + 128 * gg, 1024 + 128 * gg) if gg < 4 else (1536 + 128 * (gg - 4), 2048 + 128 * (gg - 4), 2560 + 128 * (gg - 4))
                        for j in range(3):
                            fw.emit("pool", lambda e, j=j, o=o3[j]: e.dma_start(out=wsl[:, :, j, :], in_=win_v[:, :, o:o + 128]),
                                    writes=[("wsl", j)], dma="wsl%d" % j)
                    if g == 0:
                        load_wsl(0)
                    if g == 4:
                        for m2 in range(2):
                            fw.emit("pool", lambda e, m2=m2: e.memset(QA[m2][64:128, :], 0.0), writes=[("QA", m2)])
                            fw.emit("pool", lambda e, m2=m2: e.memset(KA[m2][64:128, :], 0.0), writes=[("KA", m2)])
                    if not is_sb:
                        for m2 in range(2):
                            fw.emit("pool", lambda e, m2=m2, h=h: e.dma_start(out=QA[m2][64:67, :], in_=aug_d[h, 0, :, :]), writes=[("QA", m2)], dma="augq%d" % m2)
                            fw.emit("pool", lambda e, m2=m2, h=h: e.dma_start(out=KA[m2][64:67, :], in_=aug_d[h, 1, :, :]), writes=[("KA", m2)], dma="augk%d" % m2)
                    def sb_inproj_pieces(tb):
                        cols = slice(tb * 512, (tb + 1) * 512)
                        pk = ("OB", 1)
                        pieces = []

                        def mmK(c_lo, c_hi, j):
                            def f():
                                for c in range(c_lo, c_hi):
                                    fw.emit("pe", lambda e, c=c: e.matmul(O2[:, :], lhsT=wsl[:, c, j, :], rhs=hT[:, c, cols], start=(c == 0), stop=(c == NCH - 1)),
                                            reads=[("wsl", j)], writes=[pk])
                            return f

                        def evK():
                            fw.emit("dve", lambda e: e.tensor_copy(out=KA[0][:, cols], in_=O2[:, :]), reads=[pk], writes=[("KA", 0)])

                        def evQ():
                            fw.emit("dve", lambda e: e.tensor_scalar(out=QA[0][0:64, cols], in0=O2[0:64, :], scalar1=float(SCALE), scalar2=None, op0=ALU.mult),
                                    reads=[pk], writes=[("QA", 0)])
                            fw.emit("dve", lambda e: e.tensor_scalar(out=QA[1][64:128, cols], in0=O2[64:128, :], scalar1=float(SCALE), scalar2=None, op0=ALU.mult),
                                    reads=[pk], writes=[("QA", 1)])

                        def mmV(q):
                            def f():
                                tt = tb * 4 + q
                                for c in range(NCH):
                                    fw.emit("pe", lambda e, c=c: e.matmul(O2[:, q * 128:(q + 1) * 128], lhsT=hT[:, c, tt * 128:(tt + 1) * 128],
                                                                         rhs=wsl[:, c, 2, :], start=(c == 0), stop=(c == NCH - 1)),
                                            reads=[("wsl", 2)], writes=[pk])
                            return f

                        def evV():
                            fw.emit("dve", lambda e: e.tensor_copy(out=V[:, tb * 4:(tb + 1) * 4, :].rearrange("p a b -> p (a b)"), in_=O2[:, :]),
                                    reads=[pk], writes=["V"])

                        def seq(*fs):
                            def f():
                                for x in fs:
                                    x()
                            return f
                        pieces.append(mmK(0, 4, 1))
                        pieces.append(seq(mmK(4, 8, 1), evK))
                        pieces.append(mmK(0, 4, 0))
                        pieces.append(seq(mmK(4, 8, 0), evQ))
                        pieces.append(mmV(0)); pieces.append(mmV(1)); pieces.append(mmV(2))
                        pieces.append(seq(mmV(3), evV))
                        return pieces

                    if is_sb:
                        for u in sb_inproj_pieces(0):
                            u()
                    if not is_sb:
                        inb = Zv
                        ib = 0
                        ish = 0
                        for j in (1, 0):
                            for tb in range(NQB):
                                cols = slice(tb * 512, (tb + 1) * 512)
                                if is_sb:
                                    ps = inb[ib % 6]; pk = ("zb", ib % 6); ib += 1
                                    for c in range(NCH):
                                        fw.emit("pe", lambda e, ps=ps, j=j, c=c, cols=cols: e.matmul(ps[:, :], lhsT=wsl[:, c, j, :], rhs=hT[:, c, cols],
                                                                                                    start=(c == 0), stop=(c == NCH - 1)),
                                                reads=[("wsl", j)], writes=[pk])
                                    if j == 0:
                                        fw.emit("dve", lambda e, ps=ps, cols=cols: e.tensor_scalar(out=QA[0][0:64, cols], in0=ps[0:64, :], scalar1=float(SCALE),
                                                                                                  scalar2=None, op0=ALU.mult),
                                                reads=[pk], writes=[("QA", 0)])
                                        fw.emit("dve", lambda e, ps=ps, cols=cols: e.tensor_scalar(out=QA[1][64:128, cols], in0=ps[64:128, :], scalar1=float(SCALE),
                                                                                                  scalar2=None, op0=ALU.mult),
                                                reads=[pk], writes=[("QA", 1)])
                                    else:
                                        fw.emit("act", lambda e, ps=ps, cols=cols: e.activation(out=KA[0][:, cols], in_=ps[:, :], func=AF.Copy),
                                                reads=[pk], writes=[("KA", 0)])
                                else:
                                    ps = inb[ib % 6]; pk = ("zb", ib % 6); ib += 1
                                    for c in range(NCH):
                                        fw.emit("pe", lambda e, ps=ps, j=j, c=c, cols=cols: e.matmul(ps[:, :], lhsT=wsl[:, c, j, :], rhs=hT[:, c, cols],
                                                                                                    start=(c == 0), stop=(c == NCH - 1)),
                                                reads=[("wsl", j)], writes=[pk])
                                    dst = QA if j == 0 else KA
                                    dkey = "QA" if j == 0 else "KA"
                                    sh = SH[ish % 2]; shk = ("SH", ish % 2); ish += 1
                                    if j == 0:
                                        fw.emit("dve", lambda e, ps=ps, cols=cols: e.tensor_scalar(out=QA[0][0:64, cols], in0=ps[0:64, :], scalar1=float(SCALE),
                                                                                                  scalar2=None, op0=ALU.mult),
                                                reads=[pk], writes=[("QA", 0)])
                                        fw.emit("dve", lambda e, ps=ps, sh=sh: e.tensor_scalar(out=sh[64:128, :], in0=ps[64:128, :], scalar1=float(SCALE),
                                                                                              scalar2=None, op0=ALU.mult),
                                                reads=[pk], writes=[shk])
                                    else:
                                        fw.emit("act", lambda e, ps=ps, cols=cols: e.activation(out=KA[0][0:64, cols], in_=ps[0:64, :], func=AF.Copy),
                                                reads=[pk], writes=[("KA", 0)])
                                        fw.emit("act", lambda e, ps=ps, sh=sh: e.activation(out=sh[64:128, :], in_=ps[64:128, :], func=AF.Copy),
                                                reads=[pk], writes=[shk])
                                    fw.emit("sp", lambda e, dst=dst, sh=sh, cols=cols: e.dma_start(out=dst[1][0:64, cols], in_=sh[64:128, :]),
                                            reads=[shk], writes=[(dkey, 1)], dma="sh%d" % ((ish - 1) % 2))
                        for t4 in range(NT // 4):
                            ps = inb[ib % 6]; pk = ("zb", ib % 6); ib += 1
                            for q in range(4):
                                tt = t4 * 4 + q
                                for c in range(NCH):
                                    fw.emit("pe", lambda e, ps=ps, q=q, c=c, tt=tt: e.matmul(ps[:, q * 128:(q + 1) * 128], lhsT=hT[:, c, tt * 128:(tt + 1) * 128],
                                                                                            rhs=wsl[:, c, 2, :], start=(c == 0), stop=(c == NCH - 1)),
                                            reads=[("wsl", 2)], writes=[pk])
                            if t4 % 2 == 0:
                                fw.emit("dve", lambda e, ps=ps, t4=t4: e.tensor_copy(out=V[:, t4 * 4:(t4 + 1) * 4, :].rearrange("p a b -> p (a b)"), in_=ps[:, :]),
                                        reads=[pk], writes=["V"])
                            else:
                                fw.emit("act", lambda e, ps=ps, t4=t4: e.activation(out=V[:, t4 * 4:(t4 + 1) * 4, :].rearrange("p a b -> p (a b)"), in_=ps[:, :], func=AF.Copy),
                                        reads=[pk], writes=["V"])
                    if not is_sb and g + 1 < 8:
                        load_wsl(g + 1)
                    zk = [("zb", 0), ("zb", 1), ("zb", 2), ("zb", 3)]

                    if is_sb:
                        tasks = []
                        for qb in range(NQB):
                            for hh in range(2):
                                nkb = 4 * (qb + 1)
                                kbs = list(reversed(range(nkb)))
                                for n in range(nkb // 2):
                                    kA, kB = kbs[2 * n], kbs[2 * n + 1]
                                    tasks.append(dict(qb=qb, hh=hh, kA=kA, kB=kB, first=(n == 0), last=(n == nkb // 2 - 1),
                                                      lA=kA - 4 * qb, lB=kB - 4 * qb))
                        NTK = len(tasks)

                        def pairv(tile, c0):
                            if c0 == 0:
                                return tile[:, :]
                            return tile[:, :].rearrange("p (h x) -> p h x", h=2)[:, :, c0:512]

                        def sb_s1(i):
                            t = tasks[i]
                            hh = t["hh"]; q0 = 512 * t["qb"]
                            c0 = 128 * max(t["lB"], 0)
                            zz = ZZ[i % 3]
                            zkA, zkB = ("zb", 2 * (i % 3)), ("zb", 2 * (i % 3) + 1)
                            for half, kb, zkk in ((0, t["kA"], zkA), (1, t["kB"], zkB)):
                                fw.emit("pe", lambda e, half=half, kb=kb: e.matmul(zz[:, half * 512 + c0:(half + 1) * 512], lhsT=KA[0][:, kb * 128:(kb + 1) * 128],
                                                                                  rhs=QA[hh][:, q0 + c0:q0 + 512], start=True, stop=True),
                                        reads=[("KA", 0), ("QA", hh)], writes=[zkk])
                            if t["lA"] >= 0:
                                fw.emit("pe", lambda e: e.matmul(zz[:, c0:c0 + 256], lhsT=ident, rhs=cb[:, C_FULL:C_FULL + 256], start=False, stop=True, skip_group_check=True),
                                        reads=["cb"], writes=[zkA])
                            if t["lB"] >= 0:
                                fw.emit("pe", lambda e: e.matmul(zz[:, 512 + c0:512 + c0 + 128], lhsT=ident, rhs=cb[:, C_TRI:C_TRI + 128], start=False, stop=True, skip_group_check=True),
                                        reads=["cb"], writes=[zkB])
                            fw.emit("act", lambda e: e.activation(out=pairv(E2[i % 2], c0), in_=pairv(zz, c0), func=AF.Exp),
                                    reads=[zkA, zkB], writes=[("E", i % 2)])
                            fw.emit("act", lambda e: e.activation(out=pairv(SP2[i % 2], c0), in_=pairv(E2[i % 2], c0), func=AF.Ln, bias=1.0),
                                    reads=[("E", i % 2)], writes=[("SP", i % 2)])

                        def sb_s2(i):
                            t = tasks[i]
                            c0 = 128 * max(t["lB"], 0)
                            zz = ZZ[i % 3]
                            sp = SP2[i % 2]
                            zkA, zkB = ("zb", 2 * (i % 3)), ("zb", 2 * (i % 3) + 1)
                            fw.emit("pe", lambda e: e.matmul(zz[:, c0:512], lhsT=negU, rhs=sp[:, c0:512], start=False, stop=True, skip_group_check=True),
                                    reads=[("SP", i % 2), "cb"], writes=[zkA])
                            if not t["first"]:
                                fw.emit("pe", lambda e: e.matmul(zz[:, c0:512], lhsT=negones, rhs=SC[1][:, c0:512], start=False, stop=True, skip_group_check=True),
                                        reads=[("SC", 1), "cb"], writes=[zkA])
                            if t["first"]:
                                fw.emit("pool", lambda e: e.memset(SC[0][:, 0:256], 0.0), writes=[("SC", 0)])
                                fw.emit("pool", lambda e: e.memset(SC[1][:, 0:256], 0.0), writes=[("SC", 1)])
                                fw.emit("dve", lambda e: e.tensor_copy(out=SC[0][:, c0:512], in_=sp[:, c0:512]),
                                        reads=[("SP", i % 2)], writes=[("SC", 0)])
                            else:
                                fw.emit("dve", lambda e: e.tensor_tensor(out=SC[0][:, c0:512], in0=SC[1][:, c0:512], in1=sp[:, c0:512], op=ALU.add),
                                        reads=[("SP", i % 2), ("SC", 1)], writes=[("SC", 0)])
                            fw.emit("pe", lambda e: e.matmul(zz[:, 512 + c0:1024], lhsT=negU, rhs=sp[:, 512 + c0:1024], start=False, stop=True, skip_group_check=True),
                                    reads=[("SP", i % 2), "cb"], writes=[zkB])
                            fw.emit("pe", lambda e: e.matmul(zz[:, 512 + c0:1024], lhsT=negones, rhs=SC[0][:, c0:512], start=False, stop=True, skip_group_check=True),
                                    reads=[("SC", 0), "cb"], writes=[zkB])
                            if not t["last"]:
                                fw.emit("dve", lambda e: e.tensor_tensor(out=SC[1][:, c0:512], in0=SC[0][:, c0:512], in1=sp[:, 512 + c0:1024], op=ALU.add),
                                        reads=[("SP", i % 2), ("SC", 0)], writes=[("SC", 1)])
                            fw.emit("act", lambda e: e.activation(out=pairv(W2[i % 3], c0), in_=pairv(zz, c0), func=AF.Exp),
                                    reads=[zkA, zkB], writes=[("W", i % 3)])

                        def sb_s3(i):
                            t = tasks[i]
                            hh = t["hh"]; qb = t["qb"]
                            c0 = 128 * max(t["lB"], 0)
                            for half, kb in ((0, t["kA"]), (1, t["kB"])):
                                fw.emit("pe", lambda e, half=half, kb=kb: e.matmul(O1[:, c0:512], lhsT=V[:, kb, :], rhs=W2[i % 3][:, half * 512 + c0:(half + 1) * 512],
                                                                                  start=(t["first"] and half == 0), stop=(t["last"] and half == 1), skip_group_check=True),
                                        reads=[("W", i % 3), "V"], writes=[("OB", 0)])
                            if t["last"]:
                                hb = 64 * hh
                                fw.emit("dve", lambda e: e.tensor_copy(out=oT[hb:hb + 64, g, qb * 512:(qb + 1) * 512], in_=O1[hb:hb + 64, :]),
                                        reads=[("OB", 0)], writes=[("oT", g, qb, hh)])

                        pending = []
                        last_qb = -1
                        for i in range(NTK + 2):
                            if i < NTK:
                                if tasks[i]["qb"] != last_qb:
                                    last_qb = tasks[i]["qb"]
                                    assert not pending
                                    if last_qb + 1 < NQB:
                                        pending = sb_inproj_pieces(last_qb + 1)
                                sb_s1(i)
                            if 0 <= i - 1 < NTK:
                                sb_s2(i - 1)
                            if 0 <= i - 2 < NTK:
                                sb_s3(i - 2)
                            if i < NTK:
                                for _ in range(2 if tasks[i]["qb"] == 0 else 1):
                                    if pending:
                                        pending.pop(0)()
                                        if not pending and tasks[i]["qb"] == NQB - 2 and g + 1 < 8:
                                            load_wsl(g + 1)
                    else:
                        h = g - 4
                        tasks = []
                        for qb in range(NQB):
                            for c in range(2):
                                nkb = 4 * (qb + 1)
                                for kb in range(nkb):
                                    tasks.append(dict(qb=qb, c=c, kb=kb, first=(kb == 0), last=(kb == nkb - 1), kbl=kb - 4 * qb))
                        NTK = len(tasks)

                        def df_s1(i):
                            t = tasks[i]
                            m2 = t["c"]; q0 = 512 * t["qb"]; kb = t["kb"]
                            c0 = 128 * max(t["kbl"], 0)
                            z = Z[i % 4]
                            fw.emit("pe", lambda e: e.matmul(z[:, c0:512], lhsT=KA[m2][:, kb * 128:(kb + 1) * 128], rhs=QA[m2][:, q0 + c0:q0 + 512],
                                                             start=True, stop=True),
                                    reads=[("KA", m2), ("QA", m2)], writes=[zk[i % 4]])
                            if t["kbl"] >= 0:
                                fw.emit("pe", lambda e: e.matmul(z[:, c0:c0 + 128], lhsT=ident, rhs=cb[:, C_TRI2:C_TRI2 + 128], start=False, stop=True, skip_group_check=True),
                                        reads=["cb"], writes=[zk[i % 4]])
                            cblk = -SLOPES[h] * 128.0 * (4 * t["qb"] - kb)
                            fw.emit("act", lambda e: e.activation(out=W[i % 3][:, c0:512], in_=z[:, c0:512], func=AF.Exp, bias=float(cblk)),
                                    reads=[zk[i % 4]], writes=[("W", i % 3)])

                        def df_s2(i):
                            t = tasks[i]
                            kb = t["kb"]; qb = t["qb"]; m2 = t["c"]
                            c0 = 128 * max(t["kbl"], 0)
                            OO = OB[m2]
                            DD = D1 if m2 == 0 else D2
                            fw.emit("pe", lambda e: e.matmul(OO[:, c0:512], lhsT=V[:, kb, :], rhs=W[i % 3][:, c0:512], start=t["first"], stop=t["last"]),
                                    reads=[("W", i % 3), "V"], writes=[("OB", m2)])
                            fw.emit("pe", lambda e: e.matmul(DD[:, c0:512], lhsT=ones, rhs=W[i % 3][:, c0:512], start=t["first"], stop=t["last"]),
                                    reads=[("W", i % 3), "cb"], writes=[("zb", 4 + m2)])
                            if t["last"] and m2 == 1:
                                fw.emit("act", lambda e: e.activation(out=OS[0][:], in_=O1[:, :], func=AF.Copy), reads=[("OB", 0)], writes=[("OS", 0)])
                                fw.emit("dve", lambda e: e.reciprocal(out=R1, in_=D1), reads=[("zb", 4)], writes=[("E", 0)])
                                fw.emit("act", lambda e: e.activation(out=OS[1][:], in_=O2[:, :], func=AF.Copy), reads=[("OB", 1)], writes=[("OS", 1)])
                                fw.emit("dve", lambda e: e.reciprocal(out=R2, in_=D2), reads=[("zb", 5)], writes=[("E", 1)])
                                fw.emit("pool", lambda e: e.tensor_tensor(out=R1, in0=OS[0][:], in1=R1, op=ALU.mult),
                                        reads=[("OS", 0), ("E", 0)], writes=[("E", 0)])
                                fw.emit("dve", lambda e: e.scalar_tensor_tensor(out=R2, in0=R2, scalar=neglam[:, 0:1], in1=OS[1][:], op0=ALU.mult, op1=ALU.mult),
                                        reads=[("OS", 1), ("E", 1), "neglam"], writes=[("E", 1)])
                                fw.emit("pool", lambda e: e.tensor_tensor(out=oT[:, g, qb * 512:(qb + 1) * 512], in0=R1, in1=R2, op=ALU.add),
                                        reads=[("E", 0), ("E", 1)], writes=[("oT", g, qb, 0)])

                        for i in range(NTK + 2):
                            if i < NTK:
                                df_s1(i)
                            if 0 <= i - 2 < NTK:
                                df_s2(i - 2)
                if dbg:
                    fw.emit("sp", lambda e: e.dma_start(out=dbg_o[:, :, :], in_=oT[:]), reads=[("oT", g, qb, hh) for g in range(8) for qb in range(NQB) for hh in range(2)], dma="dbgo")
                if stop == 1:
                    fw.flush("pa", final_waits=[(fw.sems["dbgo"], 16)])
                    return nc
                fw.flush("pa", final_waits=[(fw.sems["dbgo"], 16)] if dbg else ())

        with ExitStack() as esb:
            X1 = sb("X1", [128, TPC, D], F32, esb)
            wmisc = sb("wmisc", [128, NCH, D], BF16, esb)
            wpp = sb("wpp", [128, 2, D], BF16, esb)
            wg = [sb(f"wg{i}", [128, NCH, DEXP], BF16, esb) for i in range(2)]
            wu = [sb(f"wu{i}", [128, NCH, DEXP], BF16, esb) for i in range(2)]
            wd = [sb(f"wd{i}", [128, 4, D], BF16, esb) for i in range(2)]
            hid = [sb("hid0", [128, 4, 512], BF16, esb)] * 2
            sg = [sb(f"sg{i}", [128, 512], BF16, esb) for i in range(2)]
            xn32 = sb("xn32", [128, D], F32, esb)
            h32 = sb("h32", [128, NCH, 128], F32, esb)
            wr32 = sb("wr32", [128, NCH, 20], F32, esb)
            brb = sb("brb", [128, 20], F32, esb)
            gfb = sb("gfb", [128, D], F32, esb)
            sq2 = sb("sq2", [128, 1024], BF16, esb)
            sq = [sq2[:, 0:512], sq2[:, 512:1024]]
            sqf = sq2[:].bitcast(F32)
            rs_t = sb("rs", [128, 512], F32, esb)
            rs = rs_t[:]
            junkb = sb("junkb", [128, D], BF16, esb)
            st = sb("stat", [128, 3, TPC, 4], F32, esb)
            lg = sb("lg", [128, TPC, 20], F32, esb)
            rt = sb("rt", [128, TPC, 64], F32, esb)
            gates = sb("gates", [128, TPC, NEXP], F32, esb)
            pl = [sb(f"pl{i}", [128, PLE], F32, esb) for i in range(2)]
            plb = sb("plb", [128, PLE], BF16, esb)
            pT = sb("pT", [128, 2, 128], BF16, esb)
            PB = [pst(f"PB{i}", [128, 512], F32, esb) for i in range(8)]

            wout_v = wout_d.rearrange("(c p) n -> p c n", p=128)
            wpg_v = wpg_d.rearrange("(c p) n -> p c n", p=128)
            wpp_v = wpp_d.rearrange("(c p) n -> p c n", p=128)

            fw.emit("sp", lambda e: e.dma_start(out=wr32[:], in_=wr_d.rearrange("(c p) n -> p c n", p=128)), writes=["wr32"], dma="wr32")
            fw.emit("sp", lambda e: e.dma_start(out=brb[:], in_=br_d.partition_broadcast(128)), writes=["brb"], dma="brb")
            fw.emit("sp", lambda e: e.dma_start(out=gfb[:], in_=gfin_d.partition_broadcast(128)), writes=["gfb"], dma="gfb")
            fw.emit("pool", lambda e: e.dma_start(out=wpp[:], in_=wpp_v), writes=["wpp"], dma="wpp")

            def load_expert(e_idx, b):
                for c in range(0, NCH, 4):
                    fw.emit("pool", lambda e, c=c: e.dma_start(out=wg[b][:, c:c + 4, :], in_=weg_d[e_idx].rearrange("(c p) n -> p c n", p=128)[:, c:c + 4, :]),
                            writes=[("wg", b, c)], dma="wg%d_%d" % (b, c))
                    fw.emit("pool", lambda e, c=c: e.dma_start(out=wu[b][:, c:c + 4, :], in_=weu_d[e_idx].rearrange("(c p) n -> p c n", p=128)[:, c:c + 4, :]),
                            writes=[("wu", b, c)], dma="wu%d_%d" % (b, c))
                for c in range(0, 4, 2):
                    fw.emit("pool", lambda e, c=c: e.dma_start(out=wd[b][:, c:c + 2, :], in_=wed_d[e_idx].rearrange("(c p) n -> p c n", p=128)[:, c:c + 2, :]),
                            writes=[("wd", b, c)], dma="wd%d_%d" % (b, c))

            def rstd_batch(kind):
                fw.emit("act", lambda e: e.activation(out=st[:, kind, :, 1], in_=st[:, kind, :, 0], func=AF.Ln, scale=1.0 / D, bias=EPS),
                        reads=[("st", kind, t) for t in range(TPC)], writes=[("stln", kind)])
                fw.emit("act", lambda e: e.activation(out=st[:, kind, :, 2], in_=st[:, kind, :, 1], func=AF.Exp, scale=-0.5),
                        reads=[("stln", kind)], writes=[("strs", kind)])

            def norm_T(kind, t, gk, tok0, want32):
                fw.emit("dve", lambda e: e.tensor_scalar(out=xn32[:], in0=X1[:, t, :], scalar1=st[:, kind, t, 2:3], scalar2=None, op0=ALU.mult),
                        reads=[("X1", t), ("strs", kind)], writes=["xn32"])
                for c in range(NCH):
                    bk = 2 + c // 4
                    fw.emit("pe", lambda e, c=c, bk=bk: e.transpose(out=PB[bk][:, (c % 4) * 128:(c % 4 + 1) * 128], in_=xn32[:, c * 128:(c + 1) * 128], identity=identf[:]),
                            reads=["xn32", "identf"], writes=[("PB", bk)])
                for k2 in range(2):
                    bk = 2 + k2
                    src = PB[bk][:, :].rearrange("p (c t) -> p c t", c=4)
                    gb = gcols[:, gk, 4 * k2:4 * k2 + 4].unsqueeze(2).to_broadcast([128, 4, 128])
                    if want32:
                        fw.emit("dve", lambda e, k2=k2, src=src, gb=gb: e.tensor_tensor(out=h32[:, 4 * k2:4 * k2 + 4, :], in0=src, in1=gb, op=ALU.mult),
                                reads=[("PB", bk), ("gcols", gk)], writes=[("h32", 4 * k2 + cc) for cc in range(4)])
                    else:
                        fw.emit("dve", lambda e, k2=k2, src=src, gb=gb: e.tensor_tensor(out=oT[:, 4 * k2:4 * k2 + 4, tok0:tok0 + 128], in0=src, in1=gb, op=ALU.mult),
                                reads=[("PB", bk), ("gcols", gk)], writes=[("oTt", tok0)])
                if want32:
                    fw.emit("dve", lambda e: e.tensor_copy(out=oT[:, :, tok0:tok0 + 128], in_=h32[:]),
                            reads=[("h32", c) for c in range(NCH)], writes=[("oTt", tok0)])

            def onorm(t0):
                for blk in range(CH // 512):
                    c0 = t0 + blk * 512
                    for grp in range(5):
                        chunks = [0, 1, 2, 3] if grp == 0 else [3 + grp]
                        nfeat = 512.0 if grp == 0 else 128.0
                        for n, c in enumerate(chunks):
                            sqb = sq[n % 2]
                            fw.emit("act", lambda e, c=c, sqb=sqb: e.activation(out=sqb, in_=oT[:, c, c0:c0 + 512], func=AF.Square),
                                    reads=[("oTb", c, c0)], writes=[("sq", n % 2)])
                            fw.emit("pe", lambda e, sqb=sqb, n=n: e.matmul(PB[7][:, :], lhsT=ones, rhs=sqb, start=(n == 0), stop=(n == len(chunks) - 1)),
                                    reads=[("sq", n % 2), "cb"], writes=[("PB", 7)])
                        fw.emit("act", lambda e, nfeat=nfeat: e.activation(out=rs, in_=PB[7][:, :], func=AF.Ln, scale=1.0 / nfeat, bias=EPS),
                                reads=[("PB", 7)], writes=["rs"])
                        fw.emit("act", lambda e: e.activation(out=rs, in_=rs, func=AF.Exp, scale=-0.5), reads=["rs"], writes=["rs"])
                        for c in chunks:
                            gcol = gcols[:, 3, c:c + 1] if grp == 0 else gcols[:, 4, 0:1]
                            fw.emit("dve", lambda e, c=c, gcol=gcol: e.scalar_tensor_tensor(out=oT[:, c, c0:c0 + 512], in0=oT[:, c, c0:c0 + 512], scalar=gcol, in1=rs,
                                                                                           op0=ALU.mult, op1=ALU.mult),
                                    reads=["rs", ("oTb", c, c0), ("gcols", 3), ("gcols", 4)], writes=[("oTb", c, c0)])

            for ck in range(NCK):
                t0 = ck * CH
                for hc in range(2):
                    fw.emit("pool", lambda e, hc=hc: e.dma_start(out=wmisc[:, 4 * hc:4 * hc + 4, :], in_=wout_v[:, 4 * hc:4 * hc + 4, :]),
                            writes=[("wmisc", hc)], dma="wmisc%d" % hc)
                load_expert(0, 0)
                if ck == 0:
                    onorm(t0)
                for t in range(TPC):
                    tok0 = t0 + t * 128
                    c0 = t0 + (t // 4) * 512
                    b = t % 2
                    fw.emit("sp", lambda e, t=t, tok0=tok0: e.dma_start(out=X1[:, t, :], in_=x_d[tok0:tok0 + 128, :]), writes=[("X1", t)], dma="xl%d" % t)
                    for nh in range(2):
                        bk = 2 * (t % 2) + nh
                        for c in range(NCH):
                            fw.emit("pe", lambda e, nh=nh, c=c, tok0=tok0, bk=bk: e.matmul(PB[bk][:, :], lhsT=oT[:, c, tok0:tok0 + 128], rhs=wmisc[:, c, nh * 512:(nh + 1) * 512],
                                                                                          start=(c == 0), stop=(c == NCH - 1)),
                                    reads=[("oTb", c, c0), ("wmisc", c // 4), ("oTt", tok0)], writes=[("PB", bk)])
                        fw.emit("dve", lambda e, nh=nh, t=t, b=b, bk=bk: e.tensor_tensor(out=X1[:, t, nh * 512:(nh + 1) * 512], in0=PB[bk][:, :], in1=X1[:, t, nh * 512:(nh + 1) * 512], op=ALU.add),
                                reads=[("PB", bk), ("X1", t)], writes=[("X1", t)])
                    fw.emit("act", lambda e, t=t: e.activation(out=junkb[:], in_=X1[:, t, :], func=AF.Square, accum_out=st[:, 0, t, 0:1]),
                            reads=[("X1", t)], writes=["junkb", ("st", 0, t)])
                    if dbg:
                        fw.emit("sp", lambda e, t=t, tok0=tok0: e.dma_start(out=dbg_x1[tok0:tok0 + 128, :], in_=X1[:, t, :]), reads=[("X1", t)], dma="dbgx%d" % t)
                rstd_batch(0)
                for t in range(TPC):
                    tok0 = t0 + t * 128
                    norm_T(0, t, 1, tok0, True)
                    for c in range(NCH):
                        fw.emit("pe", lambda e, c=c: e.matmul(PB[7][:, 0:20], lhsT=h32[:, c, :], rhs=wr32[:, c, :], start=(c == 0), stop=(c == NCH - 1)),
                                reads=[("h32", cc) for cc in range(NCH)] + ["wr32"], writes=[("PB", 7)])
                    fw.emit("dve", lambda e, t=t: e.tensor_tensor(out=lg[:, t, :], in0=PB[7][:, 0:20], in1=brb[:], op=ALU.add),
                            reads=[("PB", 7), "brb"], writes=["lg"])
                G = lg[:, :, 0:4]
                EL = lg[:, :, 4:20]
                gmax = rt[:, :, 0:1]
                goh = rt[:, :, 1:5]
                gex = rt[:, :, 5:9]
                gsum = rt[:, :, 9:10]
                gw = rt[:, :, 10:11]
                msk = rt[:, :, 16:32]
                m1 = rt[:, :, 11:12]
                m2 = rt[:, :, 12:13]
                oh1 = rt[:, :, 32:48]
                oh2 = rt[:, :, 48:64]
                dlt = rt[:, :, 13:14]
                w1 = rt[:, :, 14:15]
                w2 = rt[:, :, 15:16]
                BIG = 1.0e4

                def dv(fn, r=("lg", "rt"), w=("rt",)):
                    fw.emit("dve", fn, reads=list(r), writes=list(w))

                dv(lambda e: e.tensor_reduce(out=gmax, in_=G, axis=AX.X, op=ALU.max))
                dv(lambda e: e.tensor_tensor(out=goh, in0=G, in1=gmax.to_broadcast([128, TPC, 4]), op=ALU.is_equal))
                dv(lambda e: e.tensor_tensor(out=gex, in0=G, in1=gmax.to_broadcast([128, TPC, 4]), op=ALU.subtract))
                fw.emit("act", lambda e: e.activation(out=gex, in_=gex, func=AF.Exp), reads=["rt"], writes=["rt"])
                dv(lambda e: e.tensor_reduce(out=gsum, in_=gex, axis=AX.X, op=ALU.add))
                dv(lambda e: e.reciprocal(out=gw, in_=gsum))
                dv(lambda e: e.tensor_scalar(out=gex, in0=goh, scalar1=-1.0, scalar2=BIG, op0=ALU.add, op1=ALU.mult))
                dv(lambda e: e.tensor_tensor(out=msk.rearrange("p t (g k) -> p t g k", k=4), in0=EL.rearrange("p t (g k) -> p t g k", k=4),
                                             in1=gex.unsqueeze(3).to_broadcast([128, TPC, 4, 4]), op=ALU.add))
                dv(lambda e: e.tensor_reduce(out=m1, in_=msk, axis=AX.X, op=ALU.max))
                dv(lambda e: e.tensor_tensor(out=oh1, in0=msk, in1=m1.to_broadcast([128, TPC, 16]), op=ALU.is_equal))
                dv(lambda e: e.scalar_tensor_tensor(out=msk, in0=oh1, scalar=-BIG, in1=msk, op0=ALU.mult, op1=ALU.add))
                dv(lambda e: e.tensor_reduce(out=m2, in_=msk, axis=AX.X, op=ALU.max))
                dv(lambda e: e.tensor_tensor(out=oh2, in0=msk, in1=m2.to_broadcast([128, TPC, 16]), op=ALU.is_equal))
                dv(lambda e: e.tensor_tensor(out=dlt, in0=m2, in1=m1, op=ALU.subtract))
                fw.emit("act", lambda e: e.activation(out=dlt, in_=dlt, func=AF.Exp), reads=["rt"], writes=["rt"])
                dv(lambda e: e.tensor_scalar(out=w1, in0=dlt, scalar1=1.0, scalar2=None, op0=ALU.add))
                dv(lambda e: e.reciprocal(out=w1, in_=w1))
                dv(lambda e: e.tensor_tensor(out=w2, in0=dlt, in1=w1, op=ALU.mult))
                dv(lambda e: e.tensor_tensor(out=w1, in0=w1, in1=gw, op=ALU.mult))
                dv(lambda e: e.tensor_tensor(out=w2, in0=w2, in1=gw, op=ALU.mult))
                dv(lambda e: e.tensor_tensor(out=oh1, in0=oh1, in1=w1.to_broadcast([128, TPC, 16]), op=ALU.mult))
                dv(lambda e: e.tensor_tensor(out=oh2, in0=oh2, in1=w2.to_broadcast([128, TPC, 16]), op=ALU.mult))
                dv(lambda e: e.tensor_tensor(out=gates[:], in0=oh1, in1=oh2, op=ALU.add), w=("gates",))

                for ex in range(NEXP):
                    b = ex % 2
                    if ex + 1 < NEXP:
                        load_expert(ex + 1, (ex + 1) % 2)
                    if ex == 4 and ck + 1 < NCK:
                        onorm(t0 + CH)
                    for tb in range(CH // 512):
                        hb_ = hid[tb % 2]
                        c0 = t0 + tb * 512
                        for jc in range(4):
                            gp = PB[jc % 2]; up = PB[2 + jc % 2]
                            for c in range(NCH):
                                fw.emit("pe", lambda e, gp=gp, c=c, jc=jc: e.matmul(gp[:, :], lhsT=wg[b][:, c, jc * 128:(jc + 1) * 128], rhs=oT[:, c, c0:c0 + 512],
                                                                                   start=(c == 0), stop=(c == NCH - 1)),
                                        reads=[("wg", b, 4 * (c // 4))] + [("oTt", c0 + 128 * q) for q in range(4)], writes=[("PB", jc % 2)])
                            for c in range(NCH):
                                fw.emit("pe", lambda e, up=up, c=c, jc=jc: e.matmul(up[:, :], lhsT=wu[b][:, c, jc * 128:(jc + 1) * 128], rhs=oT[:, c, c0:c0 + 512],
                                                                                   start=(c == 0), stop=(c == NCH - 1)),
                                        reads=[("wu", b, 4 * (c // 4))] + [("oTt", c0 + 128 * q) for q in range(4)], writes=[("PB", 2 + jc % 2)])
                            fw.emit("act", lambda e, gp=gp, jc=jc: e.activation(out=sg[jc % 2][:], in_=gp[:, :], func=AF.Silu),
                                    reads=[("PB", jc % 2)], writes=[("sg", jc % 2)])
                            fw.emit("dve", lambda e, up=up, jc=jc, hb_=hb_: e.tensor_tensor(out=hb_[:, jc, :], in0=up[:, :], in1=sg[jc % 2][:], op=ALU.mult),
                                    reads=[("PB", 2 + jc % 2), ("sg", jc % 2)], writes=[("hid", tb % 2, jc)])
                        for q in range(4):
                            t = tb * 4 + q
                            for nh in range(2):
                                yp = PB[4 + (2 * q + nh) % 3]
                                yk = ("PB", 4 + (2 * q + nh) % 3)
                                for jc in range(4):
                                    fw.emit("pe", lambda e, yp=yp, jc=jc, q=q, nh=nh, hb_=hb_: e.matmul(yp[:, :], lhsT=hb_[:, jc, q * 128:(q + 1) * 128],
                                                                                                       rhs=wd[b][:, jc, nh * 512:(nh + 1) * 512], start=(jc == 0), stop=(jc == 3)),
                                            reads=[("hid", tb % 2, jc), ("wd", b, 2 * (jc // 2))], writes=[yk])
                                fw.emit("dve", lambda e, yp=yp, t=t, nh=nh: e.scalar_tensor_tensor(out=X1[:, t, nh * 512:(nh + 1) * 512], in0=yp[:, :], scalar=gates[:, t, ex:ex + 1],
                                                                                                  in1=X1[:, t, nh * 512:(nh + 1) * 512], op0=ALU.mult, op1=ALU.add),
                                        reads=[yk, "gates", ("X1", t)], writes=[("X1", t)])
                for hc in range(2):
                    fw.emit("pool", lambda e, hc=hc: e.dma_start(out=wmisc[:, 4 * hc:4 * hc + 4, :], in_=wpg_v[:, 4 * hc:4 * hc + 4, :]),
                            writes=[("wmisc", hc)], dma="wmisc%d" % hc)
                for t in range(TPC):
                    fw.emit("act", lambda e, t=t: e.activation(out=junkb[:], in_=X1[:, t, :], func=AF.Square, accum_out=st[:, 1, t, 0:1]),
                            reads=[("X1", t)], writes=["junkb", ("st", 1, t)])
                rstd_batch(1)
                for t in range(TPC):
                    tok0 = t0 + t * 128
                    b = t % 2
                    norm_T(1, t, 2, tok0, False)
                    fw.emit("sp", lambda e, b=b, tok0=tok0: e.dma_start(out=pl[b][:], in_=p_d[tok0:tok0 + 128, :]), writes=[("pl", b)], dma="pl%d" % b)
                    fw.emit("pool", lambda e, b=b: e.tensor_copy(out=plb[:], in_=pl[b][:]), reads=[("pl", b)], writes=["plb"])
                    tpv = PB[6][:, 0:128].bitcast(BF16)
                    for c2 in range(2):
                        fw.emit("pe", lambda e, c2=c2, tpv=tpv: e.transpose(out=tpv[:, c2 * 128:(c2 + 1) * 128], in_=plb[:, c2 * 128:(c2 + 1) * 128], identity=ident),
                                reads=["plb", "cb"], writes=[("PB", 6)])
                    fw.emit("dve", lambda e, tpv=tpv: e.tensor_copy(out=pT[:].rearrange("p a b -> p (a b)"), in_=tpv), reads=[("PB", 6)], writes=["pT"])
                    for nh in range(2):
                        for c in range(NCH):
                            fw.emit("pe", lambda e, nh=nh, c=c, tok0=tok0: e.matmul(PB[nh][:, :], lhsT=oT[:, c, tok0:tok0 + 128], rhs=wmisc[:, c, nh * 512:(nh + 1) * 512],
                                                                                   start=(c == 0), stop=(c == NCH - 1)),
                                    reads=[("oTt", tok0), ("wmisc", c // 4)], writes=[("PB", nh)])
                        for c2 in range(2):
                            fw.emit("pe", lambda e, nh=nh, c2=c2: e.matmul(PB[4 + nh][:, :], lhsT=pT[:, c2, :], rhs=wpp[:, c2, nh * 512:(nh + 1) * 512],
                                                                          start=(c2 == 0), stop=(c2 == 1)),
                                    reads=["pT", "wpp"], writes=[("PB", 4 + nh)])
                        sgv = h32[:].rearrange("p a b -> p (a b)")[:, nh * 512:(nh + 1) * 512]
                        sgk = [("h32", 4 * nh + cc) for cc in range(4)]
                        fw.emit("act", lambda e, nh=nh, sgv=sgv: e.activation(out=sgv, in_=PB[nh][:, :], func=AF.Sigmoid),
                                reads=[("PB", nh)], writes=sgk)
                        tmpb = rs if nh == 0 else sqf
                        tmpk = ["rs"] if nh == 0 else [("sq", 0), ("sq", 1)]
                        fw.emit("dve", lambda e, nh=nh, tmpb=tmpb, sgv=sgv: e.tensor_tensor(out=tmpb, in0=PB[4 + nh][:, :], in1=sgv, op=ALU.mult),
                                reads=[("PB", 4 + nh)] + sgk, writes=tmpk)
                        fw.emit("pool", lambda e, nh=nh, t=t, tmpb=tmpb: e.tensor_tensor(out=X1[:, t, nh * 512:(nh + 1) * 512], in0=X1[:, t, nh * 512:(nh + 1) * 512],
                                                                                        in1=tmpb, op=ALU.add),
                                reads=tmpk + [("X1", t)], writes=[("X1", t)])
                    fw.emit("act", lambda e, t=t: e.activation(out=junkb[:], in_=X1[:, t, :], func=AF.Square, accum_out=st[:, 2, t, 0:1]),
                            reads=[("X1", t)], writes=["junkb", ("st", 2, t)])
                rstd_batch(2)
                for t in range(TPC):
                    tok0 = t0 + t * 128
                    b = t % 2
                    fw.emit("dve", lambda e, t=t, b=b: e.scalar_tensor_tensor(out=X1[:, t, :], in0=X1[:, t, :], scalar=st[:, 2, t, 2:3], in1=gfb[:], op0=ALU.mult, op1=ALU.mult),
                            reads=[("X1", t), ("strs", 2), "gfb"], writes=[("X1", t)])
                    fw.emit("sp", lambda e, t=t, tok0=tok0: e.dma_start(out=out_d[tok0:tok0 + 128, :], in_=X1[:, t, :]), reads=[("X1", t)], dma="out%d" % t)
            finals = [(fw.sems[n], fw.dma_cnt[n]) for n in fw.sems if n.startswith("out") or n.startswith("dbg")]
            fw.flush("pb", final_waits=finals)
    return nc


_NC_CACHE = {}


def _prep_inputs(inputs, S):
    f = lambda a: np.ascontiguousarray(np.asarray(a, dtype=np.float32))
    shared = {
        "cst": make_consts(),
        "aug": make_aug(S),
        "g_mix": f(inputs["g_mix"][0]),
        "w_in": f(inputs["w_in"][0]),
        "lam4": f(np.stack([inputs["lambda_q1"][0], inputs["lambda_k1"][0], inputs["lambda_q2"][0], inputs["lambda_k2"][0]], 0)),
        "g_sb_out": f(inputs["g_sb_out"][0]),
        "g_df_out": f(inputs["g_df_out"][0]),
        "w_out": f(inputs["w_out"][0]),
        "g_ffn": f(inputs["g_ffn"][0]),
        "w_router": f(np.concatenate([inputs["w_router_group"][0], inputs["w_router_expert"][0]], axis=1)),
        "b_router": f(np.concatenate([inputs["b_router_group"][0], inputs["b_router_expert"][0]], axis=0)),
        "w_expert_gate": f(inputs["w_expert_gate"][0]),
        "w_expert_up": f(inputs["w_expert_up"][0]),
        "w_expert_down": f(inputs["w_expert_down"][0]),
        "g_ple": f(inputs["g_ple"][0]),
        "w_ple_gate": f(inputs["w_ple_gate"][0]),
        "w_ple_proj": f(inputs["w_ple_proj"][0]),
        "g_final": f(inputs["g_final"]),
    }
    return shared


def kernel(**inputs):
    x = np.asarray(inputs["x"], dtype=np.float32)
    p = np.asarray(inputs["p"], dtype=np.float32)
    B, S, _ = x.shape
    if S not in _NC_CACHE:
        _NC_CACHE[S] = build(S)
    nc = _NC_CACHE[S]
    shared = _prep_inputs(inputs, S)
    in_maps = []
    for b in range(B):
        m = dict(shared)
        m["x"] = np.ascontiguousarray(x[b])
        m["p"] = np.ascontiguousarray(p[0, b])
        in_maps.append(m)
    res = run_bass_kernel_spmd(nc, in_maps, core_ids=list(range(B)))
    return np.stack([np.asarray(r["out"], dtype=np.float32) for r in res.results], axis=0)
```
